# Optimizing a Trainium2 kernel written in Bass

```python
import math
import jax
import jax.numpy as jnp
from jax import lax
import numpy as np

D_MODEL = 2048
BATCH = 1
SEQ = 16384
DEPTH = 4

N_META = 16
M_HEADS = 4
M_QK = D_MODEL // 16
M_V = D_MODEL // 8
M_CHUNK = 64
CONV_W = 4
A_HEADS = 8
A_DIM = D_MODEL // 16
Q_BLOCK = 128
TOPK_MAX = 256
IDX_HEADS = 8
IDX_DIM = 64
REL_BUCKETS = 32
REL_MAX_DIST = 128
N_GROUPS = 4
EXP_PER_GROUP = 8
N_EXPERTS = N_GROUPS * EXP_PER_GROUP
TOP_K_INNER = 2
D_FF = D_MODEL // 4
MOE_BLOCK = 128
ALPHA = (2 * DEPTH) ** 0.25
BETA = (8 * DEPTH) ** -0.25
LN_EPS = 1e-5
NEG = -1e30
BIG = 1e30

M_QK_W = M_HEADS * M_QK
M_V_W = M_HEADS * M_V
A_W = A_HEADS * A_DIM
IDX_Q_W = IDX_HEADS * IDX_DIM
IN_SPLITS = (M_QK_W, M_QK_W, M_V_W, M_V_W, M_HEADS, M_HEADS,
             A_W, A_W, A_W, IDX_Q_W, IDX_DIM, IDX_HEADS, D_MODEL, D_MODEL)
IN_W = sum(IN_SPLITS)

kernel_name = 'hybrid_mlstm_dsa_hiermoe_trunk'


def layer_norm(x, g, b):
    xf = x.astype(jnp.float32)
    mu = jnp.mean(xf, axis=-1, keepdims=True)
    var = jnp.mean(jnp.square(xf - mu), axis=-1, keepdims=True)
    y = (xf - mu) * lax.rsqrt(var + LN_EPS) * g.astype(jnp.float32) + b.astype(jnp.float32)
    return y.astype(x.dtype)


def split_cols(a, sizes):
    out, off = [], 0
    for s in sizes:
        out.append(a[..., off:off + s])
        off += s
    return out


def causal_dwconv(u, w, b):
    y = lax.conv_general_dilated(u, w[:, None, :].astype(u.dtype), window_strides=(1,),
                                 padding=[(CONV_W - 1, 0)],
                                 dimension_numbers=('NWC', 'WIO', 'NWC'),
                                 feature_group_count=u.shape[-1])
    return y + b.astype(u.dtype)


def t5_bucket(rel):
    rel = jnp.maximum(rel, 0)
    max_exact = REL_BUCKETS // 2
    rel_f = jnp.maximum(rel, 1).astype(jnp.float32)
    large = max_exact + (jnp.log(rel_f / max_exact) / math.log(REL_MAX_DIST / max_exact)
                         * (REL_BUCKETS - max_exact)).astype(jnp.int32)
    large = jnp.minimum(large, REL_BUCKETS - 1)
    return jnp.where(rel < max_exact, rel, large)


def mlstm_branch(mq, mk, mv, mo, mi, mf, b_ig, b_fg, mh_g):
    B, L, _ = mq.shape
    f32 = jnp.float32
    q = mq.reshape(B, L, M_HEADS, M_QK).astype(f32) * (M_QK ** -0.5)
    k = mk.reshape(B, L, M_HEADS, M_QK).astype(f32)
    v = mv.reshape(B, L, M_HEADS, M_V).astype(f32)
    ig = mi.astype(f32) + b_ig.astype(f32)
    lf = jax.nn.log_sigmoid(mf.astype(f32) + b_fg.astype(f32))
    p0 = (-N_META) % M_CHUNK
    p1 = (-(L + p0)) % M_CHUNK
    nc = (L + p0 + p1) // M_CHUNK

    def to_chunks(a, fill):
        a = jnp.pad(a, [(0, 0), (p0, p1)] + [(0, 0)] * (a.ndim - 2), constant_values=fill)
        a = a.reshape((B, nc, M_CHUNK) + a.shape[2:])
        return jnp.moveaxis(jnp.moveaxis(a, 1, 0), 3, 2)

    xs = (to_chunks(q, 0.0), to_chunks(k, 0.0), to_chunks(v, 0.0),
          to_chunks(ig, NEG), to_chunks(lf, 0.0))
    causal = jnp.tril(jnp.ones((M_CHUNK, M_CHUNK), dtype=bool))

    def step(carry, chunk):
        C, n, m = carry
        qc, kc, vc, igc, lfc = chunk
        b = jnp.cumsum(lfc, axis=-1)
        dmat = jnp.where(causal, b[..., :, None] - b[..., None, :] + igc[..., None, :], -jnp.inf)
        inter = b + m[..., None]
        m_t = jnp.maximum(jnp.max(dmat, axis=-1), inter)
        w_intra = jnp.exp(dmat - m_t[..., None]) * jnp.einsum('bhtd,bhsd->bhts', qc, kc)
        w_inter = jnp.exp(inter - m_t)
        num = (jnp.einsum('bhts,bhsv->bhtv', w_intra, vc)
               + w_inter[..., None] * jnp.einsum('bhtd,bhdv->bhtv', qc, C))
        den = jnp.sum(w_intra, axis=-1) + w_inter * jnp.einsum('bhtd,bhd->bht', qc, n)
        h = num / jnp.maximum(jnp.abs(den), jnp.exp(-m_t))[..., None]
        b_last = b[..., -1]
        dec = b_last[..., None] - b + igc
        m_new = jnp.maximum(b_last + m, jnp.max(dec, axis=-1))
        w_src = jnp.exp(dec - m_new[..., None])
        carry_scale = jnp.exp(b_last + m - m_new)
        C = carry_scale[..., None, None] * C + jnp.einsum('bhs,bhsd,bhsv->bhdv', w_src, kc, vc)
        n = carry_scale[..., None] * n + jnp.einsum('bhs,bhsd->bhd', w_src, kc)
        return (C, n, m_new), h

    init = (jnp.zeros((B, M_HEADS, M_QK, M_V), f32),
            jnp.zeros((B, M_HEADS, M_QK), f32),
            jnp.zeros((B, M_HEADS), f32))
    _, h = lax.scan(step, init, xs)
    h = jnp.moveaxis(jnp.moveaxis(h, 0, 1), 2, 3).reshape(B, nc * M_CHUNK, M_HEADS, M_V)[:, p0:p0 + L]
    mu = jnp.mean(h, axis=-1, keepdims=True)
    var = jnp.mean(jnp.square(h - mu), axis=-1, keepdims=True)
    h = (h - mu) * lax.rsqrt(var + LN_EPS) * mh_g.reshape(M_HEADS, M_V).astype(f32)
    return (jax.nn.sigmoid(mo.astype(f32)) * h.reshape(B, L, M_V_W)).astype(mq.dtype)


def sparse_attention(q, k, v, qi, ki, wi, rel_bias, topk):
    B, L = q.shape[0], q.shape[1]
    nb = -(-L // Q_BLOCK)
    Lp = nb * Q_BLOCK

    def pad_q(a):
        return jnp.pad(a, [(0, 0), (0, Lp - L)] + [(0, 0)] * (a.ndim - 2))

    qp, qip, wip = pad_q(q), pad_q(qi), pad_q(wi)
    key_pos = jnp.arange(L, dtype=jnp.int32)
    scale = A_DIM ** -0.5

    def block(bidx):
        start = bidx * Q_BLOCK
        qb = lax.dynamic_slice_in_dim(qp, start, Q_BLOCK, axis=1)
        qib = lax.dynamic_slice_in_dim(qip, start, Q_BLOCK, axis=1)
        wib = lax.dynamic_slice_in_dim(wip, start, Q_BLOCK, axis=1)
        q_pos = start + jnp.arange(Q_BLOCK, dtype=jnp.int32)
        s_h = jax.nn.relu(jnp.einsum('bqhd,bsd->bqhs', qib, ki))
        s_idx = jnp.einsum('bqh,bqhs->bqs', wib, s_h).astype(jnp.float32)
        visible = key_pos[None, :] <= q_pos[:, None]
        s_idx = jnp.where(visible, jnp.where(key_pos[None, :] < N_META, BIG, s_idx), NEG)
        vals, sel = lax.top_k(s_idx, topk)
        valid = vals > 0.5 * NEG
        k_sel = jax.vmap(lambda kb, ib: kb[ib])(k, sel)
        v_sel = jax.vmap(lambda vb, ib: vb[ib])(v, sel)
        logits = jnp.einsum('bqhd,bqkhd->bqhk', qb, k_sel).astype(jnp.float32) * scale
        bias = rel_bias[t5_bucket(q_pos[None, :, None] - sel)].astype(jnp.float32)
        logits = logits + jnp.moveaxis(bias, -1, 2)
        logits = jnp.where(valid[:, :, None, :], logits, -jnp.inf)
        p = jax.nn.softmax(logits, axis=-1)
        return jnp.einsum('bqhk,bqkhd->bqhd', p.astype(v.dtype), v_sel)

    outs = lax.map(block, jnp.arange(nb, dtype=jnp.int32))
    outs = jnp.moveaxis(outs, 0, 1).reshape(B, Lp, A_HEADS, A_DIM)
    return outs[:, :L]


def mixer(x, w_in, conv_w, conv_b, b_ig, b_fg, mh_g, w_pm, w_pa, w_out, rel_bias, topk):
    B, L, _ = x.shape
    (mq, mk, mv, mo, mi, mf, aq, ak, av, iq, ik, iw, gm, ga) = split_cols(x @ w_in, IN_SPLITS)
    qk = jax.nn.silu(causal_dwconv(jnp.concatenate([mq, mk], axis=-1), conv_w, conv_b))
    mq, mk = qk[..., :M_QK_W], qk[..., M_QK_W:]
    y_m = mlstm_branch(mq, mk, mv, mo, mi, mf, b_ig, b_fg, mh_g)
    y_a = sparse_attention(aq.reshape(B, L, A_HEADS, A_DIM),
                           ak.reshape(B, L, A_HEADS, A_DIM),
                           av.reshape(B, L, A_HEADS, A_DIM),
                           iq.reshape(B, L, IDX_HEADS, IDX_DIM) * (IDX_DIM ** -0.5),
                           ik, iw * (IDX_HEADS ** -0.5), rel_bias, topk)
    merged = (jax.nn.sigmoid(gm) * (y_m @ w_pm)
              + jax.nn.sigmoid(ga) * (y_a.reshape(B, L, A_W) @ w_pa))
    return merged @ w_out


def hier_moe(x2, w_group, b_group, w_router, b_router, w_gate_up, w_down):
    N = x2.shape[0]
    f32 = jnp.float32
    g_logits = (x2 @ w_group).astype(f32) + b_group.astype(f32)
    g_prob = jax.nn.softmax(g_logits, axis=-1)
    g_sel = jnp.argmax(g_logits, axis=-1).astype(jnp.int32)
    e_logits = ((x2 @ w_router).astype(f32) + b_router.astype(f32)).reshape(N, N_GROUPS, EXP_PER_GROUP)
    e_in_group = jnp.take_along_axis(e_logits, g_sel[:, None, None], axis=1)[:, 0]
    top_v, top_i = lax.top_k(e_in_group, TOP_K_INNER)
    gate = jax.nn.softmax(top_v, axis=-1) * jnp.take_along_axis(g_prob, g_sel[:, None], axis=1)
    expert = g_sel[:, None] * EXP_PER_GROUP + top_i
    A = N * TOP_K_INNER
    e_flat = expert.reshape(A)
    tok_flat = jnp.repeat(jnp.arange(N, dtype=jnp.int32), TOP_K_INNER)
    w_flat = gate.reshape(A)
    order = jnp.argsort(e_flat)
    e_s, tok_s, w_s = e_flat[order], tok_flat[order], w_flat[order]
    counts = jnp.zeros((N_EXPERTS,), jnp.int32).at[e_flat].add(1)
    start = jnp.cumsum(counts) - counts
    padded = (counts + MOE_BLOCK - 1) // MOE_BLOCK * MOE_BLOCK
    pend = jnp.cumsum(padded)
    pstart = pend - padded
    dest = pstart[e_s] + (jnp.arange(A, dtype=jnp.int32) - start[e_s])
    n_blocks = -(-A // MOE_BLOCK) + N_EXPERTS
    R = n_blocks * MOE_BLOCK
    row_tok = jnp.zeros((R,), jnp.int32).at[dest].set(tok_s)
    row_w = jnp.zeros((R,), f32).at[dest].set(w_s)
    blk_e = jnp.minimum(jnp.searchsorted(pend, jnp.arange(n_blocks, dtype=jnp.int32) * MOE_BLOCK,
                                         side='right'), N_EXPERTS - 1).astype(jnp.int32)

    def run_block(args):
        toks, ws, e = args
        xb = x2[toks]
        gu = xb @ w_gate_up[e]
        hb = jax.nn.silu(gu[:, :D_FF]) * gu[:, D_FF:]
        return ((hb @ w_down[e]) * ws[:, None]).astype(x2.dtype)

    ys = lax.map(run_block, (row_tok.reshape(n_blocks, MOE_BLOCK),
                             row_w.reshape(n_blocks, MOE_BLOCK), blk_e))
    return jax.ops.segment_sum(ys.reshape(R, x2.shape[1]), row_tok, num_segments=N)


def setup_inputs(seed: int = 0) -> dict:
    key = jax.random.key(seed)
    ks = jax.random.split(key, 26)
    D, NL = D_MODEL, DEPTH

    def nrm(k, shape, s):
        return jax.random.normal(k, shape, jnp.float32) * s

    return {
        'x': nrm(ks[0], (BATCH, SEQ, D), 1.0),
        'meta_tokens': nrm(ks[1], (N_META, D), 1.0),
        'ln_emb_g': 1.0 + nrm(ks[2], (D,), 0.02),
        'ln_emb_b': nrm(ks[3], (D,), 0.02),
        'rel_bias': nrm(ks[4], (REL_BUCKETS, A_HEADS), 0.5),
        'w_in': nrm(ks[5], (NL, D, IN_W), D ** -0.5),
        'conv_w': nrm(ks[6], (NL, CONV_W, 2 * M_QK_W), CONV_W ** -0.5),
        'conv_b': nrm(ks[7], (NL, 2 * M_QK_W), 0.02),
        'b_igate': nrm(ks[8], (NL, M_HEADS), 0.1),
        'b_fgate': 3.0 + 3.0 * jax.random.uniform(ks[9], (NL, M_HEADS), jnp.float32),
        'mh_norm_g': 1.0 + nrm(ks[10], (NL, M_V_W), 0.02),
        'w_proj_m': nrm(ks[11], (NL, M_V_W, D), M_V_W ** -0.5),
        'w_proj_a': nrm(ks[12], (NL, A_W, D), A_W ** -0.5),
        'w_out': nrm(ks[13], (NL, D, D), BETA * D ** -0.5),
        'ln1_g': 1.0 + nrm(ks[14], (NL, D), 0.02),
        'ln1_b': nrm(ks[15], (NL, D), 0.02),
        'w_group': nrm(ks[16], (NL, D, N_GROUPS), D ** -0.5),
        'b_group': nrm(ks[17], (NL, N_GROUPS), 0.01),
        'w_router': nrm(ks[18], (NL, D, N_EXPERTS), D ** -0.5),
        'b_router': nrm(ks[19], (NL, N_EXPERTS), 0.01),
        'w_gate_up': nrm(ks[20], (NL, N_EXPERTS, D, 2 * D_FF), D ** -0.5),
        'w_down': nrm(ks[21], (NL, N_EXPERTS, D_FF, D), BETA * D_FF ** -0.5),
        'ln2_g': 1.0 + nrm(ks[22], (NL, D), 0.02),
        'ln2_b': nrm(ks[23], (NL, D), 0.02),
    }


def reference(x, meta_tokens, ln_emb_g, ln_emb_b, rel_bias, w_in, conv_w, conv_b, b_igate,
              b_fgate, mh_norm_g, w_proj_m, w_proj_a, w_out, ln1_g, ln1_b, w_group, b_group,
              w_router, b_router, w_gate_up, w_down, ln2_g, ln2_b):
    B = x.shape[0]
    meta = jnp.broadcast_to(meta_tokens[None].astype(x.dtype), (B, N_META, D_MODEL))
    h = layer_norm(jnp.concatenate([meta, x], axis=1), ln_emb_g, ln_emb_b)
    L = h.shape[1]
    topk = min(TOPK_MAX, L // 4)
    for l in range(DEPTH):
        mix = mixer(h, w_in[l], conv_w[l], conv_b[l], b_igate[l], b_fgate[l], mh_norm_g[l],
                    w_proj_m[l], w_proj_a[l], w_out[l], rel_bias, topk)
        h = layer_norm(ALPHA * h + mix, ln1_g[l], ln1_b[l])
        ffn = hier_moe(h.reshape(B * L, D_MODEL), w_group[l], b_group[l], w_router[l],
                       b_router[l], w_gate_up[l], w_down[l]).reshape(B, L, D_MODEL)
        h = layer_norm(ALPHA * h + ffn, ln2_g[l], ln2_b[l])
    return h[:, N_META:]
```

```python
import numpy as np
import ml_dtypes
from contextlib import ExitStack
import concourse.bass as bass
import concourse.mybir as mybir
from concourse.bass_utils import run_bass_kernel_spmd

F32 = mybir.dt.float32
BF16 = mybir.dt.bfloat16
ALU = mybir.AluOpType
AF = mybir.ActivationFunctionType
AX = mybir.AxisListType
NPBF = ml_dtypes.bfloat16

NCORES = 8
D = 2048
SEQ = 16384
N_META = 16
L = SEQ + N_META
TPC = 17
LP = NCORES * TPC * 128
NT = LP // 128
DEPTH = 4
IN_W = 10832
ALPHA = (2 * DEPTH) ** 0.25
LN_EPS = 1e-5


class T:
    __slots__ = ("t", "name", "w", "r")

    def __init__(self, t, name):
        self.t = t
        self.name = name
        self.w = None
        self.r = {}

    def __getitem__(self, idx):
        return self.t[idx]


class K:
    NDMA = 24

    def __init__(self):
        self.nc = bass.Bass("TRN2", target_bir_lowering=False)
        nc = self.nc
        self.es = ExitStack()
        self.eng = {"pe": nc.tensor, "act": nc.scalar, "dve": nc.vector,
                    "pool": nc.gpsimd, "sp": nc.sync}
        self.sem = {e: self.es.enter_context(nc.semaphore("s_" + e)) for e in self.eng}
        self.cnt = {e: 0 for e in self.eng}
        self.waited = {e: {} for e in self.eng}
        self.dsem = [self.es.enter_context(nc.semaphore("d%d" % i)) for i in range(self.NDMA)]
        self.dcnt = [0] * self.NDMA
        self.dnext = 0
        self.out_tickets = []
        self.npsum = 0

    def dram(self, name, shape, dt, kind):
        return self.nc.dram_tensor(name, list(shape), dt, kind=kind).ap()

    def sb(self, name, shape, dt, st=None):
        self.uid = getattr(self, "uid", 0) + 1
        t = (st or self.es).enter_context(self.nc.sbuf_tensor("%s_%d" % (name, self.uid), list(shape), dt))
        return T(t, name)

    def ps(self, name, shape, dt=F32, st=None):
        self.uid = getattr(self, "uid", 0) + 1
        t = (st or self.es).enter_context(self.nc.psum_tensor("%s_%d" % (name, self.uid), list(shape), dt))
        return T(t, name)

    def barrier(self):
        for e in self.eng:
            for e2 in self.eng:
                if self.cnt[e2] and not (e2 == e and e == "pe"):
                    self._wait(e, ("e", e2), self.cnt[e2])
            for i in range(self.NDMA):
                if self.dcnt[i]:
                    self._wait(e, ("d", i), self.dcnt[i])

    def _wait(self, e, key, val):
        if self.waited[e].get(key, 0) >= val:
            return
        if key[0] == "e":
            if key[1] == e and e == "pe":
                return
            sem = self.sem[key[1]]
        else:
            sem = self.dsem[key[1]]
        self.eng[e].wait_ge(sem, val)
        self.waited[e][key] = val

    def _deps(self, e, reads, writes):
        deps = {}
        for t in reads:
            if t.w is not None:
                k, v = t.w
                deps[k] = max(deps.get(k, 0), v)
        for t in writes:
            if t.w is not None:
                k, v = t.w
                deps[k] = max(deps.get(k, 0), v)
            for k, v in t.r.items():
                deps[k] = max(deps.get(k, 0), v)
        for k, v in deps.items():
            self._wait(e, k, v)

    def _mark(self, tk, reads, writes):
        k, v = tk
        for t in reads:
            t.r[k] = max(t.r.get(k, 0), v)
        for t in writes:
            t.w = tk
            t.r = {}

    def op(self, e, fn, reads=(), writes=()):
        self._deps(e, reads, writes)
        ins = fn(self.eng[e])
        self.cnt[e] += 1
        ins.then_inc(self.sem[e], 1)
        self._mark((("e", e), self.cnt[e]), reads, writes)

    def dma(self, e, out, in_, reads=(), writes=(), is_output=False):
        self._deps(e, reads, writes)
        i = self.dnext
        self.dnext = (self.dnext + 1) % self.NDMA
        ins = self.eng[e].dma_start(out=out, in_=in_)
        self.dcnt[i] += 16
        ins.then_inc(self.dsem[i], 16)
        tk = (("d", i), self.dcnt[i])
        self._mark(tk, reads, writes)
        if is_output:
            self.out_tickets.append(tk)

    def finish(self):
        fin = {}
        for k, v in self.out_tickets:
            fin[k] = max(fin.get(k, 0), v)
        for k, v in fin.items():
            self._wait("sp", k, v)
        for e in ("pe", "act", "dve", "pool"):
            if self.cnt[e]:
                self._wait("sp", ("e", e), self.cnt[e])
        self.es.close()
        return self.nc


def run(k, in_maps):
    res = run_bass_kernel_spmd(k.nc, in_maps, core_ids=list(range(len(in_maps))))
    return res.results


def _p1_blocks():
    blocks = []
    segs = [(0, 1024, False),
            (1024, 1024, True),
            (2048, 1024, False),
            (3072, 8, False),
            (3080, 3072 + 512 + 64, True),
            (6728, 8, False),
            (6736, 4096, False)]
    for s, w, b in segs:
        o = 0
        while o < w:
            ww = min(512, w - o)
            blocks.append((s + o, ww, b))
            o += ww
    return blocks


P1_BLOCKS = _p1_blocks()
N32 = sum(w for _, w, b in P1_BLOCKS if not b)
N16 = sum(w for _, w, b in P1_BLOCKS if b)


def build_p1(do_ln=True, do_proj=True):
    k = K()
    NTOK = TPC * 128
    xin = k.dram("xin", [NTOK, D], F32, "ExternalInput")
    g_rep = k.dram("g_rep", [128, D], F32, "ExternalInput")
    b_rep = k.dram("b_rep", [128, D], F32, "ExternalInput")
    ident_d = k.dram("ident", [128, 128], F32, "ExternalInput")
    h_out = k.dram("h", [NTOK, D], F32, "ExternalOutput")
    if do_proj:
        w = k.dram("w", [D, IN_W], F32, "ExternalInput")
        o32 = k.dram("o32", [NTOK, N32], F32, "ExternalOutput")
        o16 = k.dram("o16", [NTOK, N16], BF16, "ExternalOutput")

    gt = k.sb("gt", [128, D], F32)
    bt = k.sb("bt", [128, D], F32)
    idf = k.sb("idf", [128, 128], F32)
    idb = k.sb("idb", [128, 128], BF16)
    k.dma("sp", gt[:], g_rep, writes=[gt])
    k.dma("sp", bt[:], b_rep, writes=[bt])
    k.dma("sp", idf[:], ident_d, writes=[idf])
    k.op("dve", lambda e: e.tensor_copy(out=idb[:], in_=idf[:]), reads=[idf], writes=[idb])

    hT = k.sb("hT", [128, 16, NTOK], BF16) if do_proj else None
    xt = [k.sb("xt%d" % i, [128, D], F32) for i in range(2)]
    yt = [k.sb("yt%d" % i, [128, D], F32) for i in range(2)]
    yb = [k.sb("yb%d" % i, [128, D], BF16) for i in range(2)]
    sq = k.sb("sq", [128, D], BF16)
    st = [k.sb("st%d" % i, [128, 8], F32) for i in range(2)]
    pst = [k.ps("pst%d" % i, [128, 4, 128], BF16) for i in range(2)]
    pmm = [k.ps("pmm%d" % i, [128, 512], F32) for i in range(4)]

    for ti in range(TPC):
        x = xt[ti % 2]
        y = yt[ti % 2]
        s = st[ti % 2]
        k.dma("sp", x[:], xin[ti * 128:(ti + 1) * 128, :], writes=[x])
        if do_ln:
            k.op("dve", lambda e: e.tensor_reduce(out=s[:, 0:1], in_=x[:], op=ALU.add, axis=AX.X),
                 reads=[x], writes=[s])
            k.op("act", lambda e: e.activation(out=sq[:], in_=x[:], func=AF.Square, accum_out=s[:, 1:2]),
                 reads=[x], writes=[sq, s])
            k.op("dve", lambda e: e.tensor_scalar(out=s[:, 2:4], in0=s[:, 0:2], scalar1=1.0 / D, scalar2=None,
                                                  op0=ALU.mult), reads=[s], writes=[s])
            k.op("dve", lambda e: e.tensor_tensor(out=s[:, 4:5], in0=s[:, 2:3], in1=s[:, 2:3], op=ALU.mult),
                 reads=[s], writes=[s])
            k.op("dve", lambda e: e.tensor_tensor(out=s[:, 5:6], in0=s[:, 3:4], in1=s[:, 4:5], op=ALU.subtract),
                 reads=[s], writes=[s])
            k.op("dve", lambda e: e.tensor_scalar(out=s[:, 5:6], in0=s[:, 5:6], scalar1=LN_EPS, scalar2=None,
                                                  op0=ALU.add), reads=[s], writes=[s])
            k.op("act", lambda e: e.activation(out=s[:, 6:7], in_=s[:, 5:6], func=AF.Sqrt),
                 reads=[s], writes=[s])
            k.op("dve", lambda e: e.reciprocal(out=s[:, 7:8], in_=s[:, 6:7]), reads=[s], writes=[s])
            k.op("dve", lambda e: e.tensor_scalar(out=y[:], in0=x[:], scalar1=s[:, 2:3], scalar2=s[:, 7:8],
                                                  op0=ALU.subtract, op1=ALU.mult), reads=[x, s], writes=[y])
            k.op("pool", lambda e: e.tensor_tensor(out=y[:], in0=y[:], in1=gt[:], op=ALU.mult),
                 reads=[y, gt], writes=[y])
            k.op("dve", lambda e: e.tensor_tensor(out=y[:], in0=y[:], in1=bt[:], op=ALU.add),
                 reads=[y, bt], writes=[y])
        else:
            k.op("dve", lambda e: e.tensor_copy(out=y[:], in_=x[:]), reads=[x], writes=[y])
        k.dma("pool", h_out[ti * 128:(ti + 1) * 128, :], y[:], reads=[y], is_output=True)
        if do_proj:
            ybt = yb[ti % 2]
            k.op("act", lambda e: e.copy(out=ybt[:], in_=y[:]), reads=[y], writes=[ybt])
            for g4 in range(4):
                p = pst[g4 % 2]
                for j in range(4):
                    c = g4 * 4 + j
                    k.op("pe", lambda e: e.transpose(out=p[:, j, :], in_=ybt[:, c * 128:(c + 1) * 128],
                                                     identity=idb[:]), reads=[ybt, idb], writes=[p])
                k.op("dve", lambda e: e.tensor_copy(out=hT[:, g4 * 4:(g4 + 1) * 4, ti * 128:(ti + 1) * 128],
                                                    in_=p[:]), reads=[p], writes=[hT])

    if do_proj:
        wst = [k.sb("wst%d" % i, [128, 8, 512], F32) for i in range(2)]
        wbf = [k.sb("wbf%d" % i, [128, 16, 512], BF16) for i in range(2)]
        ot32 = [k.sb("ot32_%d" % i, [128, 512], F32) for i in range(3)]
        ot16 = [k.sb("ot16_%d" % i, [128, 512], BF16) for i in range(3)]
        wv = w.rearrange("(c p) n -> p c n", p=128)
        c32 = 0
        c16 = 0
        nev = 0
        for bi, (cs, cw, isb) in enumerate(P1_BLOCKS):
            wb = wbf[bi % 2]
            for half in range(2):
                ws = wst[half]
                k.dma("sp" if half == 0 else "act", ws[:, :, 0:cw],
                      wv[:, half * 8:(half + 1) * 8, cs:cs + cw], writes=[ws])
                k.op("pool" if half == 0 else "dve",
                     lambda e: e.tensor_copy(out=wb[:, half * 8:(half + 1) * 8, 0:cw], in_=ws[:, :, 0:cw]),
                     reads=[ws], writes=[wb])
            for ti in range(TPC):
                p = pmm[nev % 4]
                for c in range(16):
                    k.op("pe", lambda e: e.matmul(p[:, 0:cw], lhsT=hT[:, c, ti * 128:(ti + 1) * 128],
                                                  rhs=wb[:, c, 0:cw], start=(c == 0), stop=(c == 15)),
                         reads=[hT, wb], writes=[p])
                if isb:
                    o = ot16[nev % 3]
                    dst = o16[ti * 128:(ti + 1) * 128, c16:c16 + cw]
                else:
                    o = ot32[nev % 3]
                    dst = o32[ti * 128:(ti + 1) * 128, c32:c32 + cw]
                if nev % 2 == 0:
                    k.op("act", lambda e: e.copy(out=o[:, 0:cw], in_=p[:, 0:cw]), reads=[p], writes=[o])
                else:
                    k.op("dve", lambda e: e.tensor_copy(out=o[:, 0:cw], in_=p[:, 0:cw]), reads=[p], writes=[o])
                k.dma("pool", dst, o[:, 0:cw], reads=[o], is_output=True)
                nev += 1
            if isb:
                c16 += cw
            else:
                c32 += cw
    k.finish()
    return k


NSLOT = 17
KCH = 16
TOPK_SEL = 240
REPL = -3.0e38


def build_at(slots=None, heads=None):
    slots = list(range(NSLOT)) if slots is None else slots
    heads = list(range(8)) if heads is None else heads
    k = K()
    qT_d = k.dram("qT", [NSLOT, 128, 8, 128], BF16, "ExternalInput")
    iq_d = k.dram("iqT", [NSLOT, 64, 8, 128], BF16, "ExternalInput")
    iw_d = k.dram("iw", [NSLOT, 128, 8], F32, "ExternalInput")
    kT_d = k.dram("kT", [8, 128, LP], BF16, "ExternalInput")
    v_d = k.dram("v", [8, 128, NT, 128], BF16, "ExternalInput")
    ik_d = k.dram("ikT", [64, LP], BF16, "ExternalInput")
    fb_d = k.dram("fb", [8, 128, 9, 128], F32, "ExternalInput")
    ub_d = k.dram("ub", [128, 1024], F32, "ExternalInput")
    rb_d = k.dram("rb31", [128, 8], F32, "ExternalInput")
    id_d = k.dram("ident", [128, 128], F32, "ExternalInput")
    out_d = k.dram("ya", [NSLOT, 8, 128, 128], BF16, "ExternalOutput")

    sidx = k.sb("sidx", [128, LP], F32)
    maskT = k.sb("maskT", [128, NT, 128], BF16)
    ikT = k.sb("ikT_s", [64, LP], BF16)
    kch = [k.sb("kch%d" % i, [128, KCH * 128], BF16) for i in range(2)]
    vch = [k.sb("vch%d" % i, [128, KCH, 128], BF16) for i in range(2)]
    qs = k.sb("qs", [128, 8, 128], BF16)
    iqs = k.sb("iqs", [64, 8, 128], BF16)
    iws = k.sb("iws", [128, 8], F32)
    fe = k.sb("fe", [128, 8, 9, 128], BF16)
    fbs = k.sb("fbs", [128, 9, 128], F32)
    ubs = k.sb("ubs", [128, 1024], F32)
    rb = k.sb("rb", [128, 8], F32)
    nrb = k.sb("nrb", [128, 8], F32)
    idf = k.sb("idf", [128, 128], F32)
    onesb = k.sb("onesb", [128, 128], BF16)
    rl = [k.sb("rl%d" % i, [128, 512], F32) for i in range(2)]
    pex = [k.sb("pex%d" % i, [128, 4, 128], BF16) for i in range(2)]
    m8 = k.sb("m8", [128, 8], F32)
    rden = k.sb("rden", [128, 128], F32)
    ot = [k.sb("ot%d" % i, [128, 128], BF16) for i in range(2)]

    pidx = [k.ps("pidx%d" % i, [128, 512], F32) for i in range(2)]
    ptr = k.ps("ptr", [128, 4, 128], F32)
    pst = [k.ps("pst%d" % i, [128, 4, 128], F32) for i in range(2)]
    py = k.ps("py", [128, 128], F32)
    pden = k.ps("pden", [128, 128], F32)

    k.dma("sp", ikT[:], ik_d, writes=[ikT])
    k.dma("sp", ubs[:], ub_d, writes=[ubs])
    k.dma("sp", rb[:], rb_d, writes=[rb])
    k.dma("sp", idf[:], id_d, writes=[idf])
    k.op("dve", lambda e: e.tensor_scalar(out=nrb[:], in0=rb[:], scalar1=-1.0, scalar2=None, op0=ALU.mult),
         reads=[rb], writes=[nrb])
    k.op("dve", lambda e: e.memset(onesb[:], 1.0), writes=[onesb])
    for h in range(8):
        k.dma("sp", fbs[:], fb_d[h], writes=[fbs])
        k.op("act", lambda e: e.activation(out=fe[:, h, :, :], in_=fbs[:], func=AF.Exp, bias=nrb[:, h:h + 1], scale=1.0),
             reads=[fbs, nrb], writes=[fe])

    scale = 128 ** -0.5
    nch = 0
    nev = 0
    for kslot in slots:
        nk = 8 * kslot + 8
        nkeys = nk * 128
        k.dma("sp", qs[:], qT_d[kslot], writes=[qs])
        k.dma("sp", iqs[:], iq_d[kslot], writes=[iqs])
        k.dma("sp", iws[:], iw_d[kslot], writes=[iws])
        for blk in range(nk // 4):
            cs = blk * 512
            for h in range(8):
                p = pidx[nev % 2]
                r = rl[nev % 2]
                nev += 1
                k.op("pe", lambda e: e.matmul(p[:], lhsT=iqs[:, h, :], rhs=ikT[:, cs:cs + 512], start=True, stop=True),
                     reads=[iqs, ikT], writes=[p])
                k.op("act", lambda e: e.activation(out=r[:], in_=p[:], func=AF.Relu), reads=[p], writes=[r])
                if h == 0:
                    k.op("dve", lambda e: e.tensor_scalar(out=sidx[:, cs:cs + 512], in0=r[:], scalar1=iws[:, 0:1],
                                                          scalar2=None, op0=ALU.mult), reads=[r, iws], writes=[sidx])
                else:
                    k.op("dve", lambda e: e.scalar_tensor_tensor(out=sidx[:, cs:cs + 512], in0=r[:], scalar=iws[:, h:h + 1],
                                                                 in1=sidx[:, cs:cs + 512], op0=ALU.mult, op1=ALU.add),
                         reads=[r, iws, sidx], writes=[sidx])
        k.op("dve", lambda e: e.tensor_tensor(out=sidx[:, nkeys - 1024:nkeys], in0=sidx[:, nkeys - 1024:nkeys],
                                              in1=ubs[:], op=ALU.min), reads=[sidx, ubs], writes=[sidx])
        hi = min(nkeys, L)
        for rnd in range(TOPK_SEL // 8):
            k.op("dve", lambda e: e.max(out=m8[:], in_=sidx[:, 16:hi]), reads=[sidx], writes=[m8])
            k.op("dve", lambda e: e.match_replace(out=sidx[:, 16:hi], in_to_replace=m8[:], in_values=sidx[:, 16:hi],
                                                  imm_value=REPL), reads=[sidx, m8], writes=[sidx])
        k.op("dve", lambda e: e.tensor_scalar(out=sidx[:, 16:hi], in0=sidx[:, 16:hi], scalar1=-2.0e38, scalar2=None,
                                              op0=ALU.is_le), reads=[sidx], writes=[sidx])
        k.op("dve", lambda e: e.memset(sidx[:, 0:16], 1.0), writes=[sidx])
        if hi < nkeys:
            k.op("dve", lambda e: e.memset(sidx[:, hi:nkeys], 0.0), writes=[sidx])
        for g in range(nk // 4):
            for j in range(4):
                kt = g * 4 + j
                k.op("pe", lambda e: e.transpose(out=ptr[:, j, :], in_=sidx[:, kt * 128:(kt + 1) * 128], identity=idf[:]),
                     reads=[sidx, idf], writes=[ptr])
            if g % 2 == 0:
                k.op("act", lambda e: e.copy(out=maskT[:, g * 4:(g + 1) * 4, :], in_=ptr[:]), reads=[ptr], writes=[maskT])
            else:
                k.op("dve", lambda e: e.tensor_copy(out=maskT[:, g * 4:(g + 1) * 4, :], in_=ptr[:]), reads=[ptr], writes=[maskT])
        for h in heads:
            first = True
            for c0 in range(0, nk, KCH):
                cn = min(KCH, nk - c0)
                kc = kch[nch % 2]
                vc = vch[nch % 2]
                nch += 1
                k.dma("sp", kc[:, 0:cn * 128], kT_d[h, :, c0 * 128:(c0 + cn) * 128], writes=[kc])
                k.dma("act", vc[:, 0:cn, :], v_d[h, :, c0:c0 + cn, :], writes=[vc])
                for g in range(cn // 4):
                    ps = pst[nev % 2]
                    px = pex[nev % 2]
                    nev += 1
                    for j in range(4):
                        kt = g * 4 + j
                        k.op("pe", lambda e: e.matmul(ps[:, j, :], lhsT=kc[:, kt * 128:(kt + 1) * 128], rhs=qs[:, h, :],
                                                      start=True, stop=True), reads=[kc, qs], writes=[ps])
                    k.op("act", lambda e: e.activation(out=px[:], in_=ps[:], func=AF.Exp, bias=rb[:, h:h + 1], scale=scale),
                         reads=[ps, rb], writes=[px])
                    gt0 = c0 + g * 4
                    k.op("dve", lambda e: e.tensor_tensor(out=px[:], in0=px[:], in1=maskT[:, gt0:gt0 + 4, :], op=ALU.mult),
                         reads=[px, maskT], writes=[px])
                    for j in range(4):
                        m = gt0 + j - (nk - 9)
                        if m >= 0:
                            k.op("dve", lambda e: e.tensor_tensor(out=px[:, j, :], in0=px[:, j, :], in1=fe[:, h, m, :], op=ALU.mult),
                                 reads=[px, fe], writes=[px])
                    for j in range(4):
                        kt = g * 4 + j
                        last = (c0 + kt == nk - 1)
                        k.op("pe", lambda e: e.matmul(py[:], lhsT=vc[:, kt, :], rhs=px[:, j, :], start=first, stop=last),
                             reads=[vc, px], writes=[py])
                        k.op("pe", lambda e: e.matmul(pden[:], lhsT=onesb[:], rhs=px[:, j, :], start=first, stop=last),
                             reads=[onesb, px], writes=[pden])
                        first = False
            o = ot[h % 2]
            k.op("dve", lambda e: e.reciprocal(out=rden[:], in_=pden[:]), reads=[pden], writes=[rden])
            k.op("dve", lambda e: e.tensor_tensor(out=o[:], in0=py[:], in1=rden[:], op=ALU.mult), reads=[py, rden], writes=[o])
            k.dma("pool", out_d[kslot, h], o[:], reads=[o], is_output=True)
    k.finish()
    return k


def _t5_bucket(rel):
    rel = np.maximum(rel, 0)
    me = 16
    rf = np.maximum(rel, 1).astype(np.float32)
    large = me + (np.log(rf / me) / np.log(128 / me) * (32 - me)).astype(np.int32)
    large = np.minimum(large, 31)
    return np.where(rel < me, rel, large)


def at_inputs(aq, ak, av, iq, ik, iw, rel_bias):
    kT = np.ascontiguousarray(ak.reshape(LP, 8, 128).transpose(1, 2, 0))
    v = np.ascontiguousarray(av.reshape(NT, 128, 8, 128).transpose(2, 1, 0, 3))
    ikT = np.ascontiguousarray(ik.T)
    rb31 = np.ascontiguousarray(np.broadcast_to(rel_bias[31], (128, 8))).astype(np.float32)
    ident = np.eye(128, dtype=np.float32)
    aq4 = aq.reshape(NT, 128, 8, 128)
    iq4 = iq.reshape(NT, 128, 8, 64)
    iw3 = iw.reshape(NT, 128, 8)
    s_i = np.arange(128)[:, None]
    q_i = np.arange(128)[None, :]
    maps = []
    for c in range(NCORES):
        qT = np.ascontiguousarray(aq4[c::8].transpose(0, 3, 2, 1))
        iqT = np.ascontiguousarray(iq4[c::8].transpose(0, 3, 2, 1))
        iwc = np.ascontiguousarray(iw3[c::8])
        fb = np.empty((8, 128, 9, 128), np.float32)
        for m in range(9):
            r = c + 1 - m
            dist = r * 128 + q_i - s_i
            vis = (dist >= 0) & (r >= 0)
            idx = _t5_bucket(np.where(vis, dist, 0))
            vals = rel_bias[idx]
            fb[:, :, m, :] = np.where(vis[None], vals.transpose(2, 0, 1), np.float32(-1e30))
        ub = np.empty((128, 8, 128), np.float32)
        for mp in range(8):
            if mp < c:
                ub[:, mp, :] = 3e38
            elif mp > c:
                ub[:, mp, :] = -1e30
            else:
                ub[:, mp, :] = np.where(np.arange(128)[None, :] <= np.arange(128)[:, None], 3e38, -1e30)
        maps.append({"qT": qT, "iqT": iqT, "iw": iwc, "kT": kT, "v": v, "ikT": ikT, "fb": fb,
                     "ub": ub.reshape(128, 1024), "rb31": rb31, "ident": ident})
    return maps


MG = 8


def build_ml(ngroups=None):
    ngroups = NT // MG if ngroups is None else ngroups
    k = K()
    qsh_d = k.dram("qsh", [128, NT, 4 * 128], BF16, "ExternalInput")
    ksh_d = k.dram("ksh", [128, NT, 4 * 128], BF16, "ExternalInput")
    va_d = k.dram("vaug", [128, NT, 256], BF16, "ExternalInput")
    mif_d = k.dram("mif", [128, NT, 2], F32, "ExternalInput")
    cw_d = k.dram("cwg", [128, 2, 4, MG * 128], F32, "ExternalInput")
    cb_d = k.dram("cbg", [128, 2, MG * 128], F32, "ExternalInput")
    gb_d = k.dram("gb", [128, 2], F32, "ExternalInput")
    tri_d = k.dram("tri", [128, 128], F32, "ExternalInput")
    id_d = k.dram("ident", [128, 128], F32, "ExternalInput")
    out_d = k.dram("hm", [128, LP], BF16, "ExternalOutput")

    cw = k.sb("cw", [128, 2, 4, MG * 128], F32)
    cb = k.sb("cb", [128, 2, MG * 128], F32)
    gb = k.sb("gb_s", [128, 2], F32)
    ngb = k.sb("ngb", [128, 2], F32)
    tri = k.sb("tri_s", [128, 128], F32)
    idf = k.sb("idf", [128, 128], F32)
    idb = k.sb("idb", [128, 128], BF16)
    onesf = k.sb("onesf", [128, 128], F32)
    mif = k.sb("mif_s", [128, NT, 2], F32)
    lf = k.sb("lf", [128, NT], F32)
    tmpg = k.sb("tmpg", [128, NT], F32)
    qsc = k.sb("qsc", [128, NT], F32)
    ksc = k.sb("ksc", [128, NT], F32)
    gl = k.sb("gl", [128, NT], F32)
    qsh = [k.sb("qsh%d" % i, [128, MG, 512], BF16) for i in range(2)]
    ksh = [k.sb("ksh%d" % i, [128, MG, 512], BF16) for i in range(2)]
    va = [k.sb("va%d" % i, [128, MG, 256], BF16) for i in range(2)]
    accq = k.sb("accq", [128, MG * 128], F32)
    acck = k.sb("acck", [128, MG * 128], F32)
    tq = k.sb("tq", [128, MG * 128], F32)
    tk_ = k.sb("tk", [128, MG * 128], F32)
    qtm = k.sb("qtm", [128, MG, 128], BF16)
    ktm = k.sb("ktm", [128, MG, 128], BF16)
    qT = k.sb("qT_s", [128, MG, 128], BF16)
    kT = k.sb("kT_s", [128, MG, 128], BF16)
    AT = [k.sb("AT%d" % i, [128, 128], BF16) for i in range(2)]
    Cnf = k.sb("Cnf", [128, 256], F32)
    Cnb = k.sb("Cnb", [128, 256], BF16)
    dd = k.sb("dd", [128, 128], F32)
    ho = [k.sb("ho%d" % i, [128, MG * 128], BF16) for i in range(2)]

    pg = k.ps("pg", [128, 512], F32)
    ptq = k.ps("ptq", [128, 4, 128], BF16)
    ptk = k.ps("ptk", [128, 4, 128], BF16)
    pA = [k.ps("pA%d" % i, [128, 128], F32) for i in range(2)]
    pnum = k.ps("pnum", [128, 128], F32)
    pden = k.ps("pden", [128, 128], F32)
    pCn = k.ps("pCn", [128, 256], F32)

    for dst, src in ((cw, cw_d), (cb, cb_d), (gb, gb_d), (tri, tri_d), (idf, id_d), (mif, mif_d)):
        k.dma("sp", dst[:], src, writes=[dst])
    k.op("dve", lambda e: e.tensor_copy(out=idb[:], in_=idf[:]), reads=[idf], writes=[idb])
    k.op("dve", lambda e: e.memset(onesf[:], 1.0), writes=[onesf])
    k.op("dve", lambda e: e.tensor_scalar(out=ngb[:], in0=gb[:], scalar1=-1.0, scalar2=None, op0=ALU.mult),
         reads=[gb], writes=[ngb])
    k.op("dve", lambda e: e.memset(Cnf[:], 0.0), writes=[Cnf])
    k.op("dve", lambda e: e.memset(Cnb[:], 0.0), writes=[Cnb])
    k.op("act", lambda e: e.activation(out=tmpg[:], in_=mif[:, :, 1], func=AF.Exp, bias=ngb[:, 1:2], scale=-1.0),
         reads=[mif, ngb], writes=[tmpg])
    k.op("act", lambda e: e.activation(out=tmpg[:], in_=tmpg[:], func=AF.Ln, bias=1.0, scale=1.0),
         reads=[tmpg], writes=[tmpg])
    k.op("dve", lambda e: e.tensor_scalar(out=lf[:], in0=tmpg[:], scalar1=-1.0, scalar2=None, op0=ALU.mult),
         reads=[tmpg], writes=[lf])
    k.op("pe", lambda e: e.matmul(pg[:, 0:NT], lhsT=tri[:], rhs=lf[:], start=True, stop=True), reads=[tri, lf], writes=[pg])
    k.op("pe", lambda e: e.matmul(pg[:, 256:256 + NT], lhsT=onesf[:], rhs=lf[:], start=True, stop=True),
         reads=[onesf, lf], writes=[pg])
    import math
    k.op("act", lambda e: e.activation(out=qsc[:], in_=pg[:, 0:NT], func=AF.Exp, bias=-0.5 * math.log(128.0), scale=1.0),
         reads=[pg], writes=[qsc])
    k.op("dve", lambda e: e.tensor_tensor(out=tmpg[:], in0=mif[:, :, 0], in1=pg[:, 0:NT], op=ALU.subtract),
         reads=[mif, pg], writes=[tmpg])
    k.op("act", lambda e: e.activation(out=ksc[:], in_=tmpg[:], func=AF.Exp, bias=gb[:, 0:1], scale=1.0),
         reads=[tmpg, gb], writes=[ksc])
    k.op("act", lambda e: e.activation(out=gl[:], in_=pg[:, 256:256 + NT], func=AF.Exp), reads=[pg], writes=[gl])

    for g in range(ngroups):
        c0 = g * MG
        qs_, ks_, va_ = qsh[g % 2], ksh[g % 2], va[g % 2]
        k.dma("sp", qs_[:], qsh_d[:, c0:c0 + MG, :], writes=[qs_])
        k.dma("act", ks_[:], ksh_d[:, c0:c0 + MG, :], writes=[ks_])
        k.dma("sp", va_[:], va_d[:, c0:c0 + MG, :], writes=[va_])
        for which, (src, acc, tmp, eng) in enumerate(((qs_, accq, tq, "dve"), (ks_, acck, tk_, "pool"))):
            a3 = acc[:].rearrange("p (g d) -> p g d", g=MG)
            t3 = tmp[:].rearrange("p (g d) -> p g d", g=MG)
            for j in range(4):
                w3 = cw[:, which, j, :].rearrange("p (g d) -> p g d", g=MG)
                dst = a3 if j == 0 else t3
                k.op(eng, lambda e: e.tensor_tensor(out=dst, in0=src[:, :, j * 128:(j + 1) * 128], in1=w3, op=ALU.mult),
                     reads=[src, cw], writes=[acc if j == 0 else tmp])
                if j > 0:
                    k.op(eng, lambda e: e.tensor_tensor(out=acc[:], in0=acc[:], in1=tmp[:], op=ALU.add),
                         reads=[acc, tmp], writes=[acc])
            k.op(eng, lambda e: e.tensor_tensor(out=acc[:], in0=acc[:], in1=cb[:, which, :], op=ALU.add),
                 reads=[acc, cb], writes=[acc])
            k.op("act", lambda e: e.activation(out=acc[:], in_=acc[:], func=AF.Silu), reads=[acc], writes=[acc])
        for ci in range(MG):
            c = c0 + ci
            k.op("dve", lambda e: e.tensor_scalar(out=qtm[:, ci, :], in0=accq[:, ci * 128:(ci + 1) * 128],
                                                  scalar1=qsc[:, c:c + 1], scalar2=None, op0=ALU.mult),
                 reads=[accq, qsc], writes=[qtm])
            k.op("dve", lambda e: e.tensor_scalar(out=ktm[:, ci, :], in0=acck[:, ci * 128:(ci + 1) * 128],
                                                  scalar1=ksc[:, c:c + 1], scalar2=None, op0=ALU.mult),
                 reads=[acck, ksc], writes=[ktm])
        for half in range(MG // 4):
            for j in range(4):
                ci = half * 4 + j
                k.op("pe", lambda e: e.transpose(out=ptq[:, j, :], in_=qtm[:, ci, :], identity=idb[:]),
                     reads=[qtm, idb], writes=[ptq])
                k.op("pe", lambda e: e.transpose(out=ptk[:, j, :], in_=ktm[:, ci, :], identity=idb[:]),
                     reads=[ktm, idb], writes=[ptk])
            k.op("act", lambda e: e.copy(out=qT[:, half * 4:(half + 1) * 4, :], in_=ptq[:]), reads=[ptq], writes=[qT])
            k.op("dve", lambda e: e.tensor_copy(out=kT[:, half * 4:(half + 1) * 4, :], in_=ptk[:]), reads=[ptk], writes=[kT])
        hob = ho[g % 2]
        for ci in range(MG):
            c = c0 + ci
            pa = pA[ci % 2]
            at = AT[ci % 2]
            k.op("pe", lambda e: e.matmul(pa[:], lhsT=kT[:, ci, :], rhs=qT[:, ci, :], start=True, stop=True),
                 reads=[kT, qT], writes=[pa])
            k.op("dve", lambda e: e.tensor_tensor(out=at[:], in0=pa[:], in1=tri[:], op=ALU.mult),
                 reads=[pa, tri], writes=[at])
            k.op("pe", lambda e: e.matmul(pnum[:], lhsT=va_[:, ci, 0:128], rhs=at[:], start=True, stop=False),
                 reads=[va_, at], writes=[pnum])
            k.op("pe", lambda e: e.matmul(pnum[:], lhsT=Cnb[:, 0:128], rhs=qT[:, ci, :], start=False, stop=True),
                 reads=[Cnb, qT], writes=[pnum])
            k.op("pe", lambda e: e.matmul(pden[:], lhsT=va_[:, ci, 128:256], rhs=at[:], start=True, stop=False),
                 reads=[va_, at], writes=[pden])
            k.op("pe", lambda e: e.matmul(pden[:], lhsT=Cnb[:, 128:256], rhs=qT[:, ci, :], start=False, stop=True),
                 reads=[Cnb, qT], writes=[pden])
            k.op("act", lambda e: e.activation(out=dd[:], in_=pden[:], func=AF.Abs), reads=[pden], writes=[dd])
            k.op("dve", lambda e: e.tensor_scalar(out=dd[:], in0=dd[:], scalar1=1.0, scalar2=None, op0=ALU.max),
                 reads=[dd], writes=[dd])
            k.op("dve", lambda e: e.reciprocal(out=dd[:], in_=dd[:]), reads=[dd], writes=[dd])
            k.op("dve", lambda e: e.tensor_tensor(out=hob[:, ci * 128:(ci + 1) * 128], in0=pnum[:], in1=dd[:], op=ALU.mult),
                 reads=[pnum, dd], writes=[hob])
            k.op("pe", lambda e: e.matmul(pCn[:], lhsT=ktm[:, ci, :], rhs=va_[:, ci, :], start=True, stop=True),
                 reads=[ktm, va_], writes=[pCn])
            k.op("dve", lambda e: e.tensor_tensor(out=Cnf[:], in0=Cnf[:], in1=pCn[:], op=ALU.add),
                 reads=[Cnf, pCn], writes=[Cnf])
            k.op("dve", lambda e: e.tensor_scalar(out=Cnf[:], in0=Cnf[:], scalar1=gl[:, c:c + 1], scalar2=None, op0=ALU.mult),
                 reads=[Cnf, gl], writes=[Cnf])
            k.op("act", lambda e: e.copy(out=Cnb[:], in_=Cnf[:]), reads=[Cnf], writes=[Cnb])
        k.dma("pool", out_d[:, c0 * 128:(c0 + MG) * 128], hob[:], reads=[hob], is_output=True)
    k.finish()
    return k


def ml_inputs(mq, mk, mv, mi, mf, conv_w, conv_b, b_ig, b_fg):
    tri = np.triu(np.ones((128, 128), np.float32))
    ident = np.eye(128, dtype=np.float32)

    def shifted(u):
        out = np.zeros((LP, 4, 128), u.dtype)
        for j in range(4):
            sh = 3 - j
            if sh == 0:
                out[:, j] = u
            else:
                out[sh:, j] = u[:-sh]
        return np.ascontiguousarray(out.reshape(NT, 128, 512).transpose(1, 0, 2))

    maps = []
    for c in range(NCORES):
        hd, vh = c // 2, c % 2
        qsh = shifted(mq[:, hd * 128:(hd + 1) * 128])
        ksh = shifted(mk[:, hd * 128:(hd + 1) * 128])
        vaug = np.ones((LP, 256), mv.dtype)
        vaug[:, :128] = mv[:, hd * 256 + vh * 128: hd * 256 + (vh + 1) * 128]
        vaug = np.ascontiguousarray(vaug.reshape(NT, 128, 256).transpose(1, 0, 2))
        mif = np.stack([mi[:, hd], mf[:, hd]], -1).astype(np.float32)
        mif = np.ascontiguousarray(mif.reshape(NT, 128, 2).transpose(1, 0, 2))
        cwq = conv_w[:, hd * 128:(hd + 1) * 128]
        cwk = conv_w[:, 512 + hd * 128:512 + (hd + 1) * 128]
        cw = np.stack([cwq, cwk], 0)
        cwg = np.ascontiguousarray(np.broadcast_to(cw[None, :, :, None, :], (128, 2, 4, MG, 128))).reshape(128, 2, 4, MG * 128)
        cbq = conv_b[hd * 128:(hd + 1) * 128]
        cbk = conv_b[512 + hd * 128:512 + (hd + 1) * 128]
        cb2 = np.stack([cbq, cbk], 0)
        cbg = np.ascontiguousarray(np.broadcast_to(cb2[None, :, None, :], (128, 2, MG, 128))).reshape(128, 2, MG * 128)
        gb = np.ascontiguousarray(np.broadcast_to(np.array([b_ig[hd], b_fg[hd]], np.float32), (128, 2)))
        maps.append({"qsh": qsh, "ksh": ksh, "vaug": vaug, "mif": mif, "cwg": cwg.astype(np.float32),
                     "cbg": cbg.astype(np.float32), "gb": gb, "tri": tri, "ident": ident})
    return maps


def _pa_blocks():
    blocks = []
    segs = [(0, 2048, True),
            (3072, 8, False),
            (3080, 3648, True),
            (6728, 8, False)]
    for s, w, b in segs:
        o = 0
        while o < w:
            ww = min(512, w - o)
            blocks.append((s + o, ww, b))
            o += ww
    return blocks


PA_BLOCKS = _pa_blocks()
PA16 = sum(w for _, w, b in PA_BLOCKS if b)
PA32 = sum(w for _, w, b in PA_BLOCKS if not b)
NTOK = TPC * 128


class Stager:
    def __init__(self, k, st, n=4):
        self.k = k
        self.bufs = [k.sb("stg%d" % i, [128, 2048], F32, st) for i in range(n)]
        self.i = 0
        self.ce = 0

    def load(self, src_view, dst_ap, dstT, shape3=None, nelem=2048):
        k = self.k
        b = self.bufs[self.i % len(self.bufs)]
        self.i += 1
        if shape3 is None:
            v = b[:, 0:nelem]
        else:
            c, n = shape3
            v = b[:, 0:c * n].rearrange("p (c n) -> p c n", c=c)
        k.dma("sp", v, src_view, writes=[b])
        eng = ("pool", "dve", "act")[self.ce % 3]
        self.ce += 1
        if eng == "act":
            k.op("act", lambda e: e.copy(out=dst_ap, in_=v), reads=[b], writes=[dstT])
        else:
            k.op(eng, lambda e: e.tensor_copy(out=dst_ap, in_=v), reads=[b], writes=[dstT])


def _ln_rows(k, x, y, s, sq, gt, bt, ncols=D):
    k.op("dve", lambda e: e.tensor_reduce(out=s[:, 0:1], in_=x[:], op=ALU.add, axis=AX.X), reads=[x], writes=[s])
    k.op("act", lambda e: e.activation(out=sq[:], in_=x[:], func=AF.Square, accum_out=s[:, 1:2]), reads=[x], writes=[sq, s])
    k.op("dve", lambda e: e.tensor_scalar(out=s[:, 2:4], in0=s[:, 0:2], scalar1=1.0 / ncols, scalar2=None, op0=ALU.mult),
         reads=[s], writes=[s])
    k.op("dve", lambda e: e.tensor_tensor(out=s[:, 4:5], in0=s[:, 2:3], in1=s[:, 2:3], op=ALU.mult), reads=[s], writes=[s])
    k.op("dve", lambda e: e.tensor_tensor(out=s[:, 5:6], in0=s[:, 3:4], in1=s[:, 4:5], op=ALU.subtract), reads=[s], writes=[s])
    k.op("dve", lambda e: e.tensor_scalar(out=s[:, 5:6], in0=s[:, 5:6], scalar1=LN_EPS, scalar2=None, op0=ALU.add),
         reads=[s], writes=[s])
    k.op("act", lambda e: e.activation(out=s[:, 6:7], in_=s[:, 5:6], func=AF.Sqrt), reads=[s], writes=[s])
    k.op("dve", lambda e: e.reciprocal(out=s[:, 7:8], in_=s[:, 6:7]), reads=[s], writes=[s])
    k.op("dve", lambda e: e.tensor_scalar(out=y[:], in0=x[:], scalar1=s[:, 2:3], scalar2=s[:, 7:8],
                                          op0=ALU.subtract, op1=ALU.mult), reads=[x, s], writes=[y])
    k.op("pool", lambda e: e.tensor_tensor(out=y[:], in0=y[:], in1=gt[:], op=ALU.mult), reads=[y, gt], writes=[y])
    k.op("dve", lambda e: e.tensor_tensor(out=y[:], in0=y[:], in1=bt[:], op=ALU.add), reads=[y, bt], writes=[y])


class _Stop(Exception):
    pass


def build_pba(last=False, first=False, stop_after=99, NVB=1):
    k = K()
    A_ALPHA = float(ALPHA)
    ident_d = k.dram("ident", [128, 128], F32, "ExternalInput")
    NALL = NVB * NTOK
    hout_d = k.dram("hout", [NALL, D], F32, "ExternalOutput")
    if first:
        xin_d = k.dram("xin", [NALL, D], F32, "ExternalInput")
    else:
        h_d = k.dram("h", [NALL, D], F32, "ExternalInput")
        hm_d = k.dram("hm", [NALL, 1024], BF16, "ExternalInput")
        yaT_d = k.dram("yaT", [1024, NALL], BF16, "ExternalInput")
        wg_d = k.dram("wg", [D, 5120], F32, "ExternalInput")
        mhg_d = k.dram("mhg", [128, 1024], F32, "ExternalInput")
        wpm_d = k.dram("wpm", [1024, D], F32, "ExternalInput")
        wpa_d = k.dram("wpa", [1024, D], F32, "ExternalInput")
        wout_d = k.dram("wout", [D, D], F32, "ExternalInput")
        g1_d = k.dram("g1", [128, D], F32, "ExternalInput")
        b1_d = k.dram("b1", [128, D], F32, "ExternalInput")
        wr_d = k.dram("wr", [D, 36], F32, "ExternalInput")
        br_d = k.dram("br", [128, 36], F32, "ExternalInput")
        wgu_d = k.dram("wgu", [32, D, 1024], F32, "ExternalInput")
        wd_d = k.dram("wd", [32, 512, D], F32, "ExternalInput")
        mrg_d = k.dram("s_mrg", [NTOK, D], BF16, "Internal")
        pre1_d = k.dram("s_pre1", [NTOK, D], F32, "Internal")
        h1_d = k.dram("s_h1", [NTOK, D], F32, "Internal")
        moe_d = k.dram("s_moe", [NTOK, D], F32, "Internal")
    g2_d = k.dram("g2", [128, D], F32, "ExternalInput")
    b2_d = k.dram("b2", [128, D], F32, "ExternalInput")
    if not last:
        wn_d = k.dram("wn", [D, IN_W], F32, "ExternalInput")
        o16_d = k.dram("o16", [NALL, PA16], BF16, "ExternalOutput")
        o32_d = k.dram("o32", [NALL, PA32], F32, "ExternalOutput")

    bufA = k.sb("bufA", [128, 16, NTOK], BF16)
    A = [T(bufA.t, "A%d" % i) for i in range(TPC)]
    idf = k.sb("idf", [128, 128], F32)
    idb = k.sb("idb", [128, 128], BF16)
    Gall = k.sb("Gall", [128, TPC, 32], F32)
    k.dma("sp", idf[:], ident_d, writes=[idf])
    k.op("dve", lambda e: e.tensor_copy(out=idb[:], in_=idf[:]), reads=[idf], writes=[idb])
    PB = [k.ps("pb%d" % i, [128, 512], F32) for i in range(6)]
    PT32 = k.ps("pt32", [128, 4, 128], F32)
    PT16 = k.ps("pt16", [128, 4, 128], BF16)

    for vb in range(NVB):
        base = vb * NTOK

        def tsl(ti):
            return slice(ti * 128, (ti + 1) * 128)

        def rsl(ti):
            return slice(base + ti * 128, base + (ti + 1) * 128)

        def to_bufA(src_bf, ti, nchunk=16, dstbuf=None, dstT=None):
            dstbuf = bufA if dstbuf is None else dstbuf
            dstT = A[ti] if dstT is None else dstT
            for g4 in range(nchunk // 4):
                for j in range(4):
                    c = g4 * 4 + j
                    k.op("pe", lambda e: e.transpose(out=PT16[:, j, :], in_=src_bf[:, c * 128:(c + 1) * 128], identity=idb[:]),
                         reads=[src_bf, idb], writes=[PT16])
                k.op("dve", lambda e: e.tensor_copy(out=dstbuf[:, g4 * 4:(g4 + 1) * 4, tsl(ti)], in_=PT16[:]),
                     reads=[PT16], writes=[dstT])

        if not first:
            outer = ExitStack()
            bufB = k.sb("bufB", [128, 8, NTOK], BF16, outer)
            Bg = [T(bufB.t, "B%d" % i) for i in range(TPC)]
            with ExitStack() as st:
                stg = Stager(k, st, n=2)
                wmo = k.sb("wmo", [128, 16, 1024], BF16, st)
                mhg = k.sb("mhg", [128, 1024], F32, st)
                xt = [k.sb("xt%d" % i, [128, D], F32, st) for i in range(2)]
                xb = k.sb("xb", [128, D], BF16, st)
                hmt = [k.sb("hmt%d" % i, [128, 1024], BF16, st) for i in range(2)]
                hn = k.sb("hn", [128, 1024], F32, st)
                sq = k.sb("sq", [128, 1024], F32, st)
                sg = [k.sb("sg%d" % i, [128, 512], F32, st) for i in range(2)]
                ym = k.sb("ym", [128, 1024], BF16, st)
                s4 = k.sb("s4", [128, 32], F32, st)
                k.dma("sp", mhg[:], mhg_d, writes=[mhg])
                wv = wg_d.rearrange("(c p) n -> p c n", p=128)
                for u in range(8):
                    stg.load(wv[:, 2 * u:2 * u + 2, 0:1024], wmo[:, 2 * u:2 * u + 2, :], wmo, shape3=(2, 1024))
                nev = 0
                for ti in range(TPC):
                    x = xt[ti % 2]
                    hm = hmt[ti % 2]
                    k.dma("sp", x[:], h_d[rsl(ti), :], writes=[x])
                    k.dma("sp", hm[:], hm_d[rsl(ti), :], writes=[hm])
                    k.op("act", lambda e: e.copy(out=xb[:], in_=x[:]), reads=[x], writes=[xb])
                    to_bufA(xb, ti)
                    hm3 = hm[:].rearrange("p (h v) -> p h v", h=4)
                    sq3 = sq[:].rearrange("p (h v) -> p h v", h=4)
                    k.op("dve", lambda e: e.tensor_reduce(out=s4[:, 0:4], in_=hm3, op=ALU.add, axis=AX.X), reads=[hm], writes=[s4])
                    k.op("pool", lambda e: e.tensor_tensor(out=sq[:], in0=hm[:], in1=hm[:], op=ALU.mult), reads=[hm], writes=[sq])
                    k.op("dve", lambda e: e.tensor_reduce(out=s4[:, 4:8], in_=sq3, op=ALU.add, axis=AX.X), reads=[sq], writes=[s4])
                    k.op("dve", lambda e: e.tensor_scalar(out=s4[:, 8:16], in0=s4[:, 0:8], scalar1=1.0 / 256, scalar2=None, op0=ALU.mult),
                         reads=[s4], writes=[s4])
                    k.op("dve", lambda e: e.tensor_tensor(out=s4[:, 16:20], in0=s4[:, 8:12], in1=s4[:, 8:12], op=ALU.mult),
                         reads=[s4], writes=[s4])
                    k.op("dve", lambda e: e.tensor_tensor(out=s4[:, 20:24], in0=s4[:, 12:16], in1=s4[:, 16:20], op=ALU.subtract),
                         reads=[s4], writes=[s4])
                    k.op("dve", lambda e: e.tensor_scalar(out=s4[:, 20:24], in0=s4[:, 20:24], scalar1=LN_EPS, scalar2=None, op0=ALU.add),
                         reads=[s4], writes=[s4])
                    k.op("act", lambda e: e.activation(out=s4[:, 24:28], in_=s4[:, 20:24], func=AF.Sqrt), reads=[s4], writes=[s4])
                    k.op("dve", lambda e: e.reciprocal(out=s4[:, 28:32], in_=s4[:, 24:28]), reads=[s4], writes=[s4])
                    for hd in range(4):
                        k.op("dve", lambda e: e.tensor_scalar(out=hn[:, hd * 256:(hd + 1) * 256], in0=hm[:, hd * 256:(hd + 1) * 256],
                                                              scalar1=s4[:, 8 + hd:9 + hd], scalar2=s4[:, 28 + hd:29 + hd],
                                                              op0=ALU.subtract, op1=ALU.mult), reads=[hm, s4], writes=[hn])
                    k.op("pool", lambda e: e.tensor_tensor(out=hn[:], in0=hn[:], in1=mhg[:], op=ALU.mult), reads=[hn, mhg], writes=[hn])
                    for cbk in range(2):
                        p = PB[nev % 6]
                        sgt = sg[nev % 2]
                        nev += 1
                        for c in range(16):
                            k.op("pe", lambda e: e.matmul(p[:], lhsT=bufA[:, c, tsl(ti)], rhs=wmo[:, c, cbk * 512:(cbk + 1) * 512],
                                                          start=(c == 0), stop=(c == 15)), reads=[A[ti], wmo], writes=[p])
                        k.op("act", lambda e: e.activation(out=sgt[:], in_=p[:], func=AF.Sigmoid), reads=[p], writes=[sgt])
                        k.op("dve", lambda e: e.tensor_tensor(out=ym[:, cbk * 512:(cbk + 1) * 512], in0=hn[:, cbk * 512:(cbk + 1) * 512],
                                                              in1=sgt[:], op=ALU.mult), reads=[hn, sgt], writes=[ym])
                    to_bufA(ym, ti, nchunk=8, dstbuf=bufB, dstT=Bg[ti])
                k.barrier()
            if stop_after == 1:
                outer.close()
                k.finish()
                return k
            bufC = k.sb("bufC", [128, 8, NTOK], BF16, outer)
            k.dma("sp", bufC[:], yaT_d[:, base:base + NTOK].rearrange("(c p) t -> p c t", p=128), writes=[bufC])
            with ExitStack() as st:
                stg = Stager(k, st)
                CW = 256
                wgm = k.sb("wgm", [128, 16, CW], BF16, st)
                wga = k.sb("wga", [128, 16, CW], BF16, st)
                wpm = k.sb("wpm", [128, 8, CW], BF16, st)
                wpa = k.sb("wpa", [128, 8, CW], BF16, st)
                s1 = [k.sb("s1_%d" % i, [128, CW], F32, st) for i in range(2)]
                s2 = [k.sb("s2_%d" % i, [128, CW], F32, st) for i in range(2)]
                mg = [k.sb("mg%d" % i, [128, CW], BF16, st) for i in range(2)]
                wgv = wg_d.rearrange("(c p) n -> p c n", p=128)
                wpmv = wpm_d.rearrange("(c p) n -> p c n", p=128)
                wpav = wpa_d.rearrange("(c p) n -> p c n", p=128)
                nev = 0
                for cbk in range(D // CW):
                    c0 = cbk * CW
                    for hf in range(2):
                        stg.load(wgv[:, hf * 8:(hf + 1) * 8, 1024 + c0:1024 + c0 + CW], wgm[:, hf * 8:(hf + 1) * 8, :], wgm, shape3=(8, CW))
                        stg.load(wgv[:, hf * 8:(hf + 1) * 8, 3072 + c0:3072 + c0 + CW], wga[:, hf * 8:(hf + 1) * 8, :], wga, shape3=(8, CW))
                    stg.load(wpmv[:, :, c0:c0 + CW], wpm[:], wpm, shape3=(8, CW))
                    stg.load(wpav[:, :, c0:c0 + CW], wpa[:], wpa, shape3=(8, CW))
                    for ti in range(TPC):
                        pgm, pga, ppm, ppa = PB[0], PB[1], PB[2 + 2 * (nev % 2)], PB[3 + 2 * (nev % 2)]
                        a1, a2, m_ = s1[nev % 2], s2[nev % 2], mg[nev % 2]
                        nev += 1
                        for c in range(16):
                            k.op("pe", lambda e: e.matmul(pgm[:, 0:CW], lhsT=bufA[:, c, tsl(ti)], rhs=wgm[:, c, :], start=(c == 0), stop=(c == 15)),
                                 reads=[A[ti], wgm], writes=[pgm])
                        k.op("act", lambda e: e.activation(out=a1[:], in_=pgm[:, 0:CW], func=AF.Sigmoid), reads=[pgm], writes=[a1])
                        for c in range(16):
                            k.op("pe", lambda e: e.matmul(pga[:, 0:CW], lhsT=bufA[:, c, tsl(ti)], rhs=wga[:, c, :], start=(c == 0), stop=(c == 15)),
                                 reads=[A[ti], wga], writes=[pga])
                        k.op("act", lambda e: e.activation(out=a2[:], in_=pga[:, 0:CW], func=AF.Sigmoid), reads=[pga], writes=[a2])
                        for c in range(8):
                            k.op("pe", lambda e: e.matmul(ppm[:, 0:CW], lhsT=bufB[:, c, tsl(ti)], rhs=wpm[:, c, :], start=(c == 0), stop=(c == 7)),
                                 reads=[Bg[ti], wpm], writes=[ppm])
                        for c in range(8):
                            k.op("pe", lambda e: e.matmul(ppa[:, 0:CW], lhsT=bufC[:, c, tsl(ti)], rhs=wpa[:, c, :], start=(c == 0), stop=(c == 7)),
                                 reads=[bufC, wpa], writes=[ppa])
                        k.op("dve", lambda e: e.tensor_tensor(out=a1[:], in0=a1[:], in1=ppm[:, 0:CW], op=ALU.mult), reads=[a1, ppm], writes=[a1])
                        k.op("dve", lambda e: e.tensor_tensor(out=a2[:], in0=a2[:], in1=ppa[:, 0:CW], op=ALU.mult), reads=[a2, ppa], writes=[a2])
                        k.op("pool", lambda e: e.tensor_tensor(out=m_[:], in0=a1[:], in1=a2[:], op=ALU.add), reads=[a1, a2], writes=[m_])
                        k.dma("pool", mrg_d[tsl(ti), c0:c0 + CW], m_[:], reads=[m_])
                k.barrier()
            outer.close()
            if stop_after == 2:
                k.finish()
                return k
            with ExitStack() as st:
                stg = Stager(k, st)
                mt = [k.sb("mt%d" % i, [128, D], BF16, st) for i in range(2)]
                wo = [k.sb("wo%d" % i, [128, 16, 512], BF16, st) for i in range(2)]
                hb_ = [k.sb("hb%d" % i, [128, 512], F32, st) for i in range(2)]
                pr = [k.sb("pr%d" % i, [128, 512], F32, st) for i in range(2)]
                for ti in range(TPC):
                    m_ = mt[ti % 2]
                    k.dma("sp", m_[:], mrg_d[tsl(ti), :], writes=[m_])
                    to_bufA(m_, ti)
                wov = wout_d.rearrange("(c p) n -> p c n", p=128)
                nev = 0
                for cbk in range(4):
                    w_ = wo[cbk % 2]
                    for u in range(4):
                        stg.load(wov[:, 4 * u:4 * u + 4, cbk * 512:(cbk + 1) * 512], w_[:, 4 * u:4 * u + 4, :], w_, shape3=(4, 512))
                    for ti in range(TPC):
                        p = PB[nev % 6]
                        hx = hb_[nev % 2]
                        po = pr[nev % 2]
                        nev += 1
                        k.dma("sp", hx[:], h_d[rsl(ti), cbk * 512:(cbk + 1) * 512], writes=[hx])
                        for c in range(16):
                            k.op("pe", lambda e: e.matmul(p[:], lhsT=bufA[:, c, tsl(ti)], rhs=w_[:, c, :], start=(c == 0), stop=(c == 15)),
                                 reads=[A[ti], w_], writes=[p])
                        k.op("dve", lambda e: e.scalar_tensor_tensor(out=po[:], in0=hx[:], scalar=A_ALPHA, in1=p[:], op0=ALU.mult, op1=ALU.add),
                             reads=[hx, p], writes=[po])
                        k.dma("pool", pre1_d[tsl(ti), cbk * 512:(cbk + 1) * 512], po[:], reads=[po])
                k.barrier()
            if stop_after == 3:
                k.finish()
                return k
            with ExitStack() as st:
                g1 = k.sb("g1", [128, D], F32, st)
                b1 = k.sb("b1", [128, D], F32, st)
                wr = k.sb("wr", [128, 16, 36], F32, st)
                br = k.sb("br", [128, 36], F32, st)
                xt = [k.sb("x4_%d" % i, [128, D], F32, st) for i in range(2)]
                yt = [k.sb("y4_%d" % i, [128, D], F32, st) for i in range(2)]
                sq = k.sb("sq4", [128, D], BF16, st)
                s8 = k.sb("s8", [128, 8], F32, st)
                hTf = k.sb("hTf", [128, 16, 128], F32, st)
                lg = k.sb("lg", [128, 36], F32, st)
                em = k.sb("em", [128, 32], F32, st)
                r = k.sb("r", [128, 32], F32, st)
                m8 = k.sb("m8", [128, 8], F32, st)
                G1 = k.sb("G1", [128, 32], F32, st)
                G2 = k.sb("G2", [128, 32], F32, st)
                k.dma("sp", g1[:], g1_d, writes=[g1])
                k.dma("sp", b1[:], b1_d, writes=[b1])
                k.dma("sp", wr[:], wr_d.rearrange("(c p) n -> p c n", p=128), writes=[wr])
                k.dma("sp", br[:], br_d, writes=[br])
                for ti in range(TPC):
                    x, y = xt[ti % 2], yt[ti % 2]
                    k.dma("sp", x[:], pre1_d[tsl(ti), :], writes=[x])
                    _ln_rows(k, x, y, s8, sq, g1, b1)
                    k.dma("pool", h1_d[tsl(ti), :], y[:], reads=[y])
                    import os
                    P4 = int(os.environ.get("P4", "9"))
                    if P4 < 1:
                        continue
                    for g4 in range(4):
                        for j in range(4):
                            c = g4 * 4 + j
                            k.op("pe", lambda e: e.transpose(out=PT32[:, j, :], in_=y[:, c * 128:(c + 1) * 128], identity=idf[:]),
                                 reads=[y, idf], writes=[PT32])
                        k.op("dve", lambda e: e.tensor_copy(out=hTf[:, g4 * 4:(g4 + 1) * 4, :], in_=PT32[:]), reads=[PT32], writes=[hTf])
                        k.op("dve", lambda e: e.tensor_copy(out=bufA[:, g4 * 4:(g4 + 1) * 4, tsl(ti)], in_=PT32[:]), reads=[PT32], writes=[A[ti]])
                    if P4 < 2:
                        continue
                    p = PB[ti % 6]
                    for c in range(16):
                        k.op("pe", lambda e: e.matmul(p[:, 0:36], lhsT=hTf[:, c, :], rhs=wr[:, c, :], start=(c == 0), stop=(c == 15)),
                             reads=[hTf, wr], writes=[p])
                    k.op("dve", lambda e: e.tensor_tensor(out=lg[:], in0=p[:, 0:36], in1=br[:], op=ALU.add), reads=[p, br], writes=[lg])
                    if P4 < 3:
                        continue
                    k.op("dve", lambda e: e.tensor_reduce(out=r[:, 0:1], in_=lg[:, 0:4], op=ALU.max, axis=AX.X), reads=[lg], writes=[r])
                    k.op("dve", lambda e: e.tensor_scalar(out=r[:, 1:2], in0=r[:, 0:1], scalar1=-1.0, scalar2=None, op0=ALU.mult), reads=[r], writes=[r])
                    k.op("dve", lambda e: e.tensor_scalar(out=r[:, 4:8], in0=lg[:, 0:4], scalar1=r[:, 0:1], scalar2=None, op0=ALU.is_equal),
                         reads=[lg, r], writes=[r])
                    k.op("act", lambda e: e.activation(out=r[:, 12:16], in_=lg[:, 0:4], func=AF.Exp, bias=r[:, 1:2], scale=1.0),
                         reads=[lg, r], writes=[r])
                    k.op("dve", lambda e: e.tensor_reduce(out=r[:, 2:3], in_=r[:, 12:16], op=ALU.add, axis=AX.X), reads=[r], writes=[r])
                    k.op("dve", lambda e: e.reciprocal(out=r[:, 3:4], in_=r[:, 2:3]), reads=[r], writes=[r])
                    k.op("dve", lambda e: e.tensor_scalar(out=r[:, 8:12], in0=r[:, 4:8], scalar1=-1.0, scalar2=1.0e30, op0=ALU.add, op1=ALU.mult),
                         reads=[r], writes=[r])
                    for gi in range(4):
                        k.op("dve", lambda e: e.tensor_scalar(out=em[:, gi * 8:(gi + 1) * 8], in0=lg[:, 4 + gi * 8:12 + gi * 8],
                                                              scalar1=r[:, 8 + gi:9 + gi], scalar2=None, op0=ALU.add), reads=[lg, r], writes=[em])
                    k.op("dve", lambda e: e.max(out=m8[:], in_=em[:]), reads=[em], writes=[m8])
                    k.op("dve", lambda e: e.tensor_tensor(out=r[:, 16:17], in0=m8[:, 1:2], in1=m8[:, 0:1], op=ALU.subtract), reads=[m8], writes=[r])
                    k.op("act", lambda e: e.activation(out=r[:, 17:18], in_=r[:, 16:17], func=AF.Exp), reads=[r], writes=[r])
                    k.op("dve", lambda e: e.tensor_scalar(out=r[:, 18:19], in0=r[:, 17:18], scalar1=1.0, scalar2=None, op0=ALU.add), reads=[r], writes=[r])
                    k.op("dve", lambda e: e.reciprocal(out=r[:, 19:20], in_=r[:, 18:19]), reads=[r], writes=[r])
                    k.op("dve", lambda e: e.tensor_tensor(out=r[:, 20:21], in0=r[:, 17:18], in1=r[:, 19:20], op=ALU.mult), reads=[r], writes=[r])
                    k.op("dve", lambda e: e.tensor_scalar(out=r[:, 21:23], in0=r[:, 19:21], scalar1=r[:, 3:4], scalar2=None, op0=ALU.mult),
                         reads=[r], writes=[r])
                    k.op("dve", lambda e: e.tensor_scalar(out=G1[:], in0=em[:], scalar1=m8[:, 0:1], scalar2=r[:, 21:22],
                                                          op0=ALU.is_equal, op1=ALU.mult), reads=[em, m8, r], writes=[G1])
                    k.op("dve", lambda e: e.tensor_scalar(out=G2[:], in0=em[:], scalar1=m8[:, 1:2], scalar2=r[:, 22:23],
                                                          op0=ALU.is_equal, op1=ALU.mult), reads=[em, m8, r], writes=[G2])
                    k.op("dve", lambda e: e.tensor_tensor(out=Gall[:, ti, :], in0=G1[:], in1=G2[:], op=ALU.add), reads=[G1, G2], writes=[Gall])
                k.barrier()
            if stop_after == 4:
                k.finish()
                return k
            with ExitStack() as st:
                stg = Stager(k, st)
                wgu = [k.sb("wgu%d" % i, [128, 16, 2, 128], BF16, st) for i in range(3)]
                wdn = [k.sb("wdn%d" % i, [128, 4, D], BF16, st) for i in range(2)]
                hbT = [k.sb("hbT%d" % i, [128, 4, 512], BF16, st) for i in range(2)]
                sgl = [k.sb("sgl%d" % i, [128, 512], F32, st) for i in range(2)]
                acc = k.sb("acc", [128, 4, D], F32, st)
                wguv = wgu_d.rearrange("e (c p) n -> e p c n", p=128)
                wdv = wd_d.rearrange("e (c p) n -> e p c n", p=128)
                npc = 0
                nex = 0
                nev = 0
                for t0 in range(0, TPC, 4):
                    nt_ = min(4, TPC - t0)
                    ntk = nt_ * 128
                    tok = slice(t0 * 128, t0 * 128 + ntk)
                    Ar = [A[t0 + i] for i in range(nt_)]
                    k.op("pool", lambda e: e.memset(acc[:], 0.0), writes=[acc])
                    for ex in range(32):
                        hb = hbT[nex % 2]
                        wd_ = wdn[nex % 2]
                        nex += 1
                        for fc in range(4):
                            stg.load(wdv[ex, :, fc, :], wd_[:, fc, :], wd_)
                        for fc in range(4):
                            wp = wgu[npc % 3]
                            npc += 1
                            stg.load(wguv[ex, :, :, fc * 128:(fc + 1) * 128], wp[:, :, 0, :], wp, shape3=(16, 128))
                            stg.load(wguv[ex, :, :, 512 + fc * 128:512 + (fc + 1) * 128], wp[:, :, 1, :], wp, shape3=(16, 128))
                            pg_, pu_ = PB[(nev % 2) * 2], PB[(nev % 2) * 2 + 1]
                            sg_ = sgl[nev % 2]
                            nev += 1
                            for c in range(16):
                                k.op("pe", lambda e: e.matmul(pg_[:, 0:ntk], lhsT=wp[:, c, 0, :], rhs=bufA[:, c, tok], start=(c == 0), stop=(c == 15)),
                                     reads=[wp] + Ar, writes=[pg_])
                            for c in range(16):
                                k.op("pe", lambda e: e.matmul(pu_[:, 0:ntk], lhsT=wp[:, c, 1, :], rhs=bufA[:, c, tok], start=(c == 0), stop=(c == 15)),
                                     reads=[wp] + Ar, writes=[pu_])
                            k.op("act", lambda e: e.activation(out=sg_[:, 0:ntk], in_=pg_[:, 0:ntk], func=AF.Silu), reads=[pg_], writes=[sg_])
                            k.op("dve", lambda e: e.tensor_tensor(out=hb[:, fc, 0:ntk], in0=sg_[:, 0:ntk], in1=pu_[:, 0:ntk], op=ALU.mult),
                                 reads=[sg_, pu_], writes=[hb])
                        for tl in range(nt_):
                            for db in range(4):
                                po = PB[4 + (nev % 2)]
                                nev += 1
                                for fc in range(4):
                                    k.op("pe", lambda e: e.matmul(po[:], lhsT=hb[:, fc, tl * 128:(tl + 1) * 128], rhs=wd_[:, fc, db * 512:(db + 1) * 512],
                                                                  start=(fc == 0), stop=(fc == 3)), reads=[hb, wd_], writes=[po])
                                k.op("dve", lambda e: e.scalar_tensor_tensor(out=acc[:, tl, db * 512:(db + 1) * 512], in0=po[:],
                                                                             scalar=Gall[:, t0 + tl, ex:ex + 1], in1=acc[:, tl, db * 512:(db + 1) * 512],
                                                                             op0=ALU.mult, op1=ALU.add), reads=[po, Gall, acc], writes=[acc])
                    for tl in range(nt_):
                        k.dma("pool", moe_d[tsl(t0 + tl), :], acc[:, tl, :], reads=[acc])
                k.barrier()

        if stop_after == 5:
            k.finish()
            return k
        with ExitStack() as st:
            g2 = k.sb("g2", [128, D], F32, st)
            b2 = k.sb("b2", [128, D], F32, st)
            xt = [k.sb("x6_%d" % i, [128, D], F32, st) for i in range(2)]
            mo_ = [k.sb("m6_%d" % i, [128, D], F32, st) for i in range(2)]
            yt = [k.sb("y6_%d" % i, [128, D], F32, st) for i in range(2)]
            yb = k.sb("yb6", [128, D], BF16, st)
            sq = k.sb("sq6", [128, D], BF16, st)
            s8 = k.sb("s86", [128, 8], F32, st)
            k.dma("sp", g2[:], g2_d, writes=[g2])
            k.dma("sp", b2[:], b2_d, writes=[b2])
            for ti in range(TPC):
                x, y = xt[ti % 2], yt[ti % 2]
                if first:
                    k.dma("sp", x[:], xin_d[rsl(ti), :], writes=[x])
                else:
                    m_ = mo_[ti % 2]
                    k.dma("sp", x[:], h1_d[tsl(ti), :], writes=[x])
                    k.dma("sp", m_[:], moe_d[tsl(ti), :], writes=[m_])
                    k.op("dve", lambda e: e.scalar_tensor_tensor(out=x[:], in0=x[:], scalar=A_ALPHA, in1=m_[:], op0=ALU.mult, op1=ALU.add),
                         reads=[x, m_], writes=[x])
                _ln_rows(k, x, y, s8, sq, g2, b2)
                k.dma("pool", hout_d[rsl(ti), :], y[:], reads=[y], is_output=True)
                if not last:
                    k.op("act", lambda e: e.copy(out=yb[:], in_=y[:]), reads=[y], writes=[yb])
                    to_bufA(yb, ti)
            k.barrier()
        with ExitStack() as st:
            if not last:
                stg = Stager(k, st)
                wb = [k.sb("wb%d" % i, [128, 16, 512], BF16, st) for i in range(2)]
                ot32 = [k.sb("ot32_%d" % i, [128, 512], F32, st) for i in range(2)]
                ot16 = [k.sb("ot16_%d" % i, [128, 512], BF16, st) for i in range(3)]
                wv = wn_d.rearrange("(c p) n -> p c n", p=128)
                c32 = c16 = nev = 0
                for bi, (cs, cw_, isb) in enumerate(PA_BLOCKS):
                    w_ = wb[bi % 2]
                    for u in range(4):
                        stg.load(wv[:, 4 * u:4 * u + 4, cs:cs + cw_], w_[:, 4 * u:4 * u + 4, 0:cw_], w_, shape3=(4, cw_))
                    for ti in range(TPC):
                        p = PB[nev % 6]
                        for c in range(16):
                            k.op("pe", lambda e: e.matmul(p[:, 0:cw_], lhsT=bufA[:, c, tsl(ti)], rhs=w_[:, c, 0:cw_], start=(c == 0), stop=(c == 15)),
                                 reads=[A[ti], w_], writes=[p])
                        if isb:
                            o = ot16[nev % 3]
                            dst = o16_d[rsl(ti), c16:c16 + cw_]
                        else:
                            o = ot32[nev % 2]
                            dst = o32_d[rsl(ti), c32:c32 + cw_]
                        if nev % 2 == 0:
                            k.op("act", lambda e: e.copy(out=o[:, 0:cw_], in_=p[:, 0:cw_]), reads=[p], writes=[o])
                        else:
                            k.op("dve", lambda e: e.tensor_copy(out=o[:, 0:cw_], in_=p[:, 0:cw_]), reads=[p], writes=[o])
                        k.dma("pool", dst, o[:, 0:cw_], reads=[o], is_output=True)
                        nev += 1
                    if isb:
                        c16 += cw_
                    else:
                        c32 += cw_
            k.barrier()
    k.finish()
    return k


def _rep(v, n=128):
    return np.ascontiguousarray(np.broadcast_to(np.asarray(v, np.float32), (n,) + np.shape(v)))


def pba_inputs(l, P, h, hm_tok, yaT, last):
    w_in = P["w_in"][l]
    wg = np.ascontiguousarray(np.concatenate([w_in[:, 2048:3072], w_in[:, 6736:10832]], axis=1))
    wr = np.ascontiguousarray(np.concatenate([P["w_group"][l], P["w_router"][l]], axis=1))
    br = _rep(np.concatenate([P["b_group"][l], P["b_router"][l]]))
    common = {"ident": np.eye(128, dtype=np.float32), "wg": wg, "mhg": _rep(P["mh_norm_g"][l]),
              "wpm": P["w_proj_m"][l], "wpa": P["w_proj_a"][l], "wout": P["w_out"][l],
              "g1": _rep(P["ln1_g"][l]), "b1": _rep(P["ln1_b"][l]), "wr": wr, "br": br,
              "wgu": P["w_gate_up"][l], "wd": P["w_down"][l],
              "g2": _rep(P["ln2_g"][l]), "b2": _rep(P["ln2_b"][l])}
    if not last:
        common["wn"] = P["w_in"][l + 1]
    maps = []
    for c in range(NCORES):
        m = dict(common)
        m["h"] = h[c * NTOK:(c + 1) * NTOK]
        m["hm"] = hm_tok[c * NTOK:(c + 1) * NTOK]
        m["yaT"] = np.ascontiguousarray(yaT[:, c * NTOK:(c + 1) * NTOK])
        maps.append(m)
    return maps


_PROGS = {}


def _prog(name, fn):
    if name not in _PROGS:
        _PROGS[name] = fn()
    return _PROGS[name]


PB_CORES = 2
PB_NVB = NCORES // PB_CORES


def _pba_maps(l, P, h, hm_tok, yaT, last):
    w_in = P["w_in"][l]
    wg = np.ascontiguousarray(np.concatenate([w_in[:, 2048:3072], w_in[:, 6736:10832]], axis=1))
    wr = np.ascontiguousarray(np.concatenate([P["w_group"][l], P["w_router"][l]], axis=1))
    br = _rep(np.concatenate([P["b_group"][l], P["b_router"][l]]))
    com = {"ident": np.eye(128, dtype=np.float32), "wg": wg, "mhg": _rep(P["mh_norm_g"][l]),
           "wpm": P["w_proj_m"][l], "wpa": P["w_proj_a"][l], "wout": P["w_out"][l],
           "g1": _rep(P["ln1_g"][l]), "b1": _rep(P["ln1_b"][l]), "wr": wr, "br": br,
           "wgu": P["w_gate_up"][l], "wd": P["w_down"][l],
           "g2": _rep(P["ln2_g"][l]), "b2": _rep(P["ln2_b"][l])}
    if not last:
        com["wn"] = P["w_in"][l + 1]
    n = PB_NVB * NTOK
    maps = []
    for c in range(PB_CORES):
        m = dict(com)
        m["h"] = h[c * n:(c + 1) * n]
        m["hm"] = hm_tok[c * n:(c + 1) * n]
        m["yaT"] = np.ascontiguousarray(yaT[:, c * n:(c + 1) * n])
        maps.append(m)
    return maps


def kernel(**inputs):
    import time
    t00 = time.time()
    P = {k_: np.asarray(v) for k_, v in inputs.items()}
    x = P["x"][0]
    xin = np.zeros((LP, D), np.float32)
    xin[:N_META] = P["meta_tokens"]
    xin[N_META:L] = x
    k0 = _prog("pa0", lambda: build_pba(first=True, NVB=PB_NVB))
    n_ = PB_NVB * NTOK
    res = run(k0, [{"ident": np.eye(128, dtype=np.float32), "xin": xin[c * n_:(c + 1) * n_], "g2": _rep(P["ln_emb_g"]),
                    "b2": _rep(P["ln_emb_b"]), "wn": P["w_in"][0]} for c in range(PB_CORES)])
    h, o16, o32 = (np.concatenate([r[nm] for r in res], axis=0) for nm in ("hout", "o16", "o32"))
    print("[kernel] pa0 done %.1fs" % (time.time() - t00), flush=True)
    for l in range(DEPTH):
        last = (l == DEPTH - 1)
        mq, mk, mv = o16[:, 0:512], o16[:, 512:1024], o16[:, 1024:2048]
        aq, ak, av = o16[:, 2048:3072], o16[:, 3072:4096], o16[:, 4096:5120]
        iq, ik = o16[:, 5120:5632], o16[:, 5632:5696]
        mi, mf, iw = o32[:, 0:4], o32[:, 4:8], np.ascontiguousarray(o32[:, 8:16])
        kml = _prog("ml", build_ml)
        rml = run(kml, ml_inputs(mq, mk, mv, mi, mf, P["conv_w"][l], P["conv_b"][l], P["b_igate"][l], P["b_fgate"][l]))
        hm_tok = np.ascontiguousarray(np.concatenate([r["hm"] for r in rml], axis=0).T)
        print("[kernel] L%d ml done %.1fs" % (l, time.time() - t00), flush=True)
        kat = _prog("at", build_at)
        rat = run(kat, at_inputs(np.ascontiguousarray(aq), np.ascontiguousarray(ak), np.ascontiguousarray(av),
                                 np.ascontiguousarray(iq), np.ascontiguousarray(ik), iw, P["rel_bias"]))
        ya = np.stack([r["ya"] for r in rat], axis=0)
        yaT = np.ascontiguousarray(ya.transpose(2, 3, 1, 0, 4)).reshape(1024, LP)
        print("[kernel] L%d at done %.1fs" % (l, time.time() - t00), flush=True)
        kpb = _prog("pbl" if last else "pbm",
                    (lambda: build_pba(last=True, NVB=PB_NVB)) if last else (lambda: build_pba(NVB=PB_NVB)))
        res = run(kpb, _pba_maps(l, P, h, hm_tok, yaT, last))
        h = np.concatenate([r["hout"] for r in res], axis=0)
        if not last:
            o16 = np.concatenate([r["o16"] for r in res], axis=0)
            o32 = np.concatenate([r["o32"] for r in res], axis=0)
        print("[kernel] L%d pba done %.1fs" % (l, time.time() - t00), flush=True)
    out = np.ascontiguousarray(h[N_META:L]).reshape(1, SEQ, D).astype(np.float32)
    return out
```

```python
import numpy as np
import ml_dtypes
from contextlib import ExitStack
import concourse.bass as bass
import concourse.mybir as mybir
from concourse.bass_utils import run_bass_kernel_spmd

F32 = mybir.dt.float32
BF16 = mybir.dt.bfloat16
ALU = mybir.AluOpType
AF = mybir.ActivationFunctionType
AX = mybir.AxisListType
NPBF = ml_dtypes.bfloat16

NCORES = 8
D = 2048
SEQ = 16384
N_META = 16
L = SEQ + N_META
TPC = 17
LP = NCORES * TPC * 128
NT = LP // 128
DEPTH = 4
IN_W = 10832
ALPHA = (2 * DEPTH) ** 0.25
LN_EPS = 1e-5


class T:
    __slots__ = ("t", "name", "w", "r")

    def __init__(self, t, name):
        self.t = t
        self.name = name
        self.w = None
        self.r = {}

    def __getitem__(self, idx):
        return self.t[idx]


class K:
    NDMA = 24

    def __init__(self):
        self.nc = bass.Bass("TRN2", target_bir_lowering=False)
        nc = self.nc
        self.es = ExitStack()
        self.eng = {"pe": nc.tensor, "act": nc.scalar, "dve": nc.vector,
                    "pool": nc.gpsimd, "sp": nc.sync}
        self.sem = {e: self.es.enter_context(nc.semaphore("s_" + e)) for e in self.eng}
        self.cnt = {e: 0 for e in self.eng}
        self.waited = {e: {} for e in self.eng}
        self.dsem = [self.es.enter_context(nc.semaphore("d%d" % i)) for i in range(self.NDMA)]
        self.dcnt = [0] * self.NDMA
        self.dnext = 0
        self.out_tickets = []
        self.npsum = 0

    def dram(self, name, shape, dt, kind):
        return self.nc.dram_tensor(name, list(shape), dt, kind=kind).ap()

    def sb(self, name, shape, dt, st=None):
        self.uid = getattr(self, "uid", 0) + 1
        t = (st or self.es).enter_context(self.nc.sbuf_tensor("%s_%d" % (name, self.uid), list(shape), dt))
        return T(t, name)

    def ps(self, name, shape, dt=F32, st=None):
        self.uid = getattr(self, "uid", 0) + 1
        t = (st or self.es).enter_context(self.nc.psum_tensor("%s_%d" % (name, self.uid), list(shape), dt))
        return T(t, name)

    def pe_fence(self, idb, tiles, z, ncols, reps=8):
        for i in range(reps):
            self.op("pe", lambda e: e.matmul(z[:, 0:ncols], lhsT=idb[:], rhs=self._fsrc[:, 0:ncols], start=True, stop=True),
                    reads=[idb, self._fsrc], writes=[z] + (list(tiles) if i == reps - 1 else []))

    def barrier(self):
        for e in self.eng:
            for e2 in self.eng:
                if self.cnt[e2] and not (e2 == e and e == "pe"):
                    self._wait(e, ("e", e2), self.cnt[e2])
            for i in range(self.NDMA):
                if self.dcnt[i]:
                    self._wait(e, ("d", i), self.dcnt[i])

    def _wait(self, e, key, val):
        if self.waited[e].get(key, 0) >= val:
            return
        if key[0] == "e":
            if key[1] == e and e == "pe":
                return
            sem = self.sem[key[1]]
        else:
            sem = self.dsem[key[1]]
        self.eng[e].wait_ge(sem, val)
        self.waited[e][key] = val

    def _deps(self, e, reads, writes):
        deps = {}
        for t in reads:
            if t.w is not None:
                k, v = t.w
                deps[k] = max(deps.get(k, 0), v)
        for t in writes:
            if t.w is not None:
                k, v = t.w
                deps[k] = max(deps.get(k, 0), v)
            for k, v in t.r.items():
                deps[k] = max(deps.get(k, 0), v)
        for k, v in deps.items():
            self._wait(e, k, v)

    def _mark(self, tk, reads, writes):
        k, v = tk
        for t in reads:
            t.r[k] = max(t.r.get(k, 0), v)
        for t in writes:
            t.w = tk
            t.r = {}

    def op(self, e, fn, reads=(), writes=()):
        self._deps(e, reads, writes)
        ins = fn(self.eng[e])
        self.cnt[e] += 1
        ins.then_inc(self.sem[e], 1)
        self._mark((("e", e), self.cnt[e]), reads, writes)

    def dma(self, e, out, in_, reads=(), writes=(), is_output=False):
        self._deps(e, reads, writes)
        i = self.dnext
        self.dnext = (self.dnext + 1) % self.NDMA
        ins = self.eng[e].dma_start(out=out, in_=in_)
        self.dcnt[i] += 16
        ins.then_inc(self.dsem[i], 16)
        tk = (("d", i), self.dcnt[i])
        self._mark(tk, reads, writes)
        if is_output:
            self.out_tickets.append(tk)

    def finish(self):
        fin = {}
        for k, v in self.out_tickets:
            fin[k] = max(fin.get(k, 0), v)
        for k, v in fin.items():
            self._wait("sp", k, v)
        for e in ("pe", "act", "dve", "pool"):
            if self.cnt[e]:
                self._wait("sp", ("e", e), self.cnt[e])
        self.es.close()
        return self.nc


def run(k, in_maps):
    res = run_bass_kernel_spmd(k.nc, in_maps, core_ids=list(range(len(in_maps))))
    return res.results


def _p1_blocks():
    blocks = []
    segs = [(0, 1024, False),
            (1024, 1024, True),
            (2048, 1024, False),
            (3072, 8, False),
            (3080, 3072 + 512 + 64, True),
            (6728, 8, False),
            (6736, 4096, False)]
    for s, w, b in segs:
        o = 0
        while o < w:
            ww = min(512, w - o)
            blocks.append((s + o, ww, b))
            o += ww
    return blocks


P1_BLOCKS = _p1_blocks()
N32 = sum(w for _, w, b in P1_BLOCKS if not b)
N16 = sum(w for _, w, b in P1_BLOCKS if b)


def build_p1(do_ln=True, do_proj=True):
    k = K()
    NTOK = TPC * 128
    xin = k.dram("xin", [NTOK, D], F32, "ExternalInput")
    g_rep = k.dram("g_rep", [128, D], F32, "ExternalInput")
    b_rep = k.dram("b_rep", [128, D], F32, "ExternalInput")
    ident_d = k.dram("ident", [128, 128], F32, "ExternalInput")
    h_out = k.dram("h", [NTOK, D], F32, "ExternalOutput")
    if do_proj:
        w = k.dram("w", [D, IN_W], F32, "ExternalInput")
        o32 = k.dram("o32", [NTOK, N32], F32, "ExternalOutput")
        o16 = k.dram("o16", [NTOK, N16], BF16, "ExternalOutput")

    gt = k.sb("gt", [128, D], F32)
    bt = k.sb("bt", [128, D], F32)
    idf = k.sb("idf", [128, 128], F32)
    idb = k.sb("idb", [128, 128], BF16)
    k.dma("sp", gt[:], g_rep, writes=[gt])
    k.dma("sp", bt[:], b_rep, writes=[bt])
    k.dma("sp", idf[:], ident_d, writes=[idf])
    k.op("dve", lambda e: e.tensor_copy(out=idb[:], in_=idf[:]), reads=[idf], writes=[idb])

    hT = k.sb("hT", [128, 16, NTOK], BF16) if do_proj else None
    xt = [k.sb("xt%d" % i, [128, D], F32) for i in range(2)]
    yt = [k.sb("yt%d" % i, [128, D], F32) for i in range(2)]
    yb = [k.sb("yb%d" % i, [128, D], BF16) for i in range(2)]
    sq = k.sb("sq", [128, D], BF16)
    st = [k.sb("st%d" % i, [128, 8], F32) for i in range(2)]
    pst = [k.ps("pst%d" % i, [128, 4, 128], BF16) for i in range(2)]
    pmm = [k.ps("pmm%d" % i, [128, 512], F32) for i in range(4)]

    for ti in range(TPC):
        x = xt[ti % 2]
        y = yt[ti % 2]
        s = st[ti % 2]
        k.dma("sp", x[:], xin[ti * 128:(ti + 1) * 128, :], writes=[x])
        if do_ln:
            k.op("dve", lambda e: e.tensor_reduce(out=s[:, 0:1], in_=x[:], op=ALU.add, axis=AX.X),
                 reads=[x], writes=[s])
            k.op("act", lambda e: e.activation(out=sq[:], in_=x[:], func=AF.Square, accum_out=s[:, 1:2]),
                 reads=[x], writes=[sq, s])
            k.op("dve", lambda e: e.tensor_scalar(out=s[:, 2:4], in0=s[:, 0:2], scalar1=1.0 / D, scalar2=None,
                                                  op0=ALU.mult), reads=[s], writes=[s])
            k.op("dve", lambda e: e.tensor_tensor(out=s[:, 4:5], in0=s[:, 2:3], in1=s[:, 2:3], op=ALU.mult),
                 reads=[s], writes=[s])
            k.op("dve", lambda e: e.tensor_tensor(out=s[:, 5:6], in0=s[:, 3:4], in1=s[:, 4:5], op=ALU.subtract),
                 reads=[s], writes=[s])
            k.op("dve", lambda e: e.tensor_scalar(out=s[:, 5:6], in0=s[:, 5:6], scalar1=LN_EPS, scalar2=None,
                                                  op0=ALU.add), reads=[s], writes=[s])
            k.op("act", lambda e: e.activation(out=s[:, 6:7], in_=s[:, 5:6], func=AF.Sqrt),
                 reads=[s], writes=[s])
            k.op("dve", lambda e: e.reciprocal(out=s[:, 7:8], in_=s[:, 6:7]), reads=[s], writes=[s])
            k.op("dve", lambda e: e.tensor_scalar(out=y[:], in0=x[:], scalar1=s[:, 2:3], scalar2=s[:, 7:8],
                                                  op0=ALU.subtract, op1=ALU.mult), reads=[x, s], writes=[y])
            k.op("pool", lambda e: e.tensor_tensor(out=y[:], in0=y[:], in1=gt[:], op=ALU.mult),
                 reads=[y, gt], writes=[y])
            k.op("dve", lambda e: e.tensor_tensor(out=y[:], in0=y[:], in1=bt[:], op=ALU.add),
                 reads=[y, bt], writes=[y])
        else:
            k.op("dve", lambda e: e.tensor_copy(out=y[:], in_=x[:]), reads=[x], writes=[y])
        k.dma("pool", h_out[ti * 128:(ti + 1) * 128, :], y[:], reads=[y], is_output=True)
        if do_proj:
            ybt = yb[ti % 2]
            k.op("act", lambda e: e.copy(out=ybt[:], in_=y[:]), reads=[y], writes=[ybt])
            for g4 in range(4):
                p = pst[g4 % 2]
                for j in range(4):
                    c = g4 * 4 + j
                    k.op("pe", lambda e: e.transpose(out=p[:, j, :], in_=ybt[:, c * 128:(c + 1) * 128],
                                                     identity=idb[:]), reads=[ybt, idb], writes=[p])
                k.op("dve", lambda e: e.tensor_copy(out=hT[:, g4 * 4:(g4 + 1) * 4, ti * 128:(ti + 1) * 128],
                                                    in_=p[:]), reads=[p], writes=[hT])

    if do_proj:
        wst = [k.sb("wst%d" % i, [128, 8, 512], F32) for i in range(2)]
        wbf = [k.sb("wbf%d" % i, [128, 16, 512], BF16) for i in range(2)]
        ot32 = [k.sb("ot32_%d" % i, [128, 512], F32) for i in range(3)]
        ot16 = [k.sb("ot16_%d" % i, [128, 512], BF16) for i in range(3)]
        wv = w.rearrange("(c p) n -> p c n", p=128)
        c32 = 0
        c16 = 0
        nev = 0
        for bi, (cs, cw, isb) in enumerate(P1_BLOCKS):
            wb = wbf[bi % 2]
            for half in range(2):
                ws = wst[half]
                k.dma("sp" if half == 0 else "act", ws[:, :, 0:cw],
                      wv[:, half * 8:(half + 1) * 8, cs:cs + cw], writes=[ws])
                k.op("pool" if half == 0 else "dve",
                     lambda e: e.tensor_copy(out=wb[:, half * 8:(half + 1) * 8, 0:cw], in_=ws[:, :, 0:cw]),
                     reads=[ws], writes=[wb])
            for ti in range(TPC):
                p = pmm[nev % 4]
                for c in range(16):
                    k.op("pe", lambda e: e.matmul(p[:, 0:cw], lhsT=hT[:, c, ti * 128:(ti + 1) * 128],
                                                  rhs=wb[:, c, 0:cw], start=(c == 0), stop=(c == 15)),
                         reads=[hT, wb], writes=[p])
                if isb:
                    o = ot16[nev % 3]
                    dst = o16[ti * 128:(ti + 1) * 128, c16:c16 + cw]
                else:
                    o = ot32[nev % 3]
                    dst = o32[ti * 128:(ti + 1) * 128, c32:c32 + cw]
                if nev % 2 == 0:
                    k.op("act", lambda e: e.copy(out=o[:, 0:cw], in_=p[:, 0:cw]), reads=[p], writes=[o])
                else:
                    k.op("dve", lambda e: e.tensor_copy(out=o[:, 0:cw], in_=p[:, 0:cw]), reads=[p], writes=[o])
                k.dma("pool", dst, o[:, 0:cw], reads=[o], is_output=True)
                nev += 1
            if isb:
                c16 += cw
            else:
                c32 += cw
    k.finish()
    return k


NSLOT = 17
KCH = 16
TOPK_SEL = 240
REPL = -3.0e38


def build_at(slots=None, heads=None):
    slots = list(range(NSLOT)) if slots is None else slots
    heads = list(range(8)) if heads is None else heads
    k = K()
    qT_d = k.dram("qT", [NSLOT, 128, 8, 128], BF16, "ExternalInput")
    iq_d = k.dram("iqT", [NSLOT, 64, 8, 128], BF16, "ExternalInput")
    iw_d = k.dram("iw", [NSLOT, 128, 8], F32, "ExternalInput")
    kT_d = k.dram("kT", [8, 128, LP], BF16, "ExternalInput")
    v_d = k.dram("v", [8, 128, NT, 128], BF16, "ExternalInput")
    ik_d = k.dram("ikT", [64, LP], BF16, "ExternalInput")
    fb_d = k.dram("fb", [8, 128, 9, 128], F32, "ExternalInput")
    ub_d = k.dram("ub", [128, 1024], F32, "ExternalInput")
    rb_d = k.dram("rb31", [128, 8], F32, "ExternalInput")
    id_d = k.dram("ident", [128, 128], F32, "ExternalInput")
    out_d = k.dram("ya", [NSLOT, 8, 128, 128], BF16, "ExternalOutput")

    sidx = k.sb("sidx", [128, LP], F32)
    maskT = k.sb("maskT", [128, NT, 128], BF16)
    ikT = k.sb("ikT_s", [64, LP], BF16)
    kch = [k.sb("kch%d" % i, [128, KCH * 128], BF16) for i in range(2)]
    vch = [k.sb("vch%d" % i, [128, KCH, 128], BF16) for i in range(2)]
    qs = k.sb("qs", [128, 8, 128], BF16)
    iqs = k.sb("iqs", [64, 8, 128], BF16)
    iws = k.sb("iws", [128, 8], F32)
    fe = k.sb("fe", [128, 8, 9, 128], BF16)
    fbs = k.sb("fbs", [128, 9, 128], F32)
    ubs = k.sb("ubs", [128, 1024], F32)
    rb = k.sb("rb", [128, 8], F32)
    nrb = k.sb("nrb", [128, 8], F32)
    idf = k.sb("idf", [128, 128], F32)
    onesb = k.sb("onesb", [128, 128], BF16)
    rl = [k.sb("rl%d" % i, [128, 512], F32) for i in range(2)]
    pex = [k.sb("pex%d" % i, [128, 4, 128], BF16) for i in range(2)]
    m8 = k.sb("m8", [128, 8], F32)
    rden = k.sb("rden", [128, 128], F32)
    ot = [k.sb("ot%d" % i, [128, 128], BF16) for i in range(2)]

    pidx = [k.ps("pidx%d" % i, [128, 512], F32) for i in range(2)]
    ptr = k.ps("ptr", [128, 4, 128], F32)
    pst = [k.ps("pst%d" % i, [128, 4, 128], F32) for i in range(2)]
    py = k.ps("py", [128, 128], F32)
    pden = k.ps("pden", [128, 128], F32)

    k.dma("sp", ikT[:], ik_d, writes=[ikT])
    k.dma("sp", ubs[:], ub_d, writes=[ubs])
    k.dma("sp", rb[:], rb_d, writes=[rb])
    k.dma("sp", idf[:], id_d, writes=[idf])
    k.op("dve", lambda e: e.tensor_scalar(out=nrb[:], in0=rb[:], scalar1=-1.0, scalar2=None, op0=ALU.mult),
         reads=[rb], writes=[nrb])
    k.op("dve", lambda e: e.memset(onesb[:], 1.0), writes=[onesb])
    for h in range(8):
        k.dma("sp", fbs[:], fb_d[h], writes=[fbs])
        k.op("act", lambda e: e.activation(out=fe[:, h, :, :], in_=fbs[:], func=AF.Exp, bias=nrb[:, h:h + 1], scale=1.0),
             reads=[fbs, nrb], writes=[fe])

    scale = 128 ** -0.5
    nch = 0
    nev = 0
    for kslot in slots:
        nk = 8 * kslot + 8
        nkeys = nk * 128
        k.dma("sp", qs[:], qT_d[kslot], writes=[qs])
        k.dma("sp", iqs[:], iq_d[kslot], writes=[iqs])
        k.dma("sp", iws[:], iw_d[kslot], writes=[iws])
        for blk in range(nk // 4):
            cs = blk * 512
            for h in range(8):
                p = pidx[nev % 2]
                r = rl[nev % 2]
                nev += 1
                k.op("pe", lambda e: e.matmul(p[:], lhsT=iqs[:, h, :], rhs=ikT[:, cs:cs + 512], start=True, stop=True),
                     reads=[iqs, ikT], writes=[p])
                k.op("act", lambda e: e.activation(out=r[:], in_=p[:], func=AF.Relu), reads=[p], writes=[r])
                if h == 0:
                    k.op("dve", lambda e: e.tensor_scalar(out=sidx[:, cs:cs + 512], in0=r[:], scalar1=iws[:, 0:1],
                                                          scalar2=None, op0=ALU.mult), reads=[r, iws], writes=[sidx])
                else:
                    k.op("dve", lambda e: e.scalar_tensor_tensor(out=sidx[:, cs:cs + 512], in0=r[:], scalar=iws[:, h:h + 1],
                                                                 in1=sidx[:, cs:cs + 512], op0=ALU.mult, op1=ALU.add),
                         reads=[r, iws, sidx], writes=[sidx])
        k.op("dve", lambda e: e.tensor_tensor(out=sidx[:, nkeys - 1024:nkeys], in0=sidx[:, nkeys - 1024:nkeys],
                                              in1=ubs[:], op=ALU.min), reads=[sidx, ubs], writes=[sidx])
        hi = min(nkeys, L)
        for rnd in range(TOPK_SEL // 8):
            k.op("dve", lambda e: e.max(out=m8[:], in_=sidx[:, 16:hi]), reads=[sidx], writes=[m8])
            k.op("dve", lambda e: e.match_replace(out=sidx[:, 16:hi], in_to_replace=m8[:], in_values=sidx[:, 16:hi],
                                                  imm_value=REPL), reads=[sidx, m8], writes=[sidx])
        k.op("dve", lambda e: e.tensor_scalar(out=sidx[:, 16:hi], in0=sidx[:, 16:hi], scalar1=-2.0e38, scalar2=None,
                                              op0=ALU.is_le), reads=[sidx], writes=[sidx])
        k.op("dve", lambda e: e.memset(sidx[:, 0:16], 1.0), writes=[sidx])
        if hi < nkeys:
            k.op("dve", lambda e: e.memset(sidx[:, hi:nkeys], 0.0), writes=[sidx])
        for g in range(nk // 4):
            for j in range(4):
                kt = g * 4 + j
                k.op("pe", lambda e: e.transpose(out=ptr[:, j, :], in_=sidx[:, kt * 128:(kt + 1) * 128], identity=idf[:]),
                     reads=[sidx, idf], writes=[ptr])
            if g % 2 == 0:
                k.op("act", lambda e: e.copy(out=maskT[:, g * 4:(g + 1) * 4, :], in_=ptr[:]), reads=[ptr], writes=[maskT])
            else:
                k.op("dve", lambda e: e.tensor_copy(out=maskT[:, g * 4:(g + 1) * 4, :], in_=ptr[:]), reads=[ptr], writes=[maskT])
        for h in heads:
            first = True
            for c0 in range(0, nk, KCH):
                cn = min(KCH, nk - c0)
                kc = kch[nch % 2]
                vc = vch[nch % 2]
                nch += 1
                k.dma("sp", kc[:, 0:cn * 128], kT_d[h, :, c0 * 128:(c0 + cn) * 128], writes=[kc])
                k.dma("act", vc[:, 0:cn, :], v_d[h, :, c0:c0 + cn, :], writes=[vc])
                for g in range(cn // 4):
                    ps = pst[nev % 2]
                    px = pex[nev % 2]
                    nev += 1
                    for j in range(4):
                        kt = g * 4 + j
                        k.op("pe", lambda e: e.matmul(ps[:, j, :], lhsT=kc[:, kt * 128:(kt + 1) * 128], rhs=qs[:, h, :],
                                                      start=True, stop=True), reads=[kc, qs], writes=[ps])
                    k.op("act", lambda e: e.activation(out=px[:], in_=ps[:], func=AF.Exp, bias=rb[:, h:h + 1], scale=scale),
                         reads=[ps, rb], writes=[px])
                    gt0 = c0 + g * 4
                    k.op("dve", lambda e: e.tensor_tensor(out=px[:], in0=px[:], in1=maskT[:, gt0:gt0 + 4, :], op=ALU.mult),
                         reads=[px, maskT], writes=[px])
                    for j in range(4):
                        m = gt0 + j - (nk - 9)
                        if m >= 0:
                            k.op("dve", lambda e: e.tensor_tensor(out=px[:, j, :], in0=px[:, j, :], in1=fe[:, h, m, :], op=ALU.mult),
                                 reads=[px, fe], writes=[px])
                    for j in range(4):
                        kt = g * 4 + j
                        last = (c0 + kt == nk - 1)
                        k.op("pe", lambda e: e.matmul(py[:], lhsT=vc[:, kt, :], rhs=px[:, j, :], start=first, stop=last),
                             reads=[vc, px], writes=[py])
                        k.op("pe", lambda e: e.matmul(pden[:], lhsT=onesb[:], rhs=px[:, j, :], start=first, stop=last),
                             reads=[onesb, px], writes=[pden])
                        first = False
            o = ot[h % 2]
            k.op("dve", lambda e: e.reciprocal(out=rden[:], in_=pden[:]), reads=[pden], writes=[rden])
            k.op("dve", lambda e: e.tensor_tensor(out=o[:], in0=py[:], in1=rden[:], op=ALU.mult), reads=[py, rden], writes=[o])
            k.dma("pool", out_d[kslot, h], o[:], reads=[o], is_output=True)
    k.finish()
    return k


def _t5_bucket(rel):
    rel = np.maximum(rel, 0)
    me = 16
    rf = np.maximum(rel, 1).astype(np.float32)
    large = me + (np.log(rf / me) / np.log(128 / me) * (32 - me)).astype(np.int32)
    large = np.minimum(large, 31)
    return np.where(rel < me, rel, large)


def at_inputs(aq, ak, av, iq, ik, iw, rel_bias):
    kT = np.ascontiguousarray(ak.reshape(LP, 8, 128).transpose(1, 2, 0))
    v = np.ascontiguousarray(av.reshape(NT, 128, 8, 128).transpose(2, 1, 0, 3))
    ikT = np.ascontiguousarray(ik.T)
    rb31 = np.ascontiguousarray(np.broadcast_to(rel_bias[31], (128, 8))).astype(np.float32)
    ident = np.eye(128, dtype=np.float32)
    aq4 = aq.reshape(NT, 128, 8, 128)
    iq4 = iq.reshape(NT, 128, 8, 64)
    iw3 = iw.reshape(NT, 128, 8)
    s_i = np.arange(128)[:, None]
    q_i = np.arange(128)[None, :]
    maps = []
    for c in range(NCORES):
        qT = np.ascontiguousarray(aq4[c::8].transpose(0, 3, 2, 1))
        iqT = np.ascontiguousarray(iq4[c::8].transpose(0, 3, 2, 1))
        iwc = np.ascontiguousarray(iw3[c::8])
        fb = np.empty((8, 128, 9, 128), np.float32)
        for m in range(9):
            r = c + 1 - m
            dist = r * 128 + q_i - s_i
            vis = (dist >= 0) & (r >= 0)
            idx = _t5_bucket(np.where(vis, dist, 0))
            vals = rel_bias[idx]
            fb[:, :, m, :] = np.where(vis[None], vals.transpose(2, 0, 1), np.float32(-1e30))
        ub = np.empty((128, 8, 128), np.float32)
        for mp in range(8):
            if mp < c:
                ub[:, mp, :] = 3e38
            elif mp > c:
                ub[:, mp, :] = -1e30
            else:
                ub[:, mp, :] = np.where(np.arange(128)[None, :] <= np.arange(128)[:, None], 3e38, -1e30)
        maps.append({"qT": qT, "iqT": iqT, "iw": iwc, "kT": kT, "v": v, "ikT": ikT, "fb": fb,
                     "ub": ub.reshape(128, 1024), "rb31": rb31, "ident": ident})
    return maps


MG = 8


def build_ml(ngroups=None):
    ngroups = NT // MG if ngroups is None else ngroups
    k = K()
    qsh_d = k.dram("qsh", [128, NT, 4 * 128], BF16, "ExternalInput")
    ksh_d = k.dram("ksh", [128, NT, 4 * 128], BF16, "ExternalInput")
    va_d = k.dram("vaug", [128, NT, 256], BF16, "ExternalInput")
    mif_d = k.dram("mif", [128, NT, 2], F32, "ExternalInput")
    cw_d = k.dram("cwg", [128, 2, 4, MG * 128], F32, "ExternalInput")
    cb_d = k.dram("cbg", [128, 2, MG * 128], F32, "ExternalInput")
    gb_d = k.dram("gb", [128, 2], F32, "ExternalInput")
    tri_d = k.dram("tri", [128, 128], F32, "ExternalInput")
    id_d = k.dram("ident", [128, 128], F32, "ExternalInput")
    out_d = k.dram("hm", [128, LP], BF16, "ExternalOutput")

    cw = k.sb("cw", [128, 2, 4, MG * 128], F32)
    cb = k.sb("cb", [128, 2, MG * 128], F32)
    gb = k.sb("gb_s", [128, 2], F32)
    ngb = k.sb("ngb", [128, 2], F32)
    tri = k.sb("tri_s", [128, 128], F32)
    idf = k.sb("idf", [128, 128], F32)
    idb = k.sb("idb", [128, 128], BF16)
    onesf = k.sb("onesf", [128, 128], F32)
    mif = k.sb("mif_s", [128, NT, 2], F32)
    lf = k.sb("lf", [128, NT], F32)
    tmpg = k.sb("tmpg", [128, NT], F32)
    qsc = k.sb("qsc", [128, NT], F32)
    ksc = k.sb("ksc", [128, NT], F32)
    gl = k.sb("gl", [128, NT], F32)
    qsh = [k.sb("qsh%d" % i, [128, MG, 512], BF16) for i in range(2)]
    ksh = [k.sb("ksh%d" % i, [128, MG, 512], BF16) for i in range(2)]
    va = [k.sb("va%d" % i, [128, MG, 256], BF16) for i in range(2)]
    accq = k.sb("accq", [128, MG * 128], F32)
    acck = k.sb("acck", [128, MG * 128], F32)
    tq = k.sb("tq", [128, MG * 128], F32)
    tk_ = k.sb("tk", [128, MG * 128], F32)
    qtm = k.sb("qtm", [128, MG, 128], BF16)
    ktm = k.sb("ktm", [128, MG, 128], BF16)
    qT = k.sb("qT_s", [128, MG, 128], BF16)
    kT = k.sb("kT_s", [128, MG, 128], BF16)
    AT = [k.sb("AT%d" % i, [128, 128], BF16) for i in range(2)]
    Cnf = k.sb("Cnf", [128, 256], F32)
    Cnb = k.sb("Cnb", [128, 256], BF16)
    dd = k.sb("dd", [128, 128], F32)
    ho = [k.sb("ho%d" % i, [128, MG * 128], BF16) for i in range(2)]

    pg = k.ps("pg", [128, 512], F32)
    ptq = k.ps("ptq", [128, 4, 128], BF16)
    ptk = k.ps("ptk", [128, 4, 128], BF16)
    pA = [k.ps("pA%d" % i, [128, 128], F32) for i in range(2)]
    pnum = k.ps("pnum", [128, 128], F32)
    pden = k.ps("pden", [128, 128], F32)
    pCn = k.ps("pCn", [128, 256], F32)

    for dst, src in ((cw, cw_d), (cb, cb_d), (gb, gb_d), (tri, tri_d), (idf, id_d), (mif, mif_d)):
        k.dma("sp", dst[:], src, writes=[dst])
    k.op("dve", lambda e: e.tensor_copy(out=idb[:], in_=idf[:]), reads=[idf], writes=[idb])
    k.op("dve", lambda e: e.memset(onesf[:], 1.0), writes=[onesf])
    k.op("dve", lambda e: e.tensor_scalar(out=ngb[:], in0=gb[:], scalar1=-1.0, scalar2=None, op0=ALU.mult),
         reads=[gb], writes=[ngb])
    k.op("dve", lambda e: e.memset(Cnf[:], 0.0), writes=[Cnf])
    k.op("dve", lambda e: e.memset(Cnb[:], 0.0), writes=[Cnb])
    k.op("act", lambda e: e.activation(out=tmpg[:], in_=mif[:, :, 1], func=AF.Exp, bias=ngb[:, 1:2], scale=-1.0),
         reads=[mif, ngb], writes=[tmpg])
    k.op("act", lambda e: e.activation(out=tmpg[:], in_=tmpg[:], func=AF.Ln, bias=1.0, scale=1.0),
         reads=[tmpg], writes=[tmpg])
    k.op("dve", lambda e: e.tensor_scalar(out=lf[:], in0=tmpg[:], scalar1=-1.0, scalar2=None, op0=ALU.mult),
         reads=[tmpg], writes=[lf])
    k.op("pe", lambda e: e.matmul(pg[:, 0:NT], lhsT=tri[:], rhs=lf[:], start=True, stop=True), reads=[tri, lf], writes=[pg])
    k.op("pe", lambda e: e.matmul(pg[:, 256:256 + NT], lhsT=onesf[:], rhs=lf[:], start=True, stop=True),
         reads=[onesf, lf], writes=[pg])
    import math
    fsrc = k.sb("fsrc", [128, 512], BF16)
    k.op("dve", lambda e: e.memset(fsrc[:], 0.0), writes=[fsrc])
    k._fsrc = fsrc
    k.pe_fence(idb, [pg], pCn, 256, reps=16)
    k.op("act", lambda e: e.activation(out=qsc[:], in_=pg[:, 0:NT], func=AF.Exp, bias=-0.5 * math.log(128.0), scale=1.0),
         reads=[pg], writes=[qsc])
    k.op("dve", lambda e: e.tensor_tensor(out=tmpg[:], in0=mif[:, :, 0], in1=pg[:, 0:NT], op=ALU.subtract),
         reads=[mif, pg], writes=[tmpg])
    k.op("act", lambda e: e.activation(out=ksc[:], in_=tmpg[:], func=AF.Exp, bias=gb[:, 0:1], scale=1.0),
         reads=[tmpg, gb], writes=[ksc])
    k.op("act", lambda e: e.activation(out=gl[:], in_=pg[:, 256:256 + NT], func=AF.Exp), reads=[pg], writes=[gl])

    for g in range(ngroups):
        c0 = g * MG
        qs_, ks_, va_ = qsh[g % 2], ksh[g % 2], va[g % 2]
        k.dma("sp", qs_[:], qsh_d[:, c0:c0 + MG, :], writes=[qs_])
        k.dma("act", ks_[:], ksh_d[:, c0:c0 + MG, :], writes=[ks_])
        k.dma("sp", va_[:], va_d[:, c0:c0 + MG, :], writes=[va_])
        for which, (src, acc, tmp, eng) in enumerate(((qs_, accq, tq, "dve"), (ks_, acck, tk_, "pool"))):
            a3 = acc[:].rearrange("p (g d) -> p g d", g=MG)
            t3 = tmp[:].rearrange("p (g d) -> p g d", g=MG)
            for j in range(4):
                w3 = cw[:, which, j, :].rearrange("p (g d) -> p g d", g=MG)
                dst = a3 if j == 0 else t3
                k.op(eng, lambda e: e.tensor_tensor(out=dst, in0=src[:, :, j * 128:(j + 1) * 128], in1=w3, op=ALU.mult),
                     reads=[src, cw], writes=[acc if j == 0 else tmp])
                if j > 0:
                    k.op(eng, lambda e: e.tensor_tensor(out=acc[:], in0=acc[:], in1=tmp[:], op=ALU.add),
                         reads=[acc, tmp], writes=[acc])
            k.op(eng, lambda e: e.tensor_tensor(out=acc[:], in0=acc[:], in1=cb[:, which, :], op=ALU.add),
                 reads=[acc, cb], writes=[acc])
            k.op("act", lambda e: e.activation(out=acc[:], in_=acc[:], func=AF.Silu), reads=[acc], writes=[acc])
        for ci in range(MG):
            c = c0 + ci
            k.op("dve", lambda e: e.tensor_scalar(out=qtm[:, ci, :], in0=accq[:, ci * 128:(ci + 1) * 128],
                                                  scalar1=qsc[:, c:c + 1], scalar2=None, op0=ALU.mult),
                 reads=[accq, qsc], writes=[qtm])
            k.op("dve", lambda e: e.tensor_scalar(out=ktm[:, ci, :], in0=acck[:, ci * 128:(ci + 1) * 128],
                                                  scalar1=ksc[:, c:c + 1], scalar2=None, op0=ALU.mult),
                 reads=[acck, ksc], writes=[ktm])
        for half in range(MG // 4):
            for j in range(4):
                ci = half * 4 + j
                k.op("pe", lambda e: e.transpose(out=ptq[:, j, :], in_=qtm[:, ci, :], identity=idb[:]),
                     reads=[qtm, idb], writes=[ptq])
                k.op("pe", lambda e: e.transpose(out=ptk[:, j, :], in_=ktm[:, ci, :], identity=idb[:]),
                     reads=[ktm, idb], writes=[ptk])
            k.op("act", lambda e: e.copy(out=qT[:, half * 4:(half + 1) * 4, :], in_=ptq[:]), reads=[ptq], writes=[qT])
            k.op("dve", lambda e: e.tensor_copy(out=kT[:, half * 4:(half + 1) * 4, :], in_=ptk[:]), reads=[ptk], writes=[kT])
        hob = ho[g % 2]
        for ci in range(MG):
            c = c0 + ci
            pa = pA[ci % 2]
            at = AT[ci % 2]
            k.op("pe", lambda e: e.matmul(pa[:], lhsT=kT[:, ci, :], rhs=qT[:, ci, :], start=True, stop=True),
                 reads=[kT, qT], writes=[pa])
            k.op("dve", lambda e: e.tensor_tensor(out=at[:], in0=pa[:], in1=tri[:], op=ALU.mult),
                 reads=[pa, tri], writes=[at])
            k.op("pe", lambda e: e.matmul(pnum[:], lhsT=va_[:, ci, 0:128], rhs=at[:], start=True, stop=False),
                 reads=[va_, at], writes=[pnum])
            k.op("pe", lambda e: e.matmul(pnum[:], lhsT=Cnb[:, 0:128], rhs=qT[:, ci, :], start=False, stop=True),
                 reads=[Cnb, qT], writes=[pnum])
            k.op("pe", lambda e: e.matmul(pden[:], lhsT=va_[:, ci, 128:256], rhs=at[:], start=True, stop=False),
                 reads=[va_, at], writes=[pden])
            k.op("pe", lambda e: e.matmul(pden[:], lhsT=Cnb[:, 128:256], rhs=qT[:, ci, :], start=False, stop=True),
                 reads=[Cnb, qT], writes=[pden])
            k.op("act", lambda e: e.activation(out=dd[:], in_=pden[:], func=AF.Abs), reads=[pden], writes=[dd])
            k.op("dve", lambda e: e.tensor_scalar(out=dd[:], in0=dd[:], scalar1=1.0, scalar2=None, op0=ALU.max),
                 reads=[dd], writes=[dd])
            k.op("dve", lambda e: e.reciprocal(out=dd[:], in_=dd[:]), reads=[dd], writes=[dd])
            k.op("dve", lambda e: e.tensor_tensor(out=hob[:, ci * 128:(ci + 1) * 128], in0=pnum[:], in1=dd[:], op=ALU.mult),
                 reads=[pnum, dd], writes=[hob])
            k.op("pe", lambda e: e.matmul(pCn[:], lhsT=ktm[:, ci, :], rhs=va_[:, ci, :], start=True, stop=True),
                 reads=[ktm, va_], writes=[pCn])
            k.op("dve", lambda e: e.tensor_tensor(out=Cnf[:], in0=Cnf[:], in1=pCn[:], op=ALU.add),
                 reads=[Cnf, pCn], writes=[Cnf])
            k.op("dve", lambda e: e.tensor_scalar(out=Cnf[:], in0=Cnf[:], scalar1=gl[:, c:c + 1], scalar2=None, op0=ALU.mult),
                 reads=[Cnf, gl], writes=[Cnf])
            k.op("act", lambda e: e.copy(out=Cnb[:], in_=Cnf[:]), reads=[Cnf], writes=[Cnb])
        k.dma("pool", out_d[:, c0 * 128:(c0 + MG) * 128], hob[:], reads=[hob], is_output=True)
    k.finish()
    return k


def ml_inputs(mq, mk, mv, mi, mf, conv_w, conv_b, b_ig, b_fg):
    tri = np.triu(np.ones((128, 128), np.float32))
    ident = np.eye(128, dtype=np.float32)

    def shifted(u):
        out = np.zeros((LP, 4, 128), u.dtype)
        for j in range(4):
            sh = 3 - j
            if sh == 0:
                out[:, j] = u
            else:
                out[sh:, j] = u[:-sh]
        return np.ascontiguousarray(out.reshape(NT, 128, 512).transpose(1, 0, 2))

    maps = []
    for c in range(NCORES):
        hd, vh = c // 2, c % 2
        qsh = shifted(mq[:, hd * 128:(hd + 1) * 128])
        ksh = shifted(mk[:, hd * 128:(hd + 1) * 128])
        vaug = np.ones((LP, 256), mv.dtype)
        vaug[:, :128] = mv[:, hd * 256 + vh * 128: hd * 256 + (vh + 1) * 128]
        vaug = np.ascontiguousarray(vaug.reshape(NT, 128, 256).transpose(1, 0, 2))
        mif = np.stack([mi[:, hd], mf[:, hd]], -1).astype(np.float32)
        mif = np.ascontiguousarray(mif.reshape(NT, 128, 2).transpose(1, 0, 2))
        cwq = conv_w[:, hd * 128:(hd + 1) * 128]
        cwk = conv_w[:, 512 + hd * 128:512 + (hd + 1) * 128]
        cw = np.stack([cwq, cwk], 0)
        cwg = np.ascontiguousarray(np.broadcast_to(cw[None, :, :, None, :], (128, 2, 4, MG, 128))).reshape(128, 2, 4, MG * 128)
        cbq = conv_b[hd * 128:(hd + 1) * 128]
        cbk = conv_b[512 + hd * 128:512 + (hd + 1) * 128]
        cb2 = np.stack([cbq, cbk], 0)
        cbg = np.ascontiguousarray(np.broadcast_to(cb2[None, :, None, :], (128, 2, MG, 128))).reshape(128, 2, MG * 128)
        gb = np.ascontiguousarray(np.broadcast_to(np.array([b_ig[hd], b_fg[hd]], np.float32), (128, 2)))
        maps.append({"qsh": qsh, "ksh": ksh, "vaug": vaug, "mif": mif, "cwg": cwg.astype(np.float32),
                     "cbg": cbg.astype(np.float32), "gb": gb, "tri": tri, "ident": ident})
    return maps


def _pa_blocks():
    blocks = []
    segs = [(0, 2048, True),
            (3072, 8, False),
            (3080, 3648, True),
            (6728, 8, False)]
    for s, w, b in segs:
        o = 0
        while o < w:
            ww = min(512, w - o)
            blocks.append((s + o, ww, b))
            o += ww
    return blocks


PA_BLOCKS = _pa_blocks()
PA16 = sum(w for _, w, b in PA_BLOCKS if b)
PA32 = sum(w for _, w, b in PA_BLOCKS if not b)
NTOK = TPC * 128


class Stager:
    def __init__(self, k, st, n=4):
        self.k = k
        self.bufs = [k.sb("stg%d" % i, [128, 2048], F32, st) for i in range(n)]
        self.i = 0
        self.ce = 0

    def load(self, src_view, dst_ap, dstT, shape3=None, nelem=2048):
        k = self.k
        b = self.bufs[self.i % len(self.bufs)]
        self.i += 1
        if shape3 is None:
            v = b[:, 0:nelem]
        else:
            c, n = shape3
            v = b[:, 0:c * n].rearrange("p (c n) -> p c n", c=c)
        k.dma("sp", v, src_view, writes=[b])
        eng = ("pool", "dve", "act")[self.ce % 3]
        self.ce += 1
        if eng == "act":
            k.op("act", lambda e: e.copy(out=dst_ap, in_=v), reads=[b], writes=[dstT])
        else:
            k.op(eng, lambda e: e.tensor_copy(out=dst_ap, in_=v), reads=[b], writes=[dstT])


def _ln_rows(k, x, y, s, sq, gt, bt, ncols=D):
    k.op("dve", lambda e: e.tensor_reduce(out=s[:, 0:1], in_=x[:], op=ALU.add, axis=AX.X), reads=[x], writes=[s])
    k.op("act", lambda e: e.activation(out=sq[:], in_=x[:], func=AF.Square, accum_out=s[:, 1:2]), reads=[x], writes=[sq, s])
    k.op("dve", lambda e: e.tensor_scalar(out=s[:, 2:4], in0=s[:, 0:2], scalar1=1.0 / ncols, scalar2=None, op0=ALU.mult),
         reads=[s], writes=[s])
    k.op("dve", lambda e: e.tensor_tensor(out=s[:, 4:5], in0=s[:, 2:3], in1=s[:, 2:3], op=ALU.mult), reads=[s], writes=[s])
    k.op("dve", lambda e: e.tensor_tensor(out=s[:, 5:6], in0=s[:, 3:4], in1=s[:, 4:5], op=ALU.subtract), reads=[s], writes=[s])
    k.op("dve", lambda e: e.tensor_scalar(out=s[:, 5:6], in0=s[:, 5:6], scalar1=LN_EPS, scalar2=None, op0=ALU.add),
         reads=[s], writes=[s])
    k.op("act", lambda e: e.activation(out=s[:, 6:7], in_=s[:, 5:6], func=AF.Sqrt), reads=[s], writes=[s])
    k.op("dve", lambda e: e.reciprocal(out=s[:, 7:8], in_=s[:, 6:7]), reads=[s], writes=[s])
    k.op("dve", lambda e: e.tensor_scalar(out=y[:], in0=x[:], scalar1=s[:, 2:3], scalar2=s[:, 7:8],
                                          op0=ALU.subtract, op1=ALU.mult), reads=[x, s], writes=[y])
    k.op("pool", lambda e: e.tensor_tensor(out=y[:], in0=y[:], in1=gt[:], op=ALU.mult), reads=[y, gt], writes=[y])
    k.op("dve", lambda e: e.tensor_tensor(out=y[:], in0=y[:], in1=bt[:], op=ALU.add), reads=[y, bt], writes=[y])


class _Stop(Exception):
    pass


def build_pba(last=False, first=False, stop_after=99, NVB=1):
    k = K()
    A_ALPHA = float(ALPHA)
    ident_d = k.dram("ident", [128, 128], F32, "ExternalInput")
    NALL = NVB * NTOK
    hout_d = k.dram("hout", [NALL, D], F32, "ExternalOutput")
    if first:
        xin_d = k.dram("xin", [NALL, D], F32, "ExternalInput")
    else:
        h_d = k.dram("h", [NALL, D], F32, "ExternalInput")
        hm_d = k.dram("hm", [NALL, 1024], BF16, "ExternalInput")
        yaT_d = k.dram("yaT", [1024, NALL], BF16, "ExternalInput")
        wg_d = k.dram("wg", [D, 5120], F32, "ExternalInput")
        mhg_d = k.dram("mhg", [128, 1024], F32, "ExternalInput")
        wpm_d = k.dram("wpm", [1024, D], F32, "ExternalInput")
        wpa_d = k.dram("wpa", [1024, D], F32, "ExternalInput")
        wout_d = k.dram("wout", [D, D], F32, "ExternalInput")
        g1_d = k.dram("g1", [128, D], F32, "ExternalInput")
        b1_d = k.dram("b1", [128, D], F32, "ExternalInput")
        wr_d = k.dram("wr", [D, 36], F32, "ExternalInput")
        br_d = k.dram("br", [128, 36], F32, "ExternalInput")
        wgu_d = k.dram("wgu", [32, D, 1024], F32, "ExternalInput")
        wd_d = k.dram("wd", [32, 512, D], F32, "ExternalInput")
        mrg_d = k.dram("s_mrg", [NTOK, D], BF16, "Internal")
        pre1_d = k.dram("s_pre1", [NTOK, D], F32, "Internal")
        h1_d = k.dram("s_h1", [NTOK, D], F32, "Internal")
        moe_d = k.dram("s_moe", [NTOK, D], F32, "Internal")
    g2_d = k.dram("g2", [128, D], F32, "ExternalInput")
    b2_d = k.dram("b2", [128, D], F32, "ExternalInput")
    if not last:
        wn_d = k.dram("wn", [D, IN_W], F32, "ExternalInput")
        o16_d = k.dram("o16", [NALL, PA16], BF16, "ExternalOutput")
        o32_d = k.dram("o32", [NALL, PA32], F32, "ExternalOutput")

    bufA = k.sb("bufA", [128, 16, NTOK], BF16)
    A = [T(bufA.t, "A%d" % i) for i in range(TPC)]
    idf = k.sb("idf", [128, 128], F32)
    idb = k.sb("idb", [128, 128], BF16)
    Gall = k.sb("Gall", [128, TPC, 32], F32)
    fsrc = k.sb("fsrc", [128, 512], BF16)
    k.op("dve", lambda e: e.memset(fsrc[:], 0.0), writes=[fsrc])
    k._fsrc = fsrc
    k.dma("sp", idf[:], ident_d, writes=[idf])
    k.op("dve", lambda e: e.tensor_copy(out=idb[:], in_=idf[:]), reads=[idf], writes=[idb])
    PB = [k.ps("pb%d" % i, [128, 512], F32) for i in range(6)]
    PT32 = k.ps("pt32", [128, 4, 128], F32)
    PT16 = k.ps("pt16", [128, 4, 128], BF16)

    for vb in range(NVB):
        base = vb * NTOK

        def tsl(ti):
            return slice(ti * 128, (ti + 1) * 128)

        def rsl(ti):
            return slice(base + ti * 128, base + (ti + 1) * 128)

        def to_bufA(src_bf, ti, nchunk=16, dstbuf=None, dstT=None):
            dstbuf = bufA if dstbuf is None else dstbuf
            dstT = A[ti] if dstT is None else dstT
            for g4 in range(nchunk // 4):
                for j in range(4):
                    c = g4 * 4 + j
                    k.op("pe", lambda e: e.transpose(out=PT16[:, j, :], in_=src_bf[:, c * 128:(c + 1) * 128], identity=idb[:]),
                         reads=[src_bf, idb], writes=[PT16])
                k.op("dve", lambda e: e.tensor_copy(out=dstbuf[:, g4 * 4:(g4 + 1) * 4, tsl(ti)], in_=PT16[:]),
                     reads=[PT16], writes=[dstT])

        if not first:
            outer = ExitStack()
            bufB = k.sb("bufB", [128, 8, NTOK], BF16, outer)
            Bg = [T(bufB.t, "B%d" % i) for i in range(TPC)]
            with ExitStack() as st:
                stg = Stager(k, st, n=2)
                wmo = k.sb("wmo", [128, 16, 1024], BF16, st)
                mhg = k.sb("mhg", [128, 1024], F32, st)
                xt = [k.sb("xt%d" % i, [128, D], F32, st) for i in range(2)]
                xb = k.sb("xb", [128, D], BF16, st)
                hmt = [k.sb("hmt%d" % i, [128, 1024], BF16, st) for i in range(2)]
                hn = k.sb("hn", [128, 1024], F32, st)
                sq = k.sb("sq", [128, 1024], F32, st)
                sg = [k.sb("sg%d" % i, [128, 512], F32, st) for i in range(2)]
                ym = k.sb("ym", [128, 1024], BF16, st)
                s4 = k.sb("s4", [128, 32], F32, st)
                k.dma("sp", mhg[:], mhg_d, writes=[mhg])
                wv = wg_d.rearrange("(c p) n -> p c n", p=128)
                for u in range(8):
                    stg.load(wv[:, 2 * u:2 * u + 2, 0:1024], wmo[:, 2 * u:2 * u + 2, :], wmo, shape3=(2, 1024))
                nev = 0
                for ti in range(TPC):
                    x = xt[ti % 2]
                    hm = hmt[ti % 2]
                    k.dma("sp", x[:], h_d[rsl(ti), :], writes=[x])
                    k.dma("sp", hm[:], hm_d[rsl(ti), :], writes=[hm])
                    k.op("act", lambda e: e.copy(out=xb[:], in_=x[:]), reads=[x], writes=[xb])
                    to_bufA(xb, ti)
                    hm3 = hm[:].rearrange("p (h v) -> p h v", h=4)
                    sq3 = sq[:].rearrange("p (h v) -> p h v", h=4)
                    k.op("dve", lambda e: e.tensor_reduce(out=s4[:, 0:4], in_=hm3, op=ALU.add, axis=AX.X), reads=[hm], writes=[s4])
                    k.op("pool", lambda e: e.tensor_tensor(out=sq[:], in0=hm[:], in1=hm[:], op=ALU.mult), reads=[hm], writes=[sq])
                    k.op("dve", lambda e: e.tensor_reduce(out=s4[:, 4:8], in_=sq3, op=ALU.add, axis=AX.X), reads=[sq], writes=[s4])
                    k.op("dve", lambda e: e.tensor_scalar(out=s4[:, 8:16], in0=s4[:, 0:8], scalar1=1.0 / 256, scalar2=None, op0=ALU.mult),
                         reads=[s4], writes=[s4])
                    k.op("dve", lambda e: e.tensor_tensor(out=s4[:, 16:20], in0=s4[:, 8:12], in1=s4[:, 8:12], op=ALU.mult),
                         reads=[s4], writes=[s4])
                    k.op("dve", lambda e: e.tensor_tensor(out=s4[:, 20:24], in0=s4[:, 12:16], in1=s4[:, 16:20], op=ALU.subtract),
                         reads=[s4], writes=[s4])
                    k.op("dve", lambda e: e.tensor_scalar(out=s4[:, 20:24], in0=s4[:, 20:24], scalar1=LN_EPS, scalar2=None, op0=ALU.add),
                         reads=[s4], writes=[s4])
                    k.op("act", lambda e: e.activation(out=s4[:, 24:28], in_=s4[:, 20:24], func=AF.Sqrt), reads=[s4], writes=[s4])
                    k.op("dve", lambda e: e.reciprocal(out=s4[:, 28:32], in_=s4[:, 24:28]), reads=[s4], writes=[s4])
                    for hd in range(4):
                        k.op("dve", lambda e: e.tensor_scalar(out=hn[:, hd * 256:(hd + 1) * 256], in0=hm[:, hd * 256:(hd + 1) * 256],
                                                              scalar1=s4[:, 8 + hd:9 + hd], scalar2=s4[:, 28 + hd:29 + hd],
                                                              op0=ALU.subtract, op1=ALU.mult), reads=[hm, s4], writes=[hn])
                    k.op("pool", lambda e: e.tensor_tensor(out=hn[:], in0=hn[:], in1=mhg[:], op=ALU.mult), reads=[hn, mhg], writes=[hn])
                    for cbk in range(2):
                        p = PB[nev % 6]
                        sgt = sg[nev % 2]
                        nev += 1
                        for c in range(16):
                            k.op("pe", lambda e: e.matmul(p[:], lhsT=bufA[:, c, tsl(ti)], rhs=wmo[:, c, cbk * 512:(cbk + 1) * 512],
                                                          start=(c == 0), stop=(c == 15)), reads=[A[ti], wmo], writes=[p])
                        k.op("act", lambda e: e.activation(out=sgt[:], in_=p[:], func=AF.Sigmoid), reads=[p], writes=[sgt])
                        k.op("dve", lambda e: e.tensor_tensor(out=ym[:, cbk * 512:(cbk + 1) * 512], in0=hn[:, cbk * 512:(cbk + 1) * 512],
                                                              in1=sgt[:], op=ALU.mult), reads=[hn, sgt], writes=[ym])
                    to_bufA(ym, ti, nchunk=8, dstbuf=bufB, dstT=Bg[ti])
                k.barrier()
            if stop_after == 1:
                outer.close()
                k.finish()
                return k
            bufC = k.sb("bufC", [128, 8, NTOK], BF16, outer)
            k.dma("sp", bufC[:], yaT_d[:, base:base + NTOK].rearrange("(c p) t -> p c t", p=128), writes=[bufC])
            with ExitStack() as st:
                stg = Stager(k, st)
                CW = 256
                wgm = k.sb("wgm", [128, 16, CW], BF16, st)
                wga = k.sb("wga", [128, 16, CW], BF16, st)
                wpm = k.sb("wpm", [128, 8, CW], BF16, st)
                wpa = k.sb("wpa", [128, 8, CW], BF16, st)
                s1 = [k.sb("s1_%d" % i, [128, CW], F32, st) for i in range(2)]
                s2 = [k.sb("s2_%d" % i, [128, CW], F32, st) for i in range(2)]
                mg = [k.sb("mg%d" % i, [128, CW], BF16, st) for i in range(2)]
                wgv = wg_d.rearrange("(c p) n -> p c n", p=128)
                wpmv = wpm_d.rearrange("(c p) n -> p c n", p=128)
                wpav = wpa_d.rearrange("(c p) n -> p c n", p=128)
                nev = 0
                for cbk in range(D // CW):
                    c0 = cbk * CW
                    for hf in range(2):
                        stg.load(wgv[:, hf * 8:(hf + 1) * 8, 1024 + c0:1024 + c0 + CW], wgm[:, hf * 8:(hf + 1) * 8, :], wgm, shape3=(8, CW))
                        stg.load(wgv[:, hf * 8:(hf + 1) * 8, 3072 + c0:3072 + c0 + CW], wga[:, hf * 8:(hf + 1) * 8, :], wga, shape3=(8, CW))
                    stg.load(wpmv[:, :, c0:c0 + CW], wpm[:], wpm, shape3=(8, CW))
                    stg.load(wpav[:, :, c0:c0 + CW], wpa[:], wpa, shape3=(8, CW))
                    for ti in range(TPC):
                        pgm, pga, ppm, ppa = PB[0], PB[1], PB[2 + 2 * (nev % 2)], PB[3 + 2 * (nev % 2)]
                        a1, a2, m_ = s1[nev % 2], s2[nev % 2], mg[nev % 2]
                        nev += 1
                        for c in range(16):
                            k.op("pe", lambda e: e.matmul(pgm[:, 0:CW], lhsT=bufA[:, c, tsl(ti)], rhs=wgm[:, c, :], start=(c == 0), stop=(c == 15)),
                                 reads=[A[ti], wgm], writes=[pgm])
                        k.op("act", lambda e: e.activation(out=a1[:], in_=pgm[:, 0:CW], func=AF.Sigmoid), reads=[pgm], writes=[a1])
                        for c in range(16):
                            k.op("pe", lambda e: e.matmul(pga[:, 0:CW], lhsT=bufA[:, c, tsl(ti)], rhs=wga[:, c, :], start=(c == 0), stop=(c == 15)),
                                 reads=[A[ti], wga], writes=[pga])
                        k.op("act", lambda e: e.activation(out=a2[:], in_=pga[:, 0:CW], func=AF.Sigmoid), reads=[pga], writes=[a2])
                        for c in range(8):
                            k.op("pe", lambda e: e.matmul(ppm[:, 0:CW], lhsT=bufB[:, c, tsl(ti)], rhs=wpm[:, c, :], start=(c == 0), stop=(c == 7)),
                                 reads=[Bg[ti], wpm], writes=[ppm])
                        for c in range(8):
                            k.op("pe", lambda e: e.matmul(ppa[:, 0:CW], lhsT=bufC[:, c, tsl(ti)], rhs=wpa[:, c, :], start=(c == 0), stop=(c == 7)),
                                 reads=[bufC, wpa], writes=[ppa])
                        k.op("dve", lambda e: e.tensor_tensor(out=a1[:], in0=a1[:], in1=ppm[:, 0:CW], op=ALU.mult), reads=[a1, ppm], writes=[a1])
                        k.op("dve", lambda e: e.tensor_tensor(out=a2[:], in0=a2[:], in1=ppa[:, 0:CW], op=ALU.mult), reads=[a2, ppa], writes=[a2])
                        k.op("pool", lambda e: e.tensor_tensor(out=m_[:], in0=a1[:], in1=a2[:], op=ALU.add), reads=[a1, a2], writes=[m_])
                        k.dma("pool", mrg_d[tsl(ti), c0:c0 + CW], m_[:], reads=[m_])
                k.barrier()
            outer.close()
            if stop_after == 2:
                k.finish()
                return k
            with ExitStack() as st:
                stg = Stager(k, st)
                mt = [k.sb("mt%d" % i, [128, D], BF16, st) for i in range(2)]
                wo = [k.sb("wo%d" % i, [128, 16, 512], BF16, st) for i in range(2)]
                hb_ = [k.sb("hb%d" % i, [128, 512], F32, st) for i in range(2)]
                pr = [k.sb("pr%d" % i, [128, 512], F32, st) for i in range(2)]
                for ti in range(TPC):
                    m_ = mt[ti % 2]
                    k.dma("sp", m_[:], mrg_d[tsl(ti), :], writes=[m_])
                    to_bufA(m_, ti)
                wov = wout_d.rearrange("(c p) n -> p c n", p=128)
                nev = 0
                for cbk in range(4):
                    w_ = wo[cbk % 2]
                    for u in range(4):
                        stg.load(wov[:, 4 * u:4 * u + 4, cbk * 512:(cbk + 1) * 512], w_[:, 4 * u:4 * u + 4, :], w_, shape3=(4, 512))
                    for ti in range(TPC):
                        p = PB[nev % 6]
                        hx = hb_[nev % 2]
                        po = pr[nev % 2]
                        nev += 1
                        k.dma("sp", hx[:], h_d[rsl(ti), cbk * 512:(cbk + 1) * 512], writes=[hx])
                        for c in range(16):
                            k.op("pe", lambda e: e.matmul(p[:], lhsT=bufA[:, c, tsl(ti)], rhs=w_[:, c, :], start=(c == 0), stop=(c == 15)),
                                 reads=[A[ti], w_], writes=[p])
                        k.op("dve", lambda e: e.scalar_tensor_tensor(out=po[:], in0=hx[:], scalar=A_ALPHA, in1=p[:], op0=ALU.mult, op1=ALU.add),
                             reads=[hx, p], writes=[po])
                        k.dma("pool", pre1_d[tsl(ti), cbk * 512:(cbk + 1) * 512], po[:], reads=[po])
                k.barrier()
            if stop_after == 3:
                k.finish()
                return k
            with ExitStack() as st:
                g1 = k.sb("g1", [128, D], F32, st)
                b1 = k.sb("b1", [128, D], F32, st)
                wr = k.sb("wr", [128, 16, 36], F32, st)
                br = k.sb("br", [128, 36], F32, st)
                xt = [k.sb("x4_%d" % i, [128, D], F32, st) for i in range(2)]
                yt = [k.sb("y4_%d" % i, [128, D], F32, st) for i in range(2)]
                sq = k.sb("sq4", [128, D], BF16, st)
                s8 = k.sb("s8", [128, 8], F32, st)
                hTf = k.sb("hTf", [128, 16, 128], F32, st)
                lg = k.sb("lg", [128, 36], F32, st)
                em = k.sb("em", [128, 32], F32, st)
                r = k.sb("r", [128, 32], F32, st)
                m8 = k.sb("m8", [128, 8], F32, st)
                G1 = k.sb("G1", [128, 32], F32, st)
                G2 = k.sb("G2", [128, 32], F32, st)
                k.dma("sp", g1[:], g1_d, writes=[g1])
                k.dma("sp", b1[:], b1_d, writes=[b1])
                k.dma("sp", wr[:], wr_d.rearrange("(c p) n -> p c n", p=128), writes=[wr])
                k.dma("sp", br[:], br_d, writes=[br])
                for ti in range(TPC):
                    x, y = xt[ti % 2], yt[ti % 2]
                    k.dma("sp", x[:], pre1_d[tsl(ti), :], writes=[x])
                    _ln_rows(k, x, y, s8, sq, g1, b1)
                    k.dma("pool", h1_d[tsl(ti), :], y[:], reads=[y])
                    import os
                    P4 = int(os.environ.get("P4", "9"))
                    if P4 < 1:
                        continue
                    for g4 in range(4):
                        for j in range(4):
                            c = g4 * 4 + j
                            k.op("pe", lambda e: e.transpose(out=PT32[:, j, :], in_=y[:, c * 128:(c + 1) * 128], identity=idf[:]),
                                 reads=[y, idf], writes=[PT32])
                        k.op("dve", lambda e: e.tensor_copy(out=hTf[:, g4 * 4:(g4 + 1) * 4, :], in_=PT32[:]), reads=[PT32], writes=[hTf])
                        k.op("dve", lambda e: e.tensor_copy(out=bufA[:, g4 * 4:(g4 + 1) * 4, tsl(ti)], in_=PT32[:]), reads=[PT32], writes=[A[ti]])
                    if P4 < 2:
                        continue
                    p = PB[ti % 6]
                    for c in range(16):
                        k.op("pe", lambda e: e.matmul(p[:, 0:36], lhsT=hTf[:, c, :], rhs=wr[:, c, :], start=(c == 0), stop=(c == 15)),
                             reads=[hTf, wr], writes=[p])
                    k.pe_fence(idb, [p], PB[(ti + 3) % 6], 512, reps=8)
                    k.op("dve", lambda e: e.tensor_tensor(out=lg[:], in0=p[:, 0:36], in1=br[:], op=ALU.add), reads=[p, br], writes=[lg])
                    if P4 < 3:
                        continue
                    k.op("dve", lambda e: e.tensor_reduce(out=r[:, 0:1], in_=lg[:, 0:4], op=ALU.max, axis=AX.X), reads=[lg], writes=[r])
                    k.op("dve", lambda e: e.tensor_scalar(out=r[:, 1:2], in0=r[:, 0:1], scalar1=-1.0, scalar2=None, op0=ALU.mult), reads=[r], writes=[r])
                    k.op("dve", lambda e: e.tensor_scalar(out=r[:, 4:8], in0=lg[:, 0:4], scalar1=r[:, 0:1], scalar2=None, op0=ALU.is_equal),
                         reads=[lg, r], writes=[r])
                    k.op("act", lambda e: e.activation(out=r[:, 12:16], in_=lg[:, 0:4], func=AF.Exp, bias=r[:, 1:2], scale=1.0),
                         reads=[lg, r], writes=[r])
                    k.op("dve", lambda e: e.tensor_reduce(out=r[:, 2:3], in_=r[:, 12:16], op=ALU.add, axis=AX.X), reads=[r], writes=[r])
                    k.op("dve", lambda e: e.reciprocal(out=r[:, 3:4], in_=r[:, 2:3]), reads=[r], writes=[r])
                    k.op("dve", lambda e: e.tensor_scalar(out=r[:, 8:12], in0=r[:, 4:8], scalar1=-1.0, scalar2=1.0e30, op0=ALU.add, op1=ALU.mult),
                         reads=[r], writes=[r])
                    for gi in range(4):
                        k.op("dve", lambda e: e.tensor_scalar(out=em[:, gi * 8:(gi + 1) * 8], in0=lg[:, 4 + gi * 8:12 + gi * 8],
                                                              scalar1=r[:, 8 + gi:9 + gi], scalar2=None, op0=ALU.add), reads=[lg, r], writes=[em])
                    k.op("dve", lambda e: e.max(out=m8[:], in_=em[:]), reads=[em], writes=[m8])
                    k.op("dve", lambda e: e.tensor_tensor(out=r[:, 16:17], in0=m8[:, 1:2], in1=m8[:, 0:1], op=ALU.subtract), reads=[m8], writes=[r])
                    k.op("act", lambda e: e.activation(out=r[:, 17:18], in_=r[:, 16:17], func=AF.Exp), reads=[r], writes=[r])
                    k.op("dve", lambda e: e.tensor_scalar(out=r[:, 18:19], in0=r[:, 17:18], scalar1=1.0, scalar2=None, op0=ALU.add), reads=[r], writes=[r])
                    k.op("dve", lambda e: e.reciprocal(out=r[:, 19:20], in_=r[:, 18:19]), reads=[r], writes=[r])
                    k.op("dve", lambda e: e.tensor_tensor(out=r[:, 20:21], in0=r[:, 17:18], in1=r[:, 19:20], op=ALU.mult), reads=[r], writes=[r])
                    k.op("dve", lambda e: e.tensor_scalar(out=r[:, 21:23], in0=r[:, 19:21], scalar1=r[:, 3:4], scalar2=None, op0=ALU.mult),
                         reads=[r], writes=[r])
                    k.op("dve", lambda e: e.tensor_scalar(out=G1[:], in0=em[:], scalar1=m8[:, 0:1], scalar2=r[:, 21:22],
                                                          op0=ALU.is_equal, op1=ALU.mult), reads=[em, m8, r], writes=[G1])
                    k.op("dve", lambda e: e.tensor_scalar(out=G2[:], in0=em[:], scalar1=m8[:, 1:2], scalar2=r[:, 22:23],
                                                          op0=ALU.is_equal, op1=ALU.mult), reads=[em, m8, r], writes=[G2])
                    k.op("dve", lambda e: e.tensor_tensor(out=Gall[:, ti, :], in0=G1[:], in1=G2[:], op=ALU.add), reads=[G1, G2], writes=[Gall])
                k.barrier()
            if stop_after == 4:
                k.finish()
                return k
            with ExitStack() as st:
                stg = Stager(k, st)
                wgu = [k.sb("wgu%d" % i, [128, 16, 2, 128], BF16, st) for i in range(3)]
                wdn = [k.sb("wdn%d" % i, [128, 4, D], BF16, st) for i in range(2)]
                hbT = [k.sb("hbT%d" % i, [128, 4, 512], BF16, st) for i in range(2)]
                sgl = [k.sb("sgl%d" % i, [128, 512], F32, st) for i in range(2)]
                acc = k.sb("acc", [128, 4, D], F32, st)
                wguv = wgu_d.rearrange("e (c p) n -> e p c n", p=128)
                wdv = wd_d.rearrange("e (c p) n -> e p c n", p=128)
                npc = 0
                nex = 0
                nev = 0
                for t0 in range(0, TPC, 4):
                    nt_ = min(4, TPC - t0)
                    ntk = nt_ * 128
                    tok = slice(t0 * 128, t0 * 128 + ntk)
                    Ar = [A[t0 + i] for i in range(nt_)]
                    k.op("pool", lambda e: e.memset(acc[:], 0.0), writes=[acc])
                    for ex in range(32):
                        hb = hbT[nex % 2]
                        wd_ = wdn[nex % 2]
                        nex += 1
                        for fc in range(4):
                            stg.load(wdv[ex, :, fc, :], wd_[:, fc, :], wd_)
                        for fc in range(4):
                            wp = wgu[npc % 3]
                            npc += 1
                            stg.load(wguv[ex, :, :, fc * 128:(fc + 1) * 128], wp[:, :, 0, :], wp, shape3=(16, 128))
                            stg.load(wguv[ex, :, :, 512 + fc * 128:512 + (fc + 1) * 128], wp[:, :, 1, :], wp, shape3=(16, 128))
                            pg_, pu_ = PB[(nev % 2) * 2], PB[(nev % 2) * 2 + 1]
                            sg_ = sgl[nev % 2]
                            nev += 1
                            for c in range(16):
                                k.op("pe", lambda e: e.matmul(pg_[:, 0:ntk], lhsT=wp[:, c, 0, :], rhs=bufA[:, c, tok], start=(c == 0), stop=(c == 15)),
                                     reads=[wp] + Ar, writes=[pg_])
                            for c in range(16):
                                k.op("pe", lambda e: e.matmul(pu_[:, 0:ntk], lhsT=wp[:, c, 1, :], rhs=bufA[:, c, tok], start=(c == 0), stop=(c == 15)),
                                     reads=[wp] + Ar, writes=[pu_])
                            k.op("act", lambda e: e.activation(out=sg_[:, 0:ntk], in_=pg_[:, 0:ntk], func=AF.Silu), reads=[pg_], writes=[sg_])
                            k.op("dve", lambda e: e.tensor_tensor(out=hb[:, fc, 0:ntk], in0=sg_[:, 0:ntk], in1=pu_[:, 0:ntk], op=ALU.mult),
                                 reads=[sg_, pu_], writes=[hb])
                        for tl in range(nt_):
                            for db in range(4):
                                po = PB[4 + (nev % 2)]
                                nev += 1
                                for fc in range(4):
                                    k.op("pe", lambda e: e.matmul(po[:], lhsT=hb[:, fc, tl * 128:(tl + 1) * 128], rhs=wd_[:, fc, db * 512:(db + 1) * 512],
                                                                  start=(fc == 0), stop=(fc == 3)), reads=[hb, wd_], writes=[po])
                                k.op("dve", lambda e: e.scalar_tensor_tensor(out=acc[:, tl, db * 512:(db + 1) * 512], in0=po[:],
                                                                             scalar=Gall[:, t0 + tl, ex:ex + 1], in1=acc[:, tl, db * 512:(db + 1) * 512],
                                                                             op0=ALU.mult, op1=ALU.add), reads=[po, Gall, acc], writes=[acc])
                    for tl in range(nt_):
                        k.dma("pool", moe_d[tsl(t0 + tl), :], acc[:, tl, :], reads=[acc])
                k.barrier()

        if stop_after == 5:
            k.finish()
            return k
        with ExitStack() as st:
            g2 = k.sb("g2", [128, D], F32, st)
            b2 = k.sb("b2", [128, D], F32, st)
            xt = [k.sb("x6_%d" % i, [128, D], F32, st) for i in range(2)]
            mo_ = [k.sb("m6_%d" % i, [128, D], F32, st) for i in range(2)]
            yt = [k.sb("y6_%d" % i, [128, D], F32, st) for i in range(2)]
            yb = k.sb("yb6", [128, D], BF16, st)
            sq = k.sb("sq6", [128, D], BF16, st)
            s8 = k.sb("s86", [128, 8], F32, st)
            k.dma("sp", g2[:], g2_d, writes=[g2])
            k.dma("sp", b2[:], b2_d, writes=[b2])
            for ti in range(TPC):
                x, y = xt[ti % 2], yt[ti % 2]
                if first:
                    k.dma("sp", x[:], xin_d[rsl(ti), :], writes=[x])
                else:
                    m_ = mo_[ti % 2]
                    k.dma("sp", x[:], h1_d[tsl(ti), :], writes=[x])
                    k.dma("sp", m_[:], moe_d[tsl(ti), :], writes=[m_])
                    k.op("dve", lambda e: e.scalar_tensor_tensor(out=x[:], in0=x[:], scalar=A_ALPHA, in1=m_[:], op0=ALU.mult, op1=ALU.add),
                         reads=[x, m_], writes=[x])
                _ln_rows(k, x, y, s8, sq, g2, b2)
                k.dma("pool", hout_d[rsl(ti), :], y[:], reads=[y], is_output=True)
                if not last:
                    k.op("act", lambda e: e.copy(out=yb[:], in_=y[:]), reads=[y], writes=[yb])
                    to_bufA(yb, ti)
            k.barrier()
        with ExitStack() as st:
            if not last:
                stg = Stager(k, st)
                wb = [k.sb("wb%d" % i, [128, 16, 512], BF16, st) for i in range(2)]
                ot32 = [k.sb("ot32_%d" % i, [128, 512], F32, st) for i in range(2)]
                ot16 = [k.sb("ot16_%d" % i, [128, 512], BF16, st) for i in range(3)]
                wv = wn_d.rearrange("(c p) n -> p c n", p=128)
                c32 = c16 = nev = 0
                for bi, (cs, cw_, isb) in enumerate(PA_BLOCKS):
                    w_ = wb[bi % 2]
                    for u in range(4):
                        stg.load(wv[:, 4 * u:4 * u + 4, cs:cs + cw_], w_[:, 4 * u:4 * u + 4, 0:cw_], w_, shape3=(4, cw_))
                    for ti in range(TPC):
                        p = PB[nev % 6]
                        for c in range(16):
                            k.op("pe", lambda e: e.matmul(p[:, 0:cw_], lhsT=bufA[:, c, tsl(ti)], rhs=w_[:, c, 0:cw_], start=(c == 0), stop=(c == 15)),
                                 reads=[A[ti], w_], writes=[p])
                        if isb:
                            o = ot16[nev % 3]
                            dst = o16_d[rsl(ti), c16:c16 + cw_]
                        else:
                            o = ot32[nev % 2]
                            dst = o32_d[rsl(ti), c32:c32 + cw_]
                        if nev % 2 == 0:
                            k.op("act", lambda e: e.copy(out=o[:, 0:cw_], in_=p[:, 0:cw_]), reads=[p], writes=[o])
                        else:
                            k.op("dve", lambda e: e.tensor_copy(out=o[:, 0:cw_], in_=p[:, 0:cw_]), reads=[p], writes=[o])
                        k.dma("pool", dst, o[:, 0:cw_], reads=[o], is_output=True)
                        nev += 1
                    if isb:
                        c16 += cw_
                    else:
                        c32 += cw_
            k.barrier()
    k.finish()
    return k


def _rep(v, n=128):
    return np.ascontiguousarray(np.broadcast_to(np.asarray(v, np.float32), (n,) + np.shape(v)))


def pba_inputs(l, P, h, hm_tok, yaT, last):
    w_in = P["w_in"][l]
    wg = np.ascontiguousarray(np.concatenate([w_in[:, 2048:3072], w_in[:, 6736:10832]], axis=1))
    wr = np.ascontiguousarray(np.concatenate([P["w_group"][l], P["w_router"][l]], axis=1))
    br = _rep(np.concatenate([P["b_group"][l], P["b_router"][l]]))
    common = {"ident": np.eye(128, dtype=np.float32), "wg": wg, "mhg": _rep(P["mh_norm_g"][l]),
              "wpm": P["w_proj_m"][l], "wpa": P["w_proj_a"][l], "wout": P["w_out"][l],
              "g1": _rep(P["ln1_g"][l]), "b1": _rep(P["ln1_b"][l]), "wr": wr, "br": br,
              "wgu": P["w_gate_up"][l], "wd": P["w_down"][l],
              "g2": _rep(P["ln2_g"][l]), "b2": _rep(P["ln2_b"][l])}
    if not last:
        common["wn"] = P["w_in"][l + 1]
    maps = []
    for c in range(NCORES):
        m = dict(common)
        m["h"] = h[c * NTOK:(c + 1) * NTOK]
        m["hm"] = hm_tok[c * NTOK:(c + 1) * NTOK]
        m["yaT"] = np.ascontiguousarray(yaT[:, c * NTOK:(c + 1) * NTOK])
        maps.append(m)
    return maps


_PROGS = {}


def _prog(name, fn):
    if name not in _PROGS:
        _PROGS[name] = fn()
    return _PROGS[name]


PB_CORES = 4
PB_NVB = NCORES // PB_CORES


def _pba_maps(l, P, h, hm_tok, yaT, last):
    w_in = P["w_in"][l]
    wg = np.ascontiguousarray(np.concatenate([w_in[:, 2048:3072], w_in[:, 6736:10832]], axis=1))
    wr = np.ascontiguousarray(np.concatenate([P["w_group"][l], P["w_router"][l]], axis=1))
    br = _rep(np.concatenate([P["b_group"][l], P["b_router"][l]]))
    com = {"ident": np.eye(128, dtype=np.float32), "wg": wg, "mhg": _rep(P["mh_norm_g"][l]),
           "wpm": P["w_proj_m"][l], "wpa": P["w_proj_a"][l], "wout": P["w_out"][l],
           "g1": _rep(P["ln1_g"][l]), "b1": _rep(P["ln1_b"][l]), "wr": wr, "br": br,
           "wgu": P["w_gate_up"][l], "wd": P["w_down"][l],
           "g2": _rep(P["ln2_g"][l]), "b2": _rep(P["ln2_b"][l])}
    if not last:
        com["wn"] = P["w_in"][l + 1]
    n = PB_NVB * NTOK
    maps = []
    for c in range(PB_CORES):
        m = dict(com)
        m["h"] = h[c * n:(c + 1) * n]
        m["hm"] = hm_tok[c * n:(c + 1) * n]
        m["yaT"] = np.ascontiguousarray(yaT[:, c * n:(c + 1) * n])
        maps.append(m)
    return maps


def kernel(**inputs):
    import time
    t00 = time.time()
    P = {k_: np.asarray(v) for k_, v in inputs.items()}
    x = P["x"][0]
    xin = np.zeros((LP, D), np.float32)
    xin[:N_META] = P["meta_tokens"]
    xin[N_META:L] = x
    k0 = _prog("pa0", lambda: build_pba(first=True, NVB=PB_NVB))
    n_ = PB_NVB * NTOK
    res = run(k0, [{"ident": np.eye(128, dtype=np.float32), "xin": xin[c * n_:(c + 1) * n_], "g2": _rep(P["ln_emb_g"]),
                    "b2": _rep(P["ln_emb_b"]), "wn": P["w_in"][0]} for c in range(PB_CORES)])
    h, o16, o32 = (np.concatenate([r[nm] for r in res], axis=0) for nm in ("hout", "o16", "o32"))
    print("[kernel] pa0 done %.1fs" % (time.time() - t00), flush=True)
    for l in range(DEPTH):
        last = (l == DEPTH - 1)
        mq, mk, mv = o16[:, 0:512], o16[:, 512:1024], o16[:, 1024:2048]
        aq, ak, av = o16[:, 2048:3072], o16[:, 3072:4096], o16[:, 4096:5120]
        iq, ik = o16[:, 5120:5632], o16[:, 5632:5696]
        mi, mf, iw = o32[:, 0:4], o32[:, 4:8], np.ascontiguousarray(o32[:, 8:16])
        kml = _prog("ml", build_ml)
        rml = run(kml, ml_inputs(mq, mk, mv, mi, mf, P["conv_w"][l], P["conv_b"][l], P["b_igate"][l], P["b_fgate"][l]))
        hm_tok = np.ascontiguousarray(np.concatenate([r["hm"] for r in rml], axis=0).T)
        print("[kernel] L%d ml done %.1fs" % (l, time.time() - t00), flush=True)
        kat = _prog("at", build_at)
        rat = run(kat, at_inputs(np.ascontiguousarray(aq), np.ascontiguousarray(ak), np.ascontiguousarray(av),
                                 np.ascontiguousarray(iq), np.ascontiguousarray(ik), iw, P["rel_bias"]))
        ya = np.stack([r["ya"] for r in rat], axis=0)
        yaT = np.ascontiguousarray(ya.transpose(2, 3, 1, 0, 4)).reshape(1024, LP)
        print("[kernel] L%d at done %.1fs" % (l, time.time() - t00), flush=True)
        kpb = _prog("pbl" if last else "pbm",
                    (lambda: build_pba(last=True, NVB=PB_NVB)) if last else (lambda: build_pba(NVB=PB_NVB)))
        res = run(kpb, _pba_maps(l, P, h, hm_tok, yaT, last))
        h = np.concatenate([r["hout"] for r in res], axis=0)
        if not last:
            o16 = np.concatenate([r["o16"] for r in res], axis=0)
            o32 = np.concatenate([r["o32"] for r in res], axis=0)
        print("[kernel] L%d pba done %.1fs" % (l, time.time() - t00), flush=True)
    out = np.ascontiguousarray(h[N_META:L]).reshape(1, SEQ, D).astype(np.float32)
    return out
```

```python
import numpy as np
import ml_dtypes
from contextlib import ExitStack
import concourse.bass as bass
import concourse.mybir as mybir
from concourse.bass_utils import run_bass_kernel_spmd

F32 = mybir.dt.float32
BF16 = mybir.dt.bfloat16
ALU = mybir.AluOpType
AF = mybir.ActivationFunctionType
AX = mybir.AxisListType
NPBF = ml_dtypes.bfloat16

NCORES = 8
D = 2048
SEQ = 16384
N_META = 16
L = SEQ + N_META
TPC = 17
LP = NCORES * TPC * 128
NT = LP // 128
DEPTH = 4
IN_W = 10832
ALPHA = (2 * DEPTH) ** 0.25
LN_EPS = 1e-5


class T:
    __slots__ = ("t", "name", "w", "r")

    def __init__(self, t, name):
        self.t = t
        self.name = name
        self.w = None
        self.r = {}

    def __getitem__(self, idx):
        return self.t[idx]


class K:
    NDMA = 24
    QSLOTS = {"sp": (0, 12), "act": (12, 16), "pool": (16, 24)}

    def __init__(self):
        self.nc = bass.Bass("TRN2", target_bir_lowering=False)
        nc = self.nc
        self.es = ExitStack()
        self.eng = {"pe": nc.tensor, "act": nc.scalar, "dve": nc.vector,
                    "pool": nc.gpsimd, "sp": nc.sync}
        self.sem = {e: self.es.enter_context(nc.semaphore("s_" + e)) for e in self.eng}
        self.cnt = {e: 0 for e in self.eng}
        self.waited = {e: {} for e in self.eng}
        self.dsem = [self.es.enter_context(nc.semaphore("d%d" % i)) for i in range(self.NDMA)]
        self.dcnt = [0] * self.NDMA
        self.dnext = 0
        self.qnext = {}
        self.out_tickets = []
        self.npsum = 0

    def dram(self, name, shape, dt, kind):
        return self.nc.dram_tensor(name, list(shape), dt, kind=kind).ap()

    def sb(self, name, shape, dt, st=None):
        self.uid = getattr(self, "uid", 0) + 1
        t = (st or self.es).enter_context(self.nc.sbuf_tensor("%s_%d" % (name, self.uid), list(shape), dt))
        return T(t, name)

    def ps(self, name, shape, dt=F32, st=None):
        self.uid = getattr(self, "uid", 0) + 1
        t = (st or self.es).enter_context(self.nc.psum_tensor("%s_%d" % (name, self.uid), list(shape), dt))
        return T(t, name)

    def pe_fence(self, idb, tiles, z, ncols, reps=8):
        for i in range(reps):
            self.op("pe", lambda e: e.matmul(z[:, 0:ncols], lhsT=idb[:], rhs=self._fsrc[:, 0:ncols], start=True, stop=True),
                    reads=[idb, self._fsrc], writes=[z] + (list(tiles) if i == reps - 1 else []))

    def barrier(self):
        for e in self.eng:
            for e2 in self.eng:
                if self.cnt[e2] and not (e2 == e and e == "pe"):
                    self._wait(e, ("e", e2), self.cnt[e2])
            for i in range(self.NDMA):
                if self.dcnt[i]:
                    self._wait(e, ("d", i), self.dcnt[i])

    def _wait(self, e, key, val):
        if self.waited[e].get(key, 0) >= val:
            return
        if key[0] == "e":
            if key[1] == e and e == "pe":
                return
            sem = self.sem[key[1]]
        else:
            sem = self.dsem[key[1]]
        self.eng[e].wait_ge(sem, val)
        self.waited[e][key] = val

    def _deps(self, e, reads, writes):
        deps = {}
        for t in reads:
            if t.w is not None:
                k, v = t.w
                deps[k] = max(deps.get(k, 0), v)
        for t in writes:
            if t.w is not None:
                k, v = t.w
                deps[k] = max(deps.get(k, 0), v)
            for k, v in t.r.items():
                deps[k] = max(deps.get(k, 0), v)
        for k, v in deps.items():
            self._wait(e, k, v)

    def _mark(self, tk, reads, writes):
        k, v = tk
        for t in reads:
            t.r[k] = max(t.r.get(k, 0), v)
        for t in writes:
            t.w = tk
            t.r = {}

    def op(self, e, fn, reads=(), writes=()):
        self._deps(e, reads, writes)
        ins = fn(self.eng[e])
        self.cnt[e] += 1
        ins.then_inc(self.sem[e], 1)
        self._mark((("e", e), self.cnt[e]), reads, writes)

    def dma(self, e, out, in_, reads=(), writes=(), is_output=False):
        self._deps(e, reads, writes)
        lo, hi = self.QSLOTS[e]
        i = lo + self.qnext.get(e, 0) % (hi - lo)
        self.qnext[e] = self.qnext.get(e, 0) + 1
        ins = self.eng[e].dma_start(out=out, in_=in_)
        self.dcnt[i] += 16
        ins.then_inc(self.dsem[i], 16)
        tk = (("d", i), self.dcnt[i])
        self._mark(tk, reads, writes)
        if is_output:
            self.out_tickets.append(tk)

    def finish(self):
        fin = {}
        for k, v in self.out_tickets:
            fin[k] = max(fin.get(k, 0), v)
        for k, v in fin.items():
            self._wait("sp", k, v)
        for e in ("pe", "act", "dve", "pool"):
            if self.cnt[e]:
                self._wait("sp", ("e", e), self.cnt[e])
        self.es.close()
        return self.nc


def run(k, in_maps):
    res = run_bass_kernel_spmd(k.nc, in_maps, core_ids=list(range(len(in_maps))))
    return res.results


def _p1_blocks():
    blocks = []
    segs = [(0, 1024, False),
            (1024, 1024, True),
            (2048, 1024, False),
            (3072, 8, False),
            (3080, 3072 + 512 + 64, True),
            (6728, 8, False),
            (6736, 4096, False)]
    for s, w, b in segs:
        o = 0
        while o < w:
            ww = min(512, w - o)
            blocks.append((s + o, ww, b))
            o += ww
    return blocks


P1_BLOCKS = _p1_blocks()
N32 = sum(w for _, w, b in P1_BLOCKS if not b)
N16 = sum(w for _, w, b in P1_BLOCKS if b)


def build_p1(do_ln=True, do_proj=True):
    k = K()
    NTOK = TPC * 128
    xin = k.dram("xin", [NTOK, D], F32, "ExternalInput")
    g_rep = k.dram("g_rep", [128, D], F32, "ExternalInput")
    b_rep = k.dram("b_rep", [128, D], F32, "ExternalInput")
    ident_d = k.dram("ident", [128, 128], F32, "ExternalInput")
    h_out = k.dram("h", [NTOK, D], F32, "ExternalOutput")
    if do_proj:
        w = k.dram("w", [D, IN_W], F32, "ExternalInput")
        o32 = k.dram("o32", [NTOK, N32], F32, "ExternalOutput")
        o16 = k.dram("o16", [NTOK, N16], BF16, "ExternalOutput")

    gt = k.sb("gt", [128, D], F32)
    bt = k.sb("bt", [128, D], F32)
    idf = k.sb("idf", [128, 128], F32)
    idb = k.sb("idb", [128, 128], BF16)
    k.dma("sp", gt[:], g_rep, writes=[gt])
    k.dma("sp", bt[:], b_rep, writes=[bt])
    k.dma("sp", idf[:], ident_d, writes=[idf])
    k.op("dve", lambda e: e.tensor_copy(out=idb[:], in_=idf[:]), reads=[idf], writes=[idb])

    hT = k.sb("hT", [128, 16, NTOK], BF16) if do_proj else None
    xt = [k.sb("xt%d" % i, [128, D], F32) for i in range(2)]
    yt = [k.sb("yt%d" % i, [128, D], F32) for i in range(2)]
    yb = [k.sb("yb%d" % i, [128, D], BF16) for i in range(2)]
    sq = k.sb("sq", [128, D], BF16)
    st = [k.sb("st%d" % i, [128, 8], F32) for i in range(2)]
    pst = [k.ps("pst%d" % i, [128, 4, 128], BF16) for i in range(2)]
    pmm = [k.ps("pmm%d" % i, [128, 512], F32) for i in range(4)]

    for ti in range(TPC):
        x = xt[ti % 2]
        y = yt[ti % 2]
        s = st[ti % 2]
        k.dma("sp", x[:], xin[ti * 128:(ti + 1) * 128, :], writes=[x])
        if do_ln:
            k.op("dve", lambda e: e.tensor_reduce(out=s[:, 0:1], in_=x[:], op=ALU.add, axis=AX.X),
                 reads=[x], writes=[s])
            k.op("act", lambda e: e.activation(out=sq[:], in_=x[:], func=AF.Square, accum_out=s[:, 1:2]),
                 reads=[x], writes=[sq, s])
            k.op("dve", lambda e: e.tensor_scalar(out=s[:, 2:4], in0=s[:, 0:2], scalar1=1.0 / D, scalar2=None,
                                                  op0=ALU.mult), reads=[s], writes=[s])
            k.op("dve", lambda e: e.tensor_tensor(out=s[:, 4:5], in0=s[:, 2:3], in1=s[:, 2:3], op=ALU.mult),
                 reads=[s], writes=[s])
            k.op("dve", lambda e: e.tensor_tensor(out=s[:, 5:6], in0=s[:, 3:4], in1=s[:, 4:5], op=ALU.subtract),
                 reads=[s], writes=[s])
            k.op("dve", lambda e: e.tensor_scalar(out=s[:, 5:6], in0=s[:, 5:6], scalar1=LN_EPS, scalar2=None,
                                                  op0=ALU.add), reads=[s], writes=[s])
            k.op("act", lambda e: e.activation(out=s[:, 6:7], in_=s[:, 5:6], func=AF.Sqrt),
                 reads=[s], writes=[s])
            k.op("dve", lambda e: e.reciprocal(out=s[:, 7:8], in_=s[:, 6:7]), reads=[s], writes=[s])
            k.op("dve", lambda e: e.tensor_scalar(out=y[:], in0=x[:], scalar1=s[:, 2:3], scalar2=s[:, 7:8],
                                                  op0=ALU.subtract, op1=ALU.mult), reads=[x, s], writes=[y])
            k.op("pool", lambda e: e.tensor_tensor(out=y[:], in0=y[:], in1=gt[:], op=ALU.mult),
                 reads=[y, gt], writes=[y])
            k.op("dve", lambda e: e.tensor_tensor(out=y[:], in0=y[:], in1=bt[:], op=ALU.add),
                 reads=[y, bt], writes=[y])
        else:
            k.op("dve", lambda e: e.tensor_copy(out=y[:], in_=x[:]), reads=[x], writes=[y])
        k.dma("pool", h_out[ti * 128:(ti + 1) * 128, :], y[:], reads=[y], is_output=True)
        if do_proj:
            ybt = yb[ti % 2]
            k.op("act", lambda e: e.copy(out=ybt[:], in_=y[:]), reads=[y], writes=[ybt])
            for g4 in range(4):
                p = pst[g4 % 2]
                for j in range(4):
                    c = g4 * 4 + j
                    k.op("pe", lambda e: e.transpose(out=p[:, j, :], in_=ybt[:, c * 128:(c + 1) * 128],
                                                     identity=idb[:]), reads=[ybt, idb], writes=[p])
                k.op("dve", lambda e: e.tensor_copy(out=hT[:, g4 * 4:(g4 + 1) * 4, ti * 128:(ti + 1) * 128],
                                                    in_=p[:]), reads=[p], writes=[hT])

    if do_proj:
        wst = [k.sb("wst%d" % i, [128, 8, 512], F32) for i in range(2)]
        wbf = [k.sb("wbf%d" % i, [128, 16, 512], BF16) for i in range(2)]
        ot32 = [k.sb("ot32_%d" % i, [128, 512], F32) for i in range(3)]
        ot16 = [k.sb("ot16_%d" % i, [128, 512], BF16) for i in range(3)]
        wv = w.rearrange("(c p) n -> p c n", p=128)
        c32 = 0
        c16 = 0
        nev = 0
        for bi, (cs, cw, isb) in enumerate(P1_BLOCKS):
            wb = wbf[bi % 2]
            for half in range(2):
                ws = wst[half]
                k.dma("sp" if half == 0 else "act", ws[:, :, 0:cw],
                      wv[:, half * 8:(half + 1) * 8, cs:cs + cw], writes=[ws])
                k.op("pool" if half == 0 else "dve",
                     lambda e: e.tensor_copy(out=wb[:, half * 8:(half + 1) * 8, 0:cw], in_=ws[:, :, 0:cw]),
                     reads=[ws], writes=[wb])
            for ti in range(TPC):
                p = pmm[nev % 4]
                for c in range(16):
                    k.op("pe", lambda e: e.matmul(p[:, 0:cw], lhsT=hT[:, c, ti * 128:(ti + 1) * 128],
                                                  rhs=wb[:, c, 0:cw], start=(c == 0), stop=(c == 15)),
                         reads=[hT, wb], writes=[p])
                if isb:
                    o = ot16[nev % 3]
                    dst = o16[ti * 128:(ti + 1) * 128, c16:c16 + cw]
                else:
                    o = ot32[nev % 3]
                    dst = o32[ti * 128:(ti + 1) * 128, c32:c32 + cw]
                if nev % 2 == 0:
                    k.op("act", lambda e: e.copy(out=o[:, 0:cw], in_=p[:, 0:cw]), reads=[p], writes=[o])
                else:
                    k.op("dve", lambda e: e.tensor_copy(out=o[:, 0:cw], in_=p[:, 0:cw]), reads=[p], writes=[o])
                k.dma("pool", dst, o[:, 0:cw], reads=[o], is_output=True)
                nev += 1
            if isb:
                c16 += cw
            else:
                c32 += cw
    k.finish()
    return k


NSLOT = 17
KCH = 16
TOPK_SEL = 240
REPL = -3.0e38


def build_at(slots=None, heads=None):
    slots = list(range(NSLOT)) if slots is None else slots
    heads = list(range(8)) if heads is None else heads
    k = K()
    qT_d = k.dram("qT", [NSLOT, 128, 8, 128], BF16, "ExternalInput")
    iq_d = k.dram("iqT", [NSLOT, 64, 8, 128], BF16, "ExternalInput")
    iw_d = k.dram("iw", [NSLOT, 128, 8], F32, "ExternalInput")
    kT_d = k.dram("kT", [8, 128, LP], BF16, "ExternalInput")
    v_d = k.dram("v", [8, 128, NT, 128], BF16, "ExternalInput")
    ik_d = k.dram("ikT", [64, LP], BF16, "ExternalInput")
    fb_d = k.dram("fb", [8, 128, 9, 128], F32, "ExternalInput")
    ub_d = k.dram("ub", [128, 1024], F32, "ExternalInput")
    rb_d = k.dram("rb31", [128, 8], F32, "ExternalInput")
    id_d = k.dram("ident", [128, 128], F32, "ExternalInput")
    out_d = k.dram("ya", [NSLOT, 8, 128, 128], BF16, "ExternalOutput")

    sidx = k.sb("sidx", [128, LP], F32)
    maskT = k.sb("maskT", [128, NT, 128], BF16)
    ikT = k.sb("ikT_s", [64, LP], BF16)
    kch = [k.sb("kch%d" % i, [128, KCH * 128], BF16) for i in range(2)]
    vch = [k.sb("vch%d" % i, [128, KCH, 128], BF16) for i in range(2)]
    qs = k.sb("qs", [128, 8, 128], BF16)
    iqs = k.sb("iqs", [64, 8, 128], BF16)
    iws = k.sb("iws", [128, 8], F32)
    fe = k.sb("fe", [128, 8, 9, 128], BF16)
    fbs = k.sb("fbs", [128, 9, 128], F32)
    ubs = k.sb("ubs", [128, 1024], F32)
    rb = k.sb("rb", [128, 8], F32)
    nrb = k.sb("nrb", [128, 8], F32)
    idf = k.sb("idf", [128, 128], F32)
    onesb = k.sb("onesb", [128, 128], BF16)
    rl = [k.sb("rl%d" % i, [128, 512], F32) for i in range(2)]
    pex = [k.sb("pex%d" % i, [128, 4, 128], BF16) for i in range(2)]
    m8 = k.sb("m8", [128, 8], F32)
    rden = k.sb("rden", [128, 128], F32)
    ot = [k.sb("ot%d" % i, [128, 128], BF16) for i in range(2)]

    pidx = [k.ps("pidx%d" % i, [128, 512], F32) for i in range(2)]
    ptr = k.ps("ptr", [128, 4, 128], F32)
    pst = [k.ps("pst%d" % i, [128, 4, 128], F32) for i in range(2)]
    py = k.ps("py", [128, 128], F32)
    pden = k.ps("pden", [128, 128], F32)

    k.dma("sp", ikT[:], ik_d, writes=[ikT])
    k.dma("sp", ubs[:], ub_d, writes=[ubs])
    k.dma("sp", rb[:], rb_d, writes=[rb])
    k.dma("sp", idf[:], id_d, writes=[idf])
    k.op("dve", lambda e: e.tensor_scalar(out=nrb[:], in0=rb[:], scalar1=-1.0, scalar2=None, op0=ALU.mult),
         reads=[rb], writes=[nrb])
    k.op("dve", lambda e: e.memset(onesb[:], 1.0), writes=[onesb])
    for h in range(8):
        k.dma("sp", fbs[:], fb_d[h], writes=[fbs])
        k.op("act", lambda e: e.activation(out=fe[:, h, :, :], in_=fbs[:], func=AF.Exp, bias=nrb[:, h:h + 1], scale=1.0),
             reads=[fbs, nrb], writes=[fe])

    scale = 128 ** -0.5
    nch = 0
    nev = 0
    for kslot in slots:
        nk = 8 * kslot + 8
        nkeys = nk * 128
        k.dma("sp", qs[:], qT_d[kslot], writes=[qs])
        k.dma("sp", iqs[:], iq_d[kslot], writes=[iqs])
        k.dma("sp", iws[:], iw_d[kslot], writes=[iws])
        for blk in range(nk // 4):
            cs = blk * 512
            for h in range(8):
                p = pidx[nev % 2]
                r = rl[nev % 2]
                nev += 1
                k.op("pe", lambda e: e.matmul(p[:], lhsT=iqs[:, h, :], rhs=ikT[:, cs:cs + 512], start=True, stop=True),
                     reads=[iqs, ikT], writes=[p])
                k.op("act", lambda e: e.activation(out=r[:], in_=p[:], func=AF.Relu), reads=[p], writes=[r])
                if h == 0:
                    k.op("dve", lambda e: e.tensor_scalar(out=sidx[:, cs:cs + 512], in0=r[:], scalar1=iws[:, 0:1],
                                                          scalar2=None, op0=ALU.mult), reads=[r, iws], writes=[sidx])
                else:
                    k.op("dve", lambda e: e.scalar_tensor_tensor(out=sidx[:, cs:cs + 512], in0=r[:], scalar=iws[:, h:h + 1],
                                                                 in1=sidx[:, cs:cs + 512], op0=ALU.mult, op1=ALU.add),
                         reads=[r, iws, sidx], writes=[sidx])
        k.op("dve", lambda e: e.tensor_tensor(out=sidx[:, nkeys - 1024:nkeys], in0=sidx[:, nkeys - 1024:nkeys],
                                              in1=ubs[:], op=ALU.min), reads=[sidx, ubs], writes=[sidx])
        hi = min(nkeys, L)
        for rnd in range(TOPK_SEL // 8):
            k.op("dve", lambda e: e.max(out=m8[:], in_=sidx[:, 16:hi]), reads=[sidx], writes=[m8])
            k.op("dve", lambda e: e.match_replace(out=sidx[:, 16:hi], in_to_replace=m8[:], in_values=sidx[:, 16:hi],
                                                  imm_value=REPL), reads=[sidx, m8], writes=[sidx])
        k.op("dve", lambda e: e.tensor_scalar(out=sidx[:, 16:hi], in0=sidx[:, 16:hi], scalar1=-2.0e38, scalar2=None,
                                              op0=ALU.is_le), reads=[sidx], writes=[sidx])
        k.op("dve", lambda e: e.memset(sidx[:, 0:16], 1.0), writes=[sidx])
        if hi < nkeys:
            k.op("dve", lambda e: e.memset(sidx[:, hi:nkeys], 0.0), writes=[sidx])
        for g in range(nk // 4):
            for j in range(4):
                kt = g * 4 + j
                k.op("pe", lambda e: e.transpose(out=ptr[:, j, :], in_=sidx[:, kt * 128:(kt + 1) * 128], identity=idf[:]),
                     reads=[sidx, idf], writes=[ptr])
            if g % 2 == 0:
                k.op("act", lambda e: e.copy(out=maskT[:, g * 4:(g + 1) * 4, :], in_=ptr[:]), reads=[ptr], writes=[maskT])
            else:
                k.op("dve", lambda e: e.tensor_copy(out=maskT[:, g * 4:(g + 1) * 4, :], in_=ptr[:]), reads=[ptr], writes=[maskT])
        for h in heads:
            first = True
            for c0 in range(0, nk, KCH):
                cn = min(KCH, nk - c0)
                kc = kch[nch % 2]
                vc = vch[nch % 2]
                nch += 1
                k.dma("sp", kc[:, 0:cn * 128], kT_d[h, :, c0 * 128:(c0 + cn) * 128], writes=[kc])
                k.dma("act", vc[:, 0:cn, :], v_d[h, :, c0:c0 + cn, :], writes=[vc])
                for g in range(cn // 4):
                    ps = pst[nev % 2]
                    px = pex[nev % 2]
                    nev += 1
                    for j in range(4):
                        kt = g * 4 + j
                        k.op("pe", lambda e: e.matmul(ps[:, j, :], lhsT=kc[:, kt * 128:(kt + 1) * 128], rhs=qs[:, h, :],
                                                      start=True, stop=True), reads=[kc, qs], writes=[ps])
                    k.op("act", lambda e: e.activation(out=px[:], in_=ps[:], func=AF.Exp, bias=rb[:, h:h + 1], scale=scale),
                         reads=[ps, rb], writes=[px])
                    gt0 = c0 + g * 4
                    k.op("dve", lambda e: e.tensor_tensor(out=px[:], in0=px[:], in1=maskT[:, gt0:gt0 + 4, :], op=ALU.mult),
                         reads=[px, maskT], writes=[px])
                    for j in range(4):
                        m = gt0 + j - (nk - 9)
                        if m >= 0:
                            k.op("dve", lambda e: e.tensor_tensor(out=px[:, j, :], in0=px[:, j, :], in1=fe[:, h, m, :], op=ALU.mult),
                                 reads=[px, fe], writes=[px])
                    for j in range(4):
                        kt = g * 4 + j
                        last = (c0 + kt == nk - 1)
                        k.op("pe", lambda e: e.matmul(py[:], lhsT=vc[:, kt, :], rhs=px[:, j, :], start=first, stop=last),
                             reads=[vc, px], writes=[py])
                        k.op("pe", lambda e: e.matmul(pden[:], lhsT=onesb[:], rhs=px[:, j, :], start=first, stop=last),
                             reads=[onesb, px], writes=[pden])
                        first = False
            o = ot[h % 2]
            k.op("dve", lambda e: e.reciprocal(out=rden[:], in_=pden[:]), reads=[pden], writes=[rden])
            k.op("dve", lambda e: e.tensor_tensor(out=o[:], in0=py[:], in1=rden[:], op=ALU.mult), reads=[py, rden], writes=[o])
            k.dma("pool", out_d[kslot, h], o[:], reads=[o], is_output=True)
    k.finish()
    return k


def _t5_bucket(rel):
    rel = np.maximum(rel, 0)
    me = 16
    rf = np.maximum(rel, 1).astype(np.float32)
    large = me + (np.log(rf / me) / np.log(128 / me) * (32 - me)).astype(np.int32)
    large = np.minimum(large, 31)
    return np.where(rel < me, rel, large)


def at_inputs(aq, ak, av, iq, ik, iw, rel_bias):
    kT = np.ascontiguousarray(ak.reshape(LP, 8, 128).transpose(1, 2, 0))
    v = np.ascontiguousarray(av.reshape(NT, 128, 8, 128).transpose(2, 1, 0, 3))
    ikT = np.ascontiguousarray(ik.T)
    rb31 = np.ascontiguousarray(np.broadcast_to(rel_bias[31], (128, 8))).astype(np.float32)
    ident = np.eye(128, dtype=np.float32)
    aq4 = aq.reshape(NT, 128, 8, 128)
    iq4 = iq.reshape(NT, 128, 8, 64)
    iw3 = iw.reshape(NT, 128, 8)
    s_i = np.arange(128)[:, None]
    q_i = np.arange(128)[None, :]
    maps = []
    for c in range(NCORES):
        qT = np.ascontiguousarray(aq4[c::8].transpose(0, 3, 2, 1))
        iqT = np.ascontiguousarray(iq4[c::8].transpose(0, 3, 2, 1))
        iwc = np.ascontiguousarray(iw3[c::8])
        fb = np.empty((8, 128, 9, 128), np.float32)
        for m in range(9):
            r = c + 1 - m
            dist = r * 128 + q_i - s_i
            vis = (dist >= 0) & (r >= 0)
            idx = _t5_bucket(np.where(vis, dist, 0))
            vals = rel_bias[idx]
            fb[:, :, m, :] = np.where(vis[None], vals.transpose(2, 0, 1), np.float32(-1e30))
        ub = np.empty((128, 8, 128), np.float32)
        for mp in range(8):
            if mp < c:
                ub[:, mp, :] = 3e38
            elif mp > c:
                ub[:, mp, :] = -1e30
            else:
                ub[:, mp, :] = np.where(np.arange(128)[None, :] <= np.arange(128)[:, None], 3e38, -1e30)
        maps.append({"qT": qT, "iqT": iqT, "iw": iwc, "kT": kT, "v": v, "ikT": ikT, "fb": fb,
                     "ub": ub.reshape(128, 1024), "rb31": rb31, "ident": ident})
    return maps


MG = 8


def build_ml(ngroups=None):
    ngroups = NT // MG if ngroups is None else ngroups
    k = K()
    qsh_d = k.dram("qsh", [128, NT, 4 * 128], BF16, "ExternalInput")
    ksh_d = k.dram("ksh", [128, NT, 4 * 128], BF16, "ExternalInput")
    va_d = k.dram("vaug", [128, NT, 256], BF16, "ExternalInput")
    mif_d = k.dram("mif", [128, NT, 2], F32, "ExternalInput")
    cw_d = k.dram("cwg", [128, 2, 4, MG * 128], F32, "ExternalInput")
    cb_d = k.dram("cbg", [128, 2, MG * 128], F32, "ExternalInput")
    gb_d = k.dram("gb", [128, 2], F32, "ExternalInput")
    tri_d = k.dram("tri", [128, 128], F32, "ExternalInput")
    id_d = k.dram("ident", [128, 128], F32, "ExternalInput")
    out_d = k.dram("hm", [128, LP], BF16, "ExternalOutput")

    cw = k.sb("cw", [128, 2, 4, MG * 128], F32)
    cb = k.sb("cb", [128, 2, MG * 128], F32)
    gb = k.sb("gb_s", [128, 2], F32)
    ngb = k.sb("ngb", [128, 2], F32)
    tri = k.sb("tri_s", [128, 128], F32)
    idf = k.sb("idf", [128, 128], F32)
    idb = k.sb("idb", [128, 128], BF16)
    onesf = k.sb("onesf", [128, 128], F32)
    mif = k.sb("mif_s", [128, NT, 2], F32)
    lf = k.sb("lf", [128, NT], F32)
    tmpg = k.sb("tmpg", [128, NT], F32)
    qsc = k.sb("qsc", [128, NT], F32)
    ksc = k.sb("ksc", [128, NT], F32)
    gl = k.sb("gl", [128, NT], F32)
    qsh = [k.sb("qsh%d" % i, [128, MG, 512], BF16) for i in range(2)]
    ksh = [k.sb("ksh%d" % i, [128, MG, 512], BF16) for i in range(2)]
    va = [k.sb("va%d" % i, [128, MG, 256], BF16) for i in range(2)]
    accq = k.sb("accq", [128, MG * 128], F32)
    acck = k.sb("acck", [128, MG * 128], F32)
    tq = k.sb("tq", [128, MG * 128], F32)
    tk_ = k.sb("tk", [128, MG * 128], F32)
    qtm = k.sb("qtm", [128, MG, 128], BF16)
    ktm = k.sb("ktm", [128, MG, 128], BF16)
    qT = k.sb("qT_s", [128, MG, 128], BF16)
    kT = k.sb("kT_s", [128, MG, 128], BF16)
    AT = [k.sb("AT%d" % i, [128, 128], BF16) for i in range(2)]
    Cnf = k.sb("Cnf", [128, 256], F32)
    Cnb = k.sb("Cnb", [128, 256], BF16)
    dd = k.sb("dd", [128, 128], F32)
    ho = [k.sb("ho%d" % i, [128, MG * 128], BF16) for i in range(2)]

    pg = k.ps("pg", [128, 512], F32)
    ptq = k.ps("ptq", [128, 4, 128], BF16)
    ptk = k.ps("ptk", [128, 4, 128], BF16)
    pA = [k.ps("pA%d" % i, [128, 128], F32) for i in range(2)]
    pnum = k.ps("pnum", [128, 128], F32)
    pden = k.ps("pden", [128, 128], F32)
    pCn = k.ps("pCn", [128, 256], F32)

    for dst, src in ((cw, cw_d), (cb, cb_d), (gb, gb_d), (tri, tri_d), (idf, id_d), (mif, mif_d)):
        k.dma("sp", dst[:], src, writes=[dst])
    k.op("dve", lambda e: e.tensor_copy(out=idb[:], in_=idf[:]), reads=[idf], writes=[idb])
    k.op("dve", lambda e: e.memset(onesf[:], 1.0), writes=[onesf])
    k.op("dve", lambda e: e.tensor_scalar(out=ngb[:], in0=gb[:], scalar1=-1.0, scalar2=None, op0=ALU.mult),
         reads=[gb], writes=[ngb])
    k.op("dve", lambda e: e.memset(Cnf[:], 0.0), writes=[Cnf])
    k.op("dve", lambda e: e.memset(Cnb[:], 0.0), writes=[Cnb])
    k.op("act", lambda e: e.activation(out=tmpg[:], in_=mif[:, :, 1], func=AF.Exp, bias=ngb[:, 1:2], scale=-1.0),
         reads=[mif, ngb], writes=[tmpg])
    k.op("act", lambda e: e.activation(out=tmpg[:], in_=tmpg[:], func=AF.Ln, bias=1.0, scale=1.0),
         reads=[tmpg], writes=[tmpg])
    k.op("dve", lambda e: e.tensor_scalar(out=lf[:], in0=tmpg[:], scalar1=-1.0, scalar2=None, op0=ALU.mult),
         reads=[tmpg], writes=[lf])
    k.op("pe", lambda e: e.matmul(pg[:, 0:NT], lhsT=tri[:], rhs=lf[:], start=True, stop=True), reads=[tri, lf], writes=[pg])
    k.op("pe", lambda e: e.matmul(pg[:, 256:256 + NT], lhsT=onesf[:], rhs=lf[:], start=True, stop=True),
         reads=[onesf, lf], writes=[pg])
    import math
    fsrc = k.sb("fsrc", [128, 512], BF16)
    k.op("dve", lambda e: e.memset(fsrc[:], 0.0), writes=[fsrc])
    k._fsrc = fsrc
    k.pe_fence(idb, [pg], pCn, 256, reps=16)
    k.op("act", lambda e: e.activation(out=qsc[:], in_=pg[:, 0:NT], func=AF.Exp, bias=-0.5 * math.log(128.0), scale=1.0),
         reads=[pg], writes=[qsc])
    k.op("dve", lambda e: e.tensor_tensor(out=tmpg[:], in0=mif[:, :, 0], in1=pg[:, 0:NT], op=ALU.subtract),
         reads=[mif, pg], writes=[tmpg])
    k.op("act", lambda e: e.activation(out=ksc[:], in_=tmpg[:], func=AF.Exp, bias=gb[:, 0:1], scale=1.0),
         reads=[tmpg, gb], writes=[ksc])
    k.op("act", lambda e: e.activation(out=gl[:], in_=pg[:, 256:256 + NT], func=AF.Exp), reads=[pg], writes=[gl])

    for g in range(ngroups):
        c0 = g * MG
        qs_, ks_, va_ = qsh[g % 2], ksh[g % 2], va[g % 2]
        k.dma("sp", qs_[:], qsh_d[:, c0:c0 + MG, :], writes=[qs_])
        k.dma("act", ks_[:], ksh_d[:, c0:c0 + MG, :], writes=[ks_])
        k.dma("sp", va_[:], va_d[:, c0:c0 + MG, :], writes=[va_])
        for which, (src, acc, tmp, eng) in enumerate(((qs_, accq, tq, "dve"), (ks_, acck, tk_, "pool"))):
            a3 = acc[:].rearrange("p (g d) -> p g d", g=MG)
            t3 = tmp[:].rearrange("p (g d) -> p g d", g=MG)
            for j in range(4):
                w3 = cw[:, which, j, :].rearrange("p (g d) -> p g d", g=MG)
                dst = a3 if j == 0 else t3
                k.op(eng, lambda e: e.tensor_tensor(out=dst, in0=src[:, :, j * 128:(j + 1) * 128], in1=w3, op=ALU.mult),
                     reads=[src, cw], writes=[acc if j == 0 else tmp])
                if j > 0:
                    k.op(eng, lambda e: e.tensor_tensor(out=acc[:], in0=acc[:], in1=tmp[:], op=ALU.add),
                         reads=[acc, tmp], writes=[acc])
            k.op(eng, lambda e: e.tensor_tensor(out=acc[:], in0=acc[:], in1=cb[:, which, :], op=ALU.add),
                 reads=[acc, cb], writes=[acc])
            k.op("act", lambda e: e.activation(out=acc[:], in_=acc[:], func=AF.Silu), reads=[acc], writes=[acc])
        for ci in range(MG):
            c = c0 + ci
            k.op("dve", lambda e: e.tensor_scalar(out=qtm[:, ci, :], in0=accq[:, ci * 128:(ci + 1) * 128],
                                                  scalar1=qsc[:, c:c + 1], scalar2=None, op0=ALU.mult),
                 reads=[accq, qsc], writes=[qtm])
            k.op("dve", lambda e: e.tensor_scalar(out=ktm[:, ci, :], in0=acck[:, ci * 128:(ci + 1) * 128],
                                                  scalar1=ksc[:, c:c + 1], scalar2=None, op0=ALU.mult),
                 reads=[acck, ksc], writes=[ktm])
        for half in range(MG // 4):
            for j in range(4):
                ci = half * 4 + j
                k.op("pe", lambda e: e.transpose(out=ptq[:, j, :], in_=qtm[:, ci, :], identity=idb[:]),
                     reads=[qtm, idb], writes=[ptq])
                k.op("pe", lambda e: e.transpose(out=ptk[:, j, :], in_=ktm[:, ci, :], identity=idb[:]),
                     reads=[ktm, idb], writes=[ptk])
            k.op("act", lambda e: e.copy(out=qT[:, half * 4:(half + 1) * 4, :], in_=ptq[:]), reads=[ptq], writes=[qT])
            k.op("dve", lambda e: e.tensor_copy(out=kT[:, half * 4:(half + 1) * 4, :], in_=ptk[:]), reads=[ptk], writes=[kT])
        hob = ho[g % 2]
        for ci in range(MG):
            c = c0 + ci
            pa = pA[ci % 2]
            at = AT[ci % 2]
            k.op("pe", lambda e: e.matmul(pa[:], lhsT=kT[:, ci, :], rhs=qT[:, ci, :], start=True, stop=True),
                 reads=[kT, qT], writes=[pa])
            k.op("dve", lambda e: e.tensor_tensor(out=at[:], in0=pa[:], in1=tri[:], op=ALU.mult),
                 reads=[pa, tri], writes=[at])
            k.op("pe", lambda e: e.matmul(pnum[:], lhsT=va_[:, ci, 0:128], rhs=at[:], start=True, stop=False),
                 reads=[va_, at], writes=[pnum])
            k.op("pe", lambda e: e.matmul(pnum[:], lhsT=Cnb[:, 0:128], rhs=qT[:, ci, :], start=False, stop=True),
                 reads=[Cnb, qT], writes=[pnum])
            k.op("pe", lambda e: e.matmul(pden[:], lhsT=va_[:, ci, 128:256], rhs=at[:], start=True, stop=False),
                 reads=[va_, at], writes=[pden])
            k.op("pe", lambda e: e.matmul(pden[:], lhsT=Cnb[:, 128:256], rhs=qT[:, ci, :], start=False, stop=True),
                 reads=[Cnb, qT], writes=[pden])
            k.op("act", lambda e: e.activation(out=dd[:], in_=pden[:], func=AF.Abs), reads=[pden], writes=[dd])
            k.op("dve", lambda e: e.tensor_scalar(out=dd[:], in0=dd[:], scalar1=1.0, scalar2=None, op0=ALU.max),
                 reads=[dd], writes=[dd])
            k.op("dve", lambda e: e.reciprocal(out=dd[:], in_=dd[:]), reads=[dd], writes=[dd])
            k.op("dve", lambda e: e.tensor_tensor(out=hob[:, ci * 128:(ci + 1) * 128], in0=pnum[:], in1=dd[:], op=ALU.mult),
                 reads=[pnum, dd], writes=[hob])
            k.op("pe", lambda e: e.matmul(pCn[:], lhsT=ktm[:, ci, :], rhs=va_[:, ci, :], start=True, stop=True),
                 reads=[ktm, va_], writes=[pCn])
            k.op("dve", lambda e: e.tensor_tensor(out=Cnf[:], in0=Cnf[:], in1=pCn[:], op=ALU.add),
                 reads=[Cnf, pCn], writes=[Cnf])
            k.op("dve", lambda e: e.tensor_scalar(out=Cnf[:], in0=Cnf[:], scalar1=gl[:, c:c + 1], scalar2=None, op0=ALU.mult),
                 reads=[Cnf, gl], writes=[Cnf])
            k.op("act", lambda e: e.copy(out=Cnb[:], in_=Cnf[:]), reads=[Cnf], writes=[Cnb])
        k.dma("pool", out_d[:, c0 * 128:(c0 + MG) * 128], hob[:], reads=[hob], is_output=True)
    k.finish()
    return k


def ml_inputs(mq, mk, mv, mi, mf, conv_w, conv_b, b_ig, b_fg):
    tri = np.triu(np.ones((128, 128), np.float32))
    ident = np.eye(128, dtype=np.float32)

    def shifted(u):
        out = np.zeros((LP, 4, 128), u.dtype)
        for j in range(4):
            sh = 3 - j
            if sh == 0:
                out[:, j] = u
            else:
                out[sh:, j] = u[:-sh]
        return np.ascontiguousarray(out.reshape(NT, 128, 512).transpose(1, 0, 2))

    maps = []
    for c in range(NCORES):
        hd, vh = c // 2, c % 2
        qsh = shifted(mq[:, hd * 128:(hd + 1) * 128])
        ksh = shifted(mk[:, hd * 128:(hd + 1) * 128])
        vaug = np.ones((LP, 256), mv.dtype)
        vaug[:, :128] = mv[:, hd * 256 + vh * 128: hd * 256 + (vh + 1) * 128]
        vaug = np.ascontiguousarray(vaug.reshape(NT, 128, 256).transpose(1, 0, 2))
        mif = np.stack([mi[:, hd], mf[:, hd]], -1).astype(np.float32)
        mif = np.ascontiguousarray(mif.reshape(NT, 128, 2).transpose(1, 0, 2))
        cwq = conv_w[:, hd * 128:(hd + 1) * 128]
        cwk = conv_w[:, 512 + hd * 128:512 + (hd + 1) * 128]
        cw = np.stack([cwq, cwk], 0)
        cwg = np.ascontiguousarray(np.broadcast_to(cw[None, :, :, None, :], (128, 2, 4, MG, 128))).reshape(128, 2, 4, MG * 128)
        cbq = conv_b[hd * 128:(hd + 1) * 128]
        cbk = conv_b[512 + hd * 128:512 + (hd + 1) * 128]
        cb2 = np.stack([cbq, cbk], 0)
        cbg = np.ascontiguousarray(np.broadcast_to(cb2[None, :, None, :], (128, 2, MG, 128))).reshape(128, 2, MG * 128)
        gb = np.ascontiguousarray(np.broadcast_to(np.array([b_ig[hd], b_fg[hd]], np.float32), (128, 2)))
        maps.append({"qsh": qsh, "ksh": ksh, "vaug": vaug, "mif": mif, "cwg": cwg.astype(np.float32),
                     "cbg": cbg.astype(np.float32), "gb": gb, "tri": tri, "ident": ident})
    return maps


def _pa_blocks():
    blocks = []
    segs = [(0, 2048, True),
            (3072, 8, False),
            (3080, 3648, True),
            (6728, 8, False)]
    for s, w, b in segs:
        o = 0
        while o < w:
            ww = min(512, w - o)
            blocks.append((s + o, ww, b))
            o += ww
    return blocks


PA_BLOCKS = _pa_blocks()
PA16 = sum(w for _, w, b in PA_BLOCKS if b)
PA32 = sum(w for _, w, b in PA_BLOCKS if not b)
NTOK = TPC * 128


class Stager:
    def __init__(self, k, st, n=4):
        self.k = k
        self.bufs = [k.sb("stg%d" % i, [128, 2048], F32, st) for i in range(n)]
        self.i = 0
        self.ce = 0

    def load(self, src_view, dst_ap, dstT, shape3=None, nelem=2048):
        k = self.k
        b = self.bufs[self.i % len(self.bufs)]
        self.i += 1
        if shape3 is None:
            v = b[:, 0:nelem]
        else:
            c, n = shape3
            v = b[:, 0:c * n].rearrange("p (c n) -> p c n", c=c)
        k.dma("sp", v, src_view, writes=[b])
        eng = ("pool", "dve", "act")[self.ce % 3]
        self.ce += 1
        if eng == "act":
            k.op("act", lambda e: e.copy(out=dst_ap, in_=v), reads=[b], writes=[dstT])
        else:
            k.op(eng, lambda e: e.tensor_copy(out=dst_ap, in_=v), reads=[b], writes=[dstT])


def _ln_rows(k, x, y, s, sq, gt, bt, ncols=D):
    k.op("dve", lambda e: e.tensor_reduce(out=s[:, 0:1], in_=x[:], op=ALU.add, axis=AX.X), reads=[x], writes=[s])
    k.op("act", lambda e: e.activation(out=sq[:], in_=x[:], func=AF.Square, accum_out=s[:, 1:2]), reads=[x], writes=[sq, s])
    k.op("dve", lambda e: e.tensor_scalar(out=s[:, 2:4], in0=s[:, 0:2], scalar1=1.0 / ncols, scalar2=None, op0=ALU.mult),
         reads=[s], writes=[s])
    k.op("dve", lambda e: e.tensor_tensor(out=s[:, 4:5], in0=s[:, 2:3], in1=s[:, 2:3], op=ALU.mult), reads=[s], writes=[s])
    k.op("dve", lambda e: e.tensor_tensor(out=s[:, 5:6], in0=s[:, 3:4], in1=s[:, 4:5], op=ALU.subtract), reads=[s], writes=[s])
    k.op("dve", lambda e: e.tensor_scalar(out=s[:, 5:6], in0=s[:, 5:6], scalar1=LN_EPS, scalar2=None, op0=ALU.add),
         reads=[s], writes=[s])
    k.op("act", lambda e: e.activation(out=s[:, 6:7], in_=s[:, 5:6], func=AF.Sqrt), reads=[s], writes=[s])
    k.op("dve", lambda e: e.reciprocal(out=s[:, 7:8], in_=s[:, 6:7]), reads=[s], writes=[s])
    k.op("dve", lambda e: e.tensor_scalar(out=y[:], in0=x[:], scalar1=s[:, 2:3], scalar2=s[:, 7:8],
                                          op0=ALU.subtract, op1=ALU.mult), reads=[x, s], writes=[y])
    k.op("pool", lambda e: e.tensor_tensor(out=y[:], in0=y[:], in1=gt[:], op=ALU.mult), reads=[y, gt], writes=[y])
    k.op("dve", lambda e: e.tensor_tensor(out=y[:], in0=y[:], in1=bt[:], op=ALU.add), reads=[y, bt], writes=[y])


class _Stop(Exception):
    pass


def build_pba(last=False, first=False, stop_after=99, NVB=1):
    k = K()
    A_ALPHA = float(ALPHA)
    ident_d = k.dram("ident", [128, 128], F32, "ExternalInput")
    NALL = NVB * NTOK
    hout_d = k.dram("hout", [NALL, D], F32, "ExternalOutput")
    if first:
        xin_d = k.dram("xin", [NALL, D], F32, "ExternalInput")
    else:
        h_d = k.dram("h", [NALL, D], F32, "ExternalInput")
        hm_d = k.dram("hm", [NALL, 1024], BF16, "ExternalInput")
        yaT_d = k.dram("yaT", [1024, NALL], BF16, "ExternalInput")
        wg_d = k.dram("wg", [D, 5120], F32, "ExternalInput")
        mhg_d = k.dram("mhg", [128, 1024], F32, "ExternalInput")
        wpm_d = k.dram("wpm", [1024, D], F32, "ExternalInput")
        wpa_d = k.dram("wpa", [1024, D], F32, "ExternalInput")
        wout_d = k.dram("wout", [D, D], F32, "ExternalInput")
        g1_d = k.dram("g1", [128, D], F32, "ExternalInput")
        b1_d = k.dram("b1", [128, D], F32, "ExternalInput")
        wr_d = k.dram("wr", [D, 36], F32, "ExternalInput")
        br_d = k.dram("br", [128, 36], F32, "ExternalInput")
        wgu_d = k.dram("wgu", [32, D, 1024], F32, "ExternalInput")
        wd_d = k.dram("wd", [32, 512, D], F32, "ExternalInput")
        mrg_d = k.dram("s_mrg", [NTOK, D], BF16, "Internal")
        pre1_d = k.dram("s_pre1", [NTOK, D], F32, "Internal")
        h1_d = k.dram("s_h1", [NTOK, D], F32, "Internal")
        moe_d = k.dram("s_moe", [NTOK, D], F32, "Internal")
    g2_d = k.dram("g2", [128, D], F32, "ExternalInput")
    b2_d = k.dram("b2", [128, D], F32, "ExternalInput")
    if not last:
        wn_d = k.dram("wn", [D, IN_W], F32, "ExternalInput")
        o16_d = k.dram("o16", [NALL, PA16], BF16, "ExternalOutput")
        o32_d = k.dram("o32", [NALL, PA32], F32, "ExternalOutput")

    bufA = k.sb("bufA", [128, 16, NTOK], BF16)
    A = [T(bufA.t, "A%d" % i) for i in range(TPC)]
    idf = k.sb("idf", [128, 128], F32)
    idb = k.sb("idb", [128, 128], BF16)
    Gall = k.sb("Gall", [128, TPC, 32], F32)
    fsrc = k.sb("fsrc", [128, 512], BF16)
    k.op("dve", lambda e: e.memset(fsrc[:], 0.0), writes=[fsrc])
    k._fsrc = fsrc
    k.dma("sp", idf[:], ident_d, writes=[idf])
    k.op("dve", lambda e: e.tensor_copy(out=idb[:], in_=idf[:]), reads=[idf], writes=[idb])
    PB = [k.ps("pb%d" % i, [128, 512], F32) for i in range(6)]
    PT32 = k.ps("pt32", [128, 4, 128], F32)
    PT16 = k.ps("pt16", [128, 4, 128], BF16)

    for vb in range(NVB):
        base = vb * NTOK

        def tsl(ti):
            return slice(ti * 128, (ti + 1) * 128)

        def rsl(ti):
            return slice(base + ti * 128, base + (ti + 1) * 128)

        def to_bufA(src_bf, ti, nchunk=16, dstbuf=None, dstT=None):
            dstbuf = bufA if dstbuf is None else dstbuf
            dstT = A[ti] if dstT is None else dstT
            for g4 in range(nchunk // 4):
                for j in range(4):
                    c = g4 * 4 + j
                    k.op("pe", lambda e: e.transpose(out=PT16[:, j, :], in_=src_bf[:, c * 128:(c + 1) * 128], identity=idb[:]),
                         reads=[src_bf, idb], writes=[PT16])
                k.op("dve", lambda e: e.tensor_copy(out=dstbuf[:, g4 * 4:(g4 + 1) * 4, tsl(ti)], in_=PT16[:]),
                     reads=[PT16], writes=[dstT])

        if not first:
            outer = ExitStack()
            bufB = k.sb("bufB", [128, 8, NTOK], BF16, outer)
            Bg = [T(bufB.t, "B%d" % i) for i in range(TPC)]
            with ExitStack() as st:
                stg = Stager(k, st, n=2)
                wmo = k.sb("wmo", [128, 16, 1024], BF16, st)
                mhg = k.sb("mhg", [128, 1024], F32, st)
                xt = [k.sb("xt%d" % i, [128, D], F32, st) for i in range(2)]
                xb = k.sb("xb", [128, D], BF16, st)
                hmt = [k.sb("hmt%d" % i, [128, 1024], BF16, st) for i in range(2)]
                hn = k.sb("hn", [128, 1024], F32, st)
                sq = k.sb("sq", [128, 1024], F32, st)
                sg = [k.sb("sg%d" % i, [128, 512], F32, st) for i in range(2)]
                ym = k.sb("ym", [128, 1024], BF16, st)
                s4 = k.sb("s4", [128, 32], F32, st)
                k.dma("sp", mhg[:], mhg_d, writes=[mhg])
                wv = wg_d.rearrange("(c p) n -> p c n", p=128)
                for u in range(8):
                    stg.load(wv[:, 2 * u:2 * u + 2, 0:1024], wmo[:, 2 * u:2 * u + 2, :], wmo, shape3=(2, 1024))
                nev = 0
                for ti in range(TPC):
                    x = xt[ti % 2]
                    hm = hmt[ti % 2]
                    k.dma("sp", x[:], h_d[rsl(ti), :], writes=[x])
                    k.dma("sp", hm[:], hm_d[rsl(ti), :], writes=[hm])
                    k.op("act", lambda e: e.copy(out=xb[:], in_=x[:]), reads=[x], writes=[xb])
                    to_bufA(xb, ti)
                    hm3 = hm[:].rearrange("p (h v) -> p h v", h=4)
                    sq3 = sq[:].rearrange("p (h v) -> p h v", h=4)
                    k.op("dve", lambda e: e.tensor_reduce(out=s4[:, 0:4], in_=hm3, op=ALU.add, axis=AX.X), reads=[hm], writes=[s4])
                    k.op("pool", lambda e: e.tensor_tensor(out=sq[:], in0=hm[:], in1=hm[:], op=ALU.mult), reads=[hm], writes=[sq])
                    k.op("dve", lambda e: e.tensor_reduce(out=s4[:, 4:8], in_=sq3, op=ALU.add, axis=AX.X), reads=[sq], writes=[s4])
                    k.op("dve", lambda e: e.tensor_scalar(out=s4[:, 8:16], in0=s4[:, 0:8], scalar1=1.0 / 256, scalar2=None, op0=ALU.mult),
                         reads=[s4], writes=[s4])
                    k.op("dve", lambda e: e.tensor_tensor(out=s4[:, 16:20], in0=s4[:, 8:12], in1=s4[:, 8:12], op=ALU.mult),
                         reads=[s4], writes=[s4])
                    k.op("dve", lambda e: e.tensor_tensor(out=s4[:, 20:24], in0=s4[:, 12:16], in1=s4[:, 16:20], op=ALU.subtract),
                         reads=[s4], writes=[s4])
                    k.op("dve", lambda e: e.tensor_scalar(out=s4[:, 20:24], in0=s4[:, 20:24], scalar1=LN_EPS, scalar2=None, op0=ALU.add),
                         reads=[s4], writes=[s4])
                    k.op("act", lambda e: e.activation(out=s4[:, 24:28], in_=s4[:, 20:24], func=AF.Sqrt), reads=[s4], writes=[s4])
                    k.op("dve", lambda e: e.reciprocal(out=s4[:, 28:32], in_=s4[:, 24:28]), reads=[s4], writes=[s4])
                    for hd in range(4):
                        k.op("dve", lambda e: e.tensor_scalar(out=hn[:, hd * 256:(hd + 1) * 256], in0=hm[:, hd * 256:(hd + 1) * 256],
                                                              scalar1=s4[:, 8 + hd:9 + hd], scalar2=s4[:, 28 + hd:29 + hd],
                                                              op0=ALU.subtract, op1=ALU.mult), reads=[hm, s4], writes=[hn])
                    k.op("pool", lambda e: e.tensor_tensor(out=hn[:], in0=hn[:], in1=mhg[:], op=ALU.mult), reads=[hn, mhg], writes=[hn])
                    for cbk in range(2):
                        p = PB[nev % 6]
                        sgt = sg[nev % 2]
                        nev += 1
                        for c in range(16):
                            k.op("pe", lambda e: e.matmul(p[:], lhsT=bufA[:, c, tsl(ti)], rhs=wmo[:, c, cbk * 512:(cbk + 1) * 512],
                                                          start=(c == 0), stop=(c == 15)), reads=[A[ti], wmo], writes=[p])
                        k.op("act", lambda e: e.activation(out=sgt[:], in_=p[:], func=AF.Sigmoid), reads=[p], writes=[sgt])
                        k.op("dve", lambda e: e.tensor_tensor(out=ym[:, cbk * 512:(cbk + 1) * 512], in0=hn[:, cbk * 512:(cbk + 1) * 512],
                                                              in1=sgt[:], op=ALU.mult), reads=[hn, sgt], writes=[ym])
                    to_bufA(ym, ti, nchunk=8, dstbuf=bufB, dstT=Bg[ti])
                k.barrier()
            if stop_after == 1:
                outer.close()
                k.finish()
                return k
            bufC = k.sb("bufC", [128, 8, NTOK], BF16, outer)
            k.dma("sp", bufC[:], yaT_d[:, base:base + NTOK].rearrange("(c p) t -> p c t", p=128), writes=[bufC])
            with ExitStack() as st:
                stg = Stager(k, st)
                CW = 256
                wgm = k.sb("wgm", [128, 16, CW], BF16, st)
                wga = k.sb("wga", [128, 16, CW], BF16, st)
                wpm = k.sb("wpm", [128, 8, CW], BF16, st)
                wpa = k.sb("wpa", [128, 8, CW], BF16, st)
                s1 = [k.sb("s1_%d" % i, [128, CW], F32, st) for i in range(2)]
                s2 = [k.sb("s2_%d" % i, [128, CW], F32, st) for i in range(2)]
                mg = [k.sb("mg%d" % i, [128, CW], BF16, st) for i in range(2)]
                wgv = wg_d.rearrange("(c p) n -> p c n", p=128)
                wpmv = wpm_d.rearrange("(c p) n -> p c n", p=128)
                wpav = wpa_d.rearrange("(c p) n -> p c n", p=128)
                nev = 0
                for cbk in range(D // CW):
                    c0 = cbk * CW
                    for hf in range(2):
                        stg.load(wgv[:, hf * 8:(hf + 1) * 8, 1024 + c0:1024 + c0 + CW], wgm[:, hf * 8:(hf + 1) * 8, :], wgm, shape3=(8, CW))
                        stg.load(wgv[:, hf * 8:(hf + 1) * 8, 3072 + c0:3072 + c0 + CW], wga[:, hf * 8:(hf + 1) * 8, :], wga, shape3=(8, CW))
                    stg.load(wpmv[:, :, c0:c0 + CW], wpm[:], wpm, shape3=(8, CW))
                    stg.load(wpav[:, :, c0:c0 + CW], wpa[:], wpa, shape3=(8, CW))
                    for ti in range(TPC):
                        pgm, pga, ppm, ppa = PB[0], PB[1], PB[2 + 2 * (nev % 2)], PB[3 + 2 * (nev % 2)]
                        a1, a2, m_ = s1[nev % 2], s2[nev % 2], mg[nev % 2]
                        nev += 1
                        for c in range(16):
                            k.op("pe", lambda e: e.matmul(pgm[:, 0:CW], lhsT=bufA[:, c, tsl(ti)], rhs=wgm[:, c, :], start=(c == 0), stop=(c == 15)),
                                 reads=[A[ti], wgm], writes=[pgm])
                        k.op("act", lambda e: e.activation(out=a1[:], in_=pgm[:, 0:CW], func=AF.Sigmoid), reads=[pgm], writes=[a1])
                        for c in range(16):
                            k.op("pe", lambda e: e.matmul(pga[:, 0:CW], lhsT=bufA[:, c, tsl(ti)], rhs=wga[:, c, :], start=(c == 0), stop=(c == 15)),
                                 reads=[A[ti], wga], writes=[pga])
                        k.op("act", lambda e: e.activation(out=a2[:], in_=pga[:, 0:CW], func=AF.Sigmoid), reads=[pga], writes=[a2])
                        for c in range(8):
                            k.op("pe", lambda e: e.matmul(ppm[:, 0:CW], lhsT=bufB[:, c, tsl(ti)], rhs=wpm[:, c, :], start=(c == 0), stop=(c == 7)),
                                 reads=[Bg[ti], wpm], writes=[ppm])
                        for c in range(8):
                            k.op("pe", lambda e: e.matmul(ppa[:, 0:CW], lhsT=bufC[:, c, tsl(ti)], rhs=wpa[:, c, :], start=(c == 0), stop=(c == 7)),
                                 reads=[bufC, wpa], writes=[ppa])
                        k.op("dve", lambda e: e.tensor_tensor(out=a1[:], in0=a1[:], in1=ppm[:, 0:CW], op=ALU.mult), reads=[a1, ppm], writes=[a1])
                        k.op("dve", lambda e: e.tensor_tensor(out=a2[:], in0=a2[:], in1=ppa[:, 0:CW], op=ALU.mult), reads=[a2, ppa], writes=[a2])
                        k.op("pool", lambda e: e.tensor_tensor(out=m_[:], in0=a1[:], in1=a2[:], op=ALU.add), reads=[a1, a2], writes=[m_])
                        k.dma("pool", mrg_d[tsl(ti), c0:c0 + CW], m_[:], reads=[m_])
                k.barrier()
            outer.close()
            if stop_after == 2:
                k.finish()
                return k
            with ExitStack() as st:
                stg = Stager(k, st)
                mt = [k.sb("mt%d" % i, [128, D], BF16, st) for i in range(2)]
                wo = [k.sb("wo%d" % i, [128, 16, 512], BF16, st) for i in range(2)]
                hb_ = [k.sb("hb%d" % i, [128, 512], F32, st) for i in range(2)]
                pr = [k.sb("pr%d" % i, [128, 512], F32, st) for i in range(2)]
                for ti in range(TPC):
                    m_ = mt[ti % 2]
                    k.dma("sp", m_[:], mrg_d[tsl(ti), :], writes=[m_])
                    to_bufA(m_, ti)
                wov = wout_d.rearrange("(c p) n -> p c n", p=128)
                nev = 0
                for cbk in range(4):
                    w_ = wo[cbk % 2]
                    for u in range(4):
                        stg.load(wov[:, 4 * u:4 * u + 4, cbk * 512:(cbk + 1) * 512], w_[:, 4 * u:4 * u + 4, :], w_, shape3=(4, 512))
                    for ti in range(TPC):
                        p = PB[nev % 6]
                        hx = hb_[nev % 2]
                        po = pr[nev % 2]
                        nev += 1
                        k.dma("sp", hx[:], h_d[rsl(ti), cbk * 512:(cbk + 1) * 512], writes=[hx])
                        for c in range(16):
                            k.op("pe", lambda e: e.matmul(p[:], lhsT=bufA[:, c, tsl(ti)], rhs=w_[:, c, :], start=(c == 0), stop=(c == 15)),
                                 reads=[A[ti], w_], writes=[p])
                        k.op("dve", lambda e: e.scalar_tensor_tensor(out=po[:], in0=hx[:], scalar=A_ALPHA, in1=p[:], op0=ALU.mult, op1=ALU.add),
                             reads=[hx, p], writes=[po])
                        k.dma("pool", pre1_d[tsl(ti), cbk * 512:(cbk + 1) * 512], po[:], reads=[po])
                k.barrier()
            if stop_after == 3:
                k.finish()
                return k
            with ExitStack() as st:
                g1 = k.sb("g1", [128, D], F32, st)
                b1 = k.sb("b1", [128, D], F32, st)
                wr = k.sb("wr", [128, 16, 36], F32, st)
                br = k.sb("br", [128, 36], F32, st)
                xt = [k.sb("x4_%d" % i, [128, D], F32, st) for i in range(2)]
                yt = [k.sb("y4_%d" % i, [128, D], F32, st) for i in range(2)]
                sq = k.sb("sq4", [128, D], BF16, st)
                s8 = k.sb("s8", [128, 8], F32, st)
                hTf = k.sb("hTf", [128, 16, 128], F32, st)
                lg = k.sb("lg", [128, 36], F32, st)
                em = k.sb("em", [128, 32], F32, st)
                r = k.sb("r", [128, 32], F32, st)
                m8 = k.sb("m8", [128, 8], F32, st)
                G1 = k.sb("G1", [128, 32], F32, st)
                G2 = k.sb("G2", [128, 32], F32, st)
                k.dma("sp", g1[:], g1_d, writes=[g1])
                k.dma("sp", b1[:], b1_d, writes=[b1])
                k.dma("sp", wr[:], wr_d.rearrange("(c p) n -> p c n", p=128), writes=[wr])
                k.dma("sp", br[:], br_d, writes=[br])
                for ti in range(TPC):
                    x, y = xt[ti % 2], yt[ti % 2]
                    k.dma("sp", x[:], pre1_d[tsl(ti), :], writes=[x])
                    _ln_rows(k, x, y, s8, sq, g1, b1)
                    k.dma("pool", h1_d[tsl(ti), :], y[:], reads=[y])
                    import os
                    P4 = int(os.environ.get("P4", "9"))
                    if P4 < 1:
                        continue
                    for g4 in range(4):
                        for j in range(4):
                            c = g4 * 4 + j
                            k.op("pe", lambda e: e.transpose(out=PT32[:, j, :], in_=y[:, c * 128:(c + 1) * 128], identity=idf[:]),
                                 reads=[y, idf], writes=[PT32])
                        k.op("dve", lambda e: e.tensor_copy(out=hTf[:, g4 * 4:(g4 + 1) * 4, :], in_=PT32[:]), reads=[PT32], writes=[hTf])
                        k.op("dve", lambda e: e.tensor_copy(out=bufA[:, g4 * 4:(g4 + 1) * 4, tsl(ti)], in_=PT32[:]), reads=[PT32], writes=[A[ti]])
                    if P4 < 2:
                        continue
                    p = PB[ti % 6]
                    for c in range(16):
                        k.op("pe", lambda e: e.matmul(p[:, 0:36], lhsT=hTf[:, c, :], rhs=wr[:, c, :], start=(c == 0), stop=(c == 15)),
                             reads=[hTf, wr], writes=[p])
                    k.pe_fence(idb, [p], PB[(ti + 3) % 6], 512, reps=8)
                    k.op("dve", lambda e: e.tensor_tensor(out=lg[:], in0=p[:, 0:36], in1=br[:], op=ALU.add), reads=[p, br], writes=[lg])
                    if P4 < 3:
                        continue
                    k.op("dve", lambda e: e.tensor_reduce(out=r[:, 0:1], in_=lg[:, 0:4], op=ALU.max, axis=AX.X), reads=[lg], writes=[r])
                    k.op("dve", lambda e: e.tensor_scalar(out=r[:, 1:2], in0=r[:, 0:1], scalar1=-1.0, scalar2=None, op0=ALU.mult), reads=[r], writes=[r])
                    k.op("dve", lambda e: e.tensor_scalar(out=r[:, 4:8], in0=lg[:, 0:4], scalar1=r[:, 0:1], scalar2=None, op0=ALU.is_equal),
                         reads=[lg, r], writes=[r])
                    k.op("act", lambda e: e.activation(out=r[:, 12:16], in_=lg[:, 0:4], func=AF.Exp, bias=r[:, 1:2], scale=1.0),
                         reads=[lg, r], writes=[r])
                    k.op("dve", lambda e: e.tensor_reduce(out=r[:, 2:3], in_=r[:, 12:16], op=ALU.add, axis=AX.X), reads=[r], writes=[r])
                    k.op("dve", lambda e: e.reciprocal(out=r[:, 3:4], in_=r[:, 2:3]), reads=[r], writes=[r])
                    k.op("dve", lambda e: e.tensor_scalar(out=r[:, 8:12], in0=r[:, 4:8], scalar1=-1.0, scalar2=1.0e30, op0=ALU.add, op1=ALU.mult),
                         reads=[r], writes=[r])
                    for gi in range(4):
                        k.op("dve", lambda e: e.tensor_scalar(out=em[:, gi * 8:(gi + 1) * 8], in0=lg[:, 4 + gi * 8:12 + gi * 8],
                                                              scalar1=r[:, 8 + gi:9 + gi], scalar2=None, op0=ALU.add), reads=[lg, r], writes=[em])
                    k.op("dve", lambda e: e.max(out=m8[:], in_=em[:]), reads=[em], writes=[m8])
                    k.op("dve", lambda e: e.tensor_tensor(out=r[:, 16:17], in0=m8[:, 1:2], in1=m8[:, 0:1], op=ALU.subtract), reads=[m8], writes=[r])
                    k.op("act", lambda e: e.activation(out=r[:, 17:18], in_=r[:, 16:17], func=AF.Exp), reads=[r], writes=[r])
                    k.op("dve", lambda e: e.tensor_scalar(out=r[:, 18:19], in0=r[:, 17:18], scalar1=1.0, scalar2=None, op0=ALU.add), reads=[r], writes=[r])
                    k.op("dve", lambda e: e.reciprocal(out=r[:, 19:20], in_=r[:, 18:19]), reads=[r], writes=[r])
                    k.op("dve", lambda e: e.tensor_tensor(out=r[:, 20:21], in0=r[:, 17:18], in1=r[:, 19:20], op=ALU.mult), reads=[r], writes=[r])
                    k.op("dve", lambda e: e.tensor_scalar(out=r[:, 21:23], in0=r[:, 19:21], scalar1=r[:, 3:4], scalar2=None, op0=ALU.mult),
                         reads=[r], writes=[r])
                    k.op("dve", lambda e: e.tensor_scalar(out=G1[:], in0=em[:], scalar1=m8[:, 0:1], scalar2=r[:, 21:22],
                                                          op0=ALU.is_equal, op1=ALU.mult), reads=[em, m8, r], writes=[G1])
                    k.op("dve", lambda e: e.tensor_scalar(out=G2[:], in0=em[:], scalar1=m8[:, 1:2], scalar2=r[:, 22:23],
                                                          op0=ALU.is_equal, op1=ALU.mult), reads=[em, m8, r], writes=[G2])
                    k.op("dve", lambda e: e.tensor_tensor(out=Gall[:, ti, :], in0=G1[:], in1=G2[:], op=ALU.add), reads=[G1, G2], writes=[Gall])
                k.barrier()
            if stop_after == 4:
                k.finish()
                return k
            with ExitStack() as st:
                stg = Stager(k, st)
                wgu = [k.sb("wgu%d" % i, [128, 16, 2, 128], BF16, st) for i in range(3)]
                wdn = [k.sb("wdn%d" % i, [128, 4, D], BF16, st) for i in range(2)]
                hbT = [k.sb("hbT%d" % i, [128, 4, 512], BF16, st) for i in range(2)]
                sgl = [k.sb("sgl%d" % i, [128, 512], F32, st) for i in range(2)]
                acc = k.sb("acc", [128, 4, D], F32, st)
                wguv = wgu_d.rearrange("e (c p) n -> e p c n", p=128)
                wdv = wd_d.rearrange("e (c p) n -> e p c n", p=128)
                npc = 0
                nex = 0
                nev = 0
                for t0 in range(0, TPC, 4):
                    nt_ = min(4, TPC - t0)
                    ntk = nt_ * 128
                    tok = slice(t0 * 128, t0 * 128 + ntk)
                    Ar = [A[t0 + i] for i in range(nt_)]
                    k.op("pool", lambda e: e.memset(acc[:], 0.0), writes=[acc])
                    for ex in range(32):
                        hb = hbT[nex % 2]
                        wd_ = wdn[nex % 2]
                        nex += 1
                        for fc in range(4):
                            stg.load(wdv[ex, :, fc, :], wd_[:, fc, :], wd_)
                        for fc in range(4):
                            wp = wgu[npc % 3]
                            npc += 1
                            stg.load(wguv[ex, :, :, fc * 128:(fc + 1) * 128], wp[:, :, 0, :], wp, shape3=(16, 128))
                            stg.load(wguv[ex, :, :, 512 + fc * 128:512 + (fc + 1) * 128], wp[:, :, 1, :], wp, shape3=(16, 128))
                            pg_, pu_ = PB[(nev % 2) * 2], PB[(nev % 2) * 2 + 1]
                            sg_ = sgl[nev % 2]
                            nev += 1
                            for c in range(16):
                                k.op("pe", lambda e: e.matmul(pg_[:, 0:ntk], lhsT=wp[:, c, 0, :], rhs=bufA[:, c, tok], start=(c == 0), stop=(c == 15)),
                                     reads=[wp] + Ar, writes=[pg_])
                            for c in range(16):
                                k.op("pe", lambda e: e.matmul(pu_[:, 0:ntk], lhsT=wp[:, c, 1, :], rhs=bufA[:, c, tok], start=(c == 0), stop=(c == 15)),
                                     reads=[wp] + Ar, writes=[pu_])
                            k.op("act", lambda e: e.activation(out=sg_[:, 0:ntk], in_=pg_[:, 0:ntk], func=AF.Silu), reads=[pg_], writes=[sg_])
                            k.op("dve", lambda e: e.tensor_tensor(out=hb[:, fc, 0:ntk], in0=sg_[:, 0:ntk], in1=pu_[:, 0:ntk], op=ALU.mult),
                                 reads=[sg_, pu_], writes=[hb])
                        for tl in range(nt_):
                            for db in range(4):
                                po = PB[4 + (nev % 2)]
                                nev += 1
                                for fc in range(4):
                                    k.op("pe", lambda e: e.matmul(po[:], lhsT=hb[:, fc, tl * 128:(tl + 1) * 128], rhs=wd_[:, fc, db * 512:(db + 1) * 512],
                                                                  start=(fc == 0), stop=(fc == 3)), reads=[hb, wd_], writes=[po])
                                k.op("dve", lambda e: e.scalar_tensor_tensor(out=acc[:, tl, db * 512:(db + 1) * 512], in0=po[:],
                                                                             scalar=Gall[:, t0 + tl, ex:ex + 1], in1=acc[:, tl, db * 512:(db + 1) * 512],
                                                                             op0=ALU.mult, op1=ALU.add), reads=[po, Gall, acc], writes=[acc])
                    for tl in range(nt_):
                        k.dma("pool", moe_d[tsl(t0 + tl), :], acc[:, tl, :], reads=[acc])
                k.barrier()

        if stop_after == 5:
            k.finish()
            return k
        with ExitStack() as st:
            g2 = k.sb("g2", [128, D], F32, st)
            b2 = k.sb("b2", [128, D], F32, st)
            xt = [k.sb("x6_%d" % i, [128, D], F32, st) for i in range(2)]
            mo_ = [k.sb("m6_%d" % i, [128, D], F32, st) for i in range(2)]
            yt = [k.sb("y6_%d" % i, [128, D], F32, st) for i in range(2)]
            yb = k.sb("yb6", [128, D], BF16, st)
            sq = k.sb("sq6", [128, D], BF16, st)
            s8 = k.sb("s86", [128, 8], F32, st)
            k.dma("sp", g2[:], g2_d, writes=[g2])
            k.dma("sp", b2[:], b2_d, writes=[b2])
            for ti in range(TPC):
                x, y = xt[ti % 2], yt[ti % 2]
                if first:
                    k.dma("sp", x[:], xin_d[rsl(ti), :], writes=[x])
                else:
                    m_ = mo_[ti % 2]
                    k.dma("sp", x[:], h1_d[tsl(ti), :], writes=[x])
                    k.dma("sp", m_[:], moe_d[tsl(ti), :], writes=[m_])
                    k.op("dve", lambda e: e.scalar_tensor_tensor(out=x[:], in0=x[:], scalar=A_ALPHA, in1=m_[:], op0=ALU.mult, op1=ALU.add),
                         reads=[x, m_], writes=[x])
                _ln_rows(k, x, y, s8, sq, g2, b2)
                k.dma("pool", hout_d[rsl(ti), :], y[:], reads=[y], is_output=True)
                if not last:
                    k.op("act", lambda e: e.copy(out=yb[:], in_=y[:]), reads=[y], writes=[yb])
                    to_bufA(yb, ti)
            k.barrier()
        with ExitStack() as st:
            if not last:
                stg = Stager(k, st)
                wb = [k.sb("wb%d" % i, [128, 16, 512], BF16, st) for i in range(2)]
                ot32 = [k.sb("ot32_%d" % i, [128, 512], F32, st) for i in range(2)]
                ot16 = [k.sb("ot16_%d" % i, [128, 512], BF16, st) for i in range(3)]
                wv = wn_d.rearrange("(c p) n -> p c n", p=128)
                c32 = c16 = nev = 0
                for bi, (cs, cw_, isb) in enumerate(PA_BLOCKS):
                    w_ = wb[bi % 2]
                    for u in range(4):
                        stg.load(wv[:, 4 * u:4 * u + 4, cs:cs + cw_], w_[:, 4 * u:4 * u + 4, 0:cw_], w_, shape3=(4, cw_))
                    for ti in range(TPC):
                        p = PB[nev % 6]
                        for c in range(16):
                            k.op("pe", lambda e: e.matmul(p[:, 0:cw_], lhsT=bufA[:, c, tsl(ti)], rhs=w_[:, c, 0:cw_], start=(c == 0), stop=(c == 15)),
                                 reads=[A[ti], w_], writes=[p])
                        if isb:
                            o = ot16[nev % 3]
                            dst = o16_d[rsl(ti), c16:c16 + cw_]
                        else:
                            o = ot32[nev % 2]
                            dst = o32_d[rsl(ti), c32:c32 + cw_]
                        if nev % 2 == 0:
                            k.op("act", lambda e: e.copy(out=o[:, 0:cw_], in_=p[:, 0:cw_]), reads=[p], writes=[o])
                        else:
                            k.op("dve", lambda e: e.tensor_copy(out=o[:, 0:cw_], in_=p[:, 0:cw_]), reads=[p], writes=[o])
                        k.dma("pool", dst, o[:, 0:cw_], reads=[o], is_output=True)
                        nev += 1
                    if isb:
                        c16 += cw_
                    else:
                        c32 += cw_
            k.barrier()
    k.finish()
    return k


def _rep(v, n=128):
    return np.ascontiguousarray(np.broadcast_to(np.asarray(v, np.float32), (n,) + np.shape(v)))


def pba_inputs(l, P, h, hm_tok, yaT, last):
    w_in = P["w_in"][l]
    wg = np.ascontiguousarray(np.concatenate([w_in[:, 2048:3072], w_in[:, 6736:10832]], axis=1))
    wr = np.ascontiguousarray(np.concatenate([P["w_group"][l], P["w_router"][l]], axis=1))
    br = _rep(np.concatenate([P["b_group"][l], P["b_router"][l]]))
    common = {"ident": np.eye(128, dtype=np.float32), "wg": wg, "mhg": _rep(P["mh_norm_g"][l]),
              "wpm": P["w_proj_m"][l], "wpa": P["w_proj_a"][l], "wout": P["w_out"][l],
              "g1": _rep(P["ln1_g"][l]), "b1": _rep(P["ln1_b"][l]), "wr": wr, "br": br,
              "wgu": P["w_gate_up"][l], "wd": P["w_down"][l],
              "g2": _rep(P["ln2_g"][l]), "b2": _rep(P["ln2_b"][l])}
    if not last:
        common["wn"] = P["w_in"][l + 1]
    maps = []
    for c in range(NCORES):
        m = dict(common)
        m["h"] = h[c * NTOK:(c + 1) * NTOK]
        m["hm"] = hm_tok[c * NTOK:(c + 1) * NTOK]
        m["yaT"] = np.ascontiguousarray(yaT[:, c * NTOK:(c + 1) * NTOK])
        maps.append(m)
    return maps


_PROGS = {}


def _prog(name, fn):
    if name not in _PROGS:
        _PROGS[name] = fn()
    return _PROGS[name]


PB_CORES = 4
PB_NVB = NCORES // PB_CORES


def _pba_maps(l, P, h, hm_tok, yaT, last):
    w_in = P["w_in"][l]
    wg = np.ascontiguousarray(np.concatenate([w_in[:, 2048:3072], w_in[:, 6736:10832]], axis=1))
    wr = np.ascontiguousarray(np.concatenate([P["w_group"][l], P["w_router"][l]], axis=1))
    br = _rep(np.concatenate([P["b_group"][l], P["b_router"][l]]))
    com = {"ident": np.eye(128, dtype=np.float32), "wg": wg, "mhg": _rep(P["mh_norm_g"][l]),
           "wpm": P["w_proj_m"][l], "wpa": P["w_proj_a"][l], "wout": P["w_out"][l],
           "g1": _rep(P["ln1_g"][l]), "b1": _rep(P["ln1_b"][l]), "wr": wr, "br": br,
           "wgu": P["w_gate_up"][l], "wd": P["w_down"][l],
           "g2": _rep(P["ln2_g"][l]), "b2": _rep(P["ln2_b"][l])}
    if not last:
        com["wn"] = P["w_in"][l + 1]
    n = PB_NVB * NTOK
    maps = []
    for c in range(PB_CORES):
        m = dict(com)
        m["h"] = h[c * n:(c + 1) * n]
        m["hm"] = hm_tok[c * n:(c + 1) * n]
        m["yaT"] = np.ascontiguousarray(yaT[:, c * n:(c + 1) * n])
        maps.append(m)
    return maps


def kernel(**inputs):
    import time
    t00 = time.time()
    P = {k_: np.asarray(v) for k_, v in inputs.items()}
    x = P["x"][0]
    xin = np.zeros((LP, D), np.float32)
    xin[:N_META] = P["meta_tokens"]
    xin[N_META:L] = x
    k0 = _prog("pa0", lambda: build_pba(first=True, NVB=PB_NVB))
    n_ = PB_NVB * NTOK
    res = run(k0, [{"ident": np.eye(128, dtype=np.float32), "xin": xin[c * n_:(c + 1) * n_], "g2": _rep(P["ln_emb_g"]),
                    "b2": _rep(P["ln_emb_b"]), "wn": P["w_in"][0]} for c in range(PB_CORES)])
    h, o16, o32 = (np.concatenate([r[nm] for r in res], axis=0) for nm in ("hout", "o16", "o32"))
    print("[kernel] pa0 done %.1fs" % (time.time() - t00), flush=True)
    for l in range(DEPTH):
        last = (l == DEPTH - 1)
        mq, mk, mv = o16[:, 0:512], o16[:, 512:1024], o16[:, 1024:2048]
        aq, ak, av = o16[:, 2048:3072], o16[:, 3072:4096], o16[:, 4096:5120]
        iq, ik = o16[:, 5120:5632], o16[:, 5632:5696]
        mi, mf, iw = o32[:, 0:4], o32[:, 4:8], np.ascontiguousarray(o32[:, 8:16])
        kml = _prog("ml", build_ml)
        rml = run(kml, ml_inputs(mq, mk, mv, mi, mf, P["conv_w"][l], P["conv_b"][l], P["b_igate"][l], P["b_fgate"][l]))
        hm_tok = np.ascontiguousarray(np.concatenate([r["hm"] for r in rml], axis=0).T)
        print("[kernel] L%d ml done %.1fs" % (l, time.time() - t00), flush=True)
        kat = _prog("at", build_at)
        rat = run(kat, at_inputs(np.ascontiguousarray(aq), np.ascontiguousarray(ak), np.ascontiguousarray(av),
                                 np.ascontiguousarray(iq), np.ascontiguousarray(ik), iw, P["rel_bias"]))
        ya = np.stack([r["ya"] for r in rat], axis=0)
        yaT = np.ascontiguousarray(ya.transpose(2, 3, 1, 0, 4)).reshape(1024, LP)
        print("[kernel] L%d at done %.1fs" % (l, time.time() - t00), flush=True)
        kpb = _prog("pbl" if last else "pbm",
                    (lambda: build_pba(last=True, NVB=PB_NVB)) if last else (lambda: build_pba(NVB=PB_NVB)))
        res = run(kpb, _pba_maps(l, P, h, hm_tok, yaT, last))
        h = np.concatenate([r["hout"] for r in res], axis=0)
        if not last:
            o16 = np.concatenate([r["o16"] for r in res], axis=0)
            o32 = np.concatenate([r["o32"] for r in res], axis=0)
        print("[kernel] L%d pba done %.1fs" % (l, time.time() - t00), flush=True)
    out = np.ascontiguousarray(h[N_META:L]).reshape(1, SEQ, D).astype(np.float32)
    return out
```
